# Optimizing a Trainium2 kernel written in Bass

```python
import math
import jax
import jax.numpy as jnp
from jax import lax
import numpy as np

D_MODEL = 2048
BATCH = 1
SEQ = 8192
DEPTH = 2

N_EVEN = (DEPTH + 1) // 2
N_ODD = DEPTH // 2

NSA_HEADS = 8
NSA_KV_HEADS = 2
NSA_HPG = NSA_HEADS // NSA_KV_HEADS
HEAD_DIM = 128
NSA_KV_W = NSA_KV_HEADS * HEAD_DIM
CMP_LEN = 32
CMP_STRIDE = 16
SLC_LEN = 64
SLC_TOPN = 16
WINDOW = 512
Q_BLOCK = 128

LRU_WIDTH = 1024
LRU_BLOCKS = 8
LRU_BW = LRU_WIDTH // LRU_BLOCKS
CONV_W = 4
LRU_C = 8.0

EVEN_COLS = NSA_HEADS * HEAD_DIM + 6 * NSA_KV_W + 3 * NSA_HEADS + 2 * LRU_WIDTH
EVEN_MIX_OUT = NSA_HEADS * HEAD_DIM + LRU_WIDTH

HG_HEADS = 16
HG_DK = 128
HG_DV = 128
HG_CHUNK = 64
HG_KW = HG_HEADS * HG_DK
HG_VW = HG_HEADS * HG_DV
ODD_COLS = 2 * HG_KW + 2 * HG_VW

N_BUCKETS = 32
MAX_DIST = 128

N_GROUPS = 8
EXP_PER_GROUP = 8
N_EXPERTS = N_GROUPS * EXP_PER_GROUP
TOPK_IN_GROUP = 2
D_EXPERT = 512
MOE_BLOCK = 128

EPS = 1e-6

kernel_name = 'hybrid_nsa_rglru_hgrn2_hmoe'


def rmsnorm(x, g):
    x32 = x.astype(jnp.float32)
    y = x32 * lax.rsqrt(jnp.mean(x32 * x32, axis=-1, keepdims=True) + EPS)
    return (y * g.astype(jnp.float32)).astype(x.dtype)


def t5_bucket(rel):
    n = jnp.maximum(rel, 0)
    max_exact = N_BUCKETS // 2
    nf = jnp.maximum(n, 1).astype(jnp.float32)
    large = max_exact + (jnp.log(nf / max_exact) / math.log(MAX_DIST / max_exact)
                         * (N_BUCKETS - max_exact)).astype(jnp.int32)
    large = jnp.minimum(large, N_BUCKETS - 1)
    return jnp.where(n < max_exact, n, large)


def masked_softmax(s, mask):
    s = jnp.where(mask, s.astype(jnp.float32), -jnp.inf)
    m = jnp.max(s, axis=-1, keepdims=True)
    m = jnp.where(jnp.isfinite(m), m, 0.0)
    e = jnp.exp(s - m)
    d = jnp.sum(e, axis=-1, keepdims=True)
    return e / jnp.where(d > 0, d, 1.0)


def compress_blocks(k, pe, w1, b1, w2, b2):
    B, G, T, hd = k.shape
    n_cmp = (T - CMP_LEN) // CMP_STRIDE + 1
    idx = jnp.arange(n_cmp)[:, None] * CMP_STRIDE + jnp.arange(CMP_LEN)[None, :]
    blocks = (k[:, :, idx] + pe).reshape(B, G, n_cmp, CMP_LEN * hd)
    hid = jax.nn.gelu(blocks @ w1 + b1)
    return hid @ w2 + b2


def nsa_rglru_mixer(h, rel_bias, w_in, w_out, cmp_pe, cmp_w1, cmp_b1, cmp_w2, cmp_b2,
                    conv_w, conv_b, lru_wa, lru_ba, lru_wi, lru_bi, lru_lambda):
    B, T, _ = h.shape
    G, HPG, HD = NSA_KV_HEADS, NSA_HPG, HEAD_DIM
    sizes = (NSA_HEADS * HD,) + (NSA_KV_W,) * 6 + (3 * NSA_HEADS, LRU_WIDTH, LRU_WIDTH)
    cuts = []
    acc = 0
    for s in sizes[:-1]:
        acc += s
        cuts.append(acc)
    q, k_c, v_c, k_s, v_s, k_w, v_w, gate_logit, y_br, x_br = jnp.split(h @ w_in, cuts, axis=-1)

    q = q.reshape(B, T, G, HPG, HD).transpose(0, 2, 3, 1, 4) * (HD ** -0.5)

    def kv_heads(t):
        return t.reshape(B, T, G, HD).transpose(0, 2, 1, 3)

    k_c, v_c, k_s, v_s, k_w, v_w = [kv_heads(t) for t in (k_c, v_c, k_s, v_s, k_w, v_w)]
    gates = jax.nn.sigmoid(gate_logit.reshape(B, T, G, HPG, 3).transpose(0, 2, 3, 1, 4))

    k_cmp = compress_blocks(k_c, cmp_pe[0], cmp_w1[0], cmp_b1[0], cmp_w2[0], cmp_b2[0])
    v_cmp = compress_blocks(v_c, cmp_pe[1], cmp_w1[1], cmp_b1[1], cmp_w2[1], cmp_b2[1])
    n_cmp = k_cmp.shape[2]
    n_slc = T // SLC_LEN
    n_top = min(SLC_TOPN, n_slc)
    ks_blk = k_s.reshape(B, G, n_slc, SLC_LEN, HD)
    vs_blk = v_s.reshape(B, G, n_slc, SLC_LEN, HD)
    pad = ((0, 0), (0, 0), (WINDOW, 0), (0, 0))
    kw_pad = jnp.pad(k_w, pad)
    vw_pad = jnp.pad(v_w, pad)

    cmp_start = jnp.arange(n_cmp) * CMP_STRIDE
    cmp_end = cmp_start + CMP_LEN - 1
    slc_start = jnp.arange(n_slc) * SLC_LEN
    cover = ((cmp_start[:, None] < slc_start[None, :] + SLC_LEN)
             & (cmp_start[:, None] + CMP_LEN > slc_start[None, :])).astype(jnp.float32)

    tbl = rel_bias.reshape(N_BUCKETS, G, HPG)
    tbl_g = tbl.transpose(1, 0, 2)
    g_idx = jnp.arange(G)[None, :, None, None]

    def static_bias(rel):
        return tbl[t5_bucket(rel)].transpose(2, 3, 0, 1)

    gather = jax.vmap(jax.vmap(lambda blocks, ix: blocks[ix]))

    def chunk(ci):
        s0 = ci * Q_BLOCK
        qc = lax.dynamic_slice_in_dim(q, s0, Q_BLOCK, axis=3)
        gc = lax.dynamic_slice_in_dim(gates, s0, Q_BLOCK, axis=3)
        t = s0 + jnp.arange(Q_BLOCK)
        rel_c = t[:, None] - cmp_end[None, :]
        sc = jnp.einsum('bghqd,bgnd->bghqn', qc, k_cmp) + static_bias(rel_c)
        p_cmp = masked_softmax(sc, rel_c >= 0)
        o_cmp = jnp.einsum('bghqn,bgnd->bghqd', p_cmp.astype(v_cmp.dtype), v_cmp)
        imp = jnp.einsum('bghqn,nj->bgqj', p_cmp, cover)
        cur = (t // SLC_LEN)[:, None]
        blk = jnp.arange(n_slc)[None, :]
        imp = jnp.where(blk == cur, jnp.inf, jnp.where(blk > cur, -jnp.inf, imp))
        _, sel = lax.top_k(imp, n_top)
        k_sel = gather(ks_blk, sel).reshape(B, G, Q_BLOCK, n_top * SLC_LEN, HD)
        v_sel = gather(vs_blk, sel).reshape(B, G, Q_BLOCK, n_top * SLC_LEN, HD)
        kpos = (sel[..., None] * SLC_LEN + jnp.arange(SLC_LEN)).reshape(B, G, Q_BLOCK, n_top * SLC_LEN)
        rel_s = t[None, None, :, None] - kpos
        bias_s = tbl_g[g_idx, t5_bucket(rel_s)].transpose(0, 1, 4, 2, 3)
        ss = jnp.einsum('bghqd,bgqkd->bghqk', qc, k_sel) + bias_s
        p_s = masked_softmax(ss, (rel_s >= 0)[:, :, None])
        o_slc = jnp.einsum('bghqk,bgqkd->bghqd', p_s.astype(v_sel.dtype), v_sel)
        kwc = lax.dynamic_slice_in_dim(kw_pad, s0, Q_BLOCK + WINDOW, axis=2)
        vwc = lax.dynamic_slice_in_dim(vw_pad, s0, Q_BLOCK + WINDOW, axis=2)
        kpos_w = s0 - WINDOW + jnp.arange(Q_BLOCK + WINDOW)
        rel_w = t[:, None] - kpos_w[None, :]
        mask_w = (rel_w >= 0) & (rel_w < WINDOW) & (kpos_w >= 0)[None, :]
        sw = jnp.einsum('bghqd,bgkd->bghqk', qc, kwc) + static_bias(rel_w)
        p_w = masked_softmax(sw, mask_w)
        o_win = jnp.einsum('bghqk,bgkd->bghqd', p_w.astype(vwc.dtype), vwc)
        return gc[..., 0:1] * o_cmp + gc[..., 1:2] * o_slc + gc[..., 2:3] * o_win

    o = lax.map(chunk, jnp.arange(T // Q_BLOCK))
    nsa_out = o.transpose(1, 0, 4, 2, 3, 5).reshape(B, T, NSA_HEADS * HD).astype(h.dtype)

    xc = lax.conv_general_dilated(x_br, conv_w[:, None, :], window_strides=(1,),
                                  padding=[(CONV_W - 1, 0)],
                                  dimension_numbers=('NWC', 'WIO', 'NWC'),
                                  feature_group_count=LRU_WIDTH) + conv_b
    xg = xc.reshape(B, T, LRU_BLOCKS, LRU_BW)
    r_gate = jax.nn.sigmoid((jnp.einsum('btnd,nde->btne', xg, lru_wa).reshape(B, T, LRU_WIDTH)
                             + lru_ba).astype(jnp.float32))
    i_gate = jax.nn.sigmoid((jnp.einsum('btnd,nde->btne', xg, lru_wi).reshape(B, T, LRU_WIDTH)
                             + lru_bi).astype(jnp.float32))
    log_a = -LRU_C * r_gate * jax.nn.softplus(-lru_lambda.astype(jnp.float32))
    a = jnp.exp(log_a)
    b = jnp.sqrt(-jnp.expm1(2.0 * log_a)) * (i_gate * xc.astype(jnp.float32))

    def combine(left, right):
        a1, b1 = left
        a2, b2 = right
        return a1 * a2, a2 * b1 + b2

    _, hseq = lax.associative_scan(combine, (a, b), axis=1)
    lru_out = (jax.nn.gelu(y_br.astype(jnp.float32)) * hseq).astype(h.dtype)

    return jnp.concatenate([nsa_out, lru_out], axis=-1) @ w_out


def hgrn2_chunked(q, k, v, log_f):
    B, T, H, DK = q.shape
    DV = v.shape[-1]
    C = HG_CHUNK
    nc = T // C

    def chunks(t):
        return t.reshape(B, nc, C, H, t.shape[-1]).transpose(1, 0, 3, 2, 4)

    causal = jnp.tril(jnp.ones((C, C), dtype=bool))[:, :, None]

    def step(S, inp):
        qc, kc, vc, gc = inp
        bcum = jnp.cumsum(gc, axis=2)
        o_inter = jnp.einsum('bhtd,bhde->bhte', qc * jnp.exp(bcum), S)
        diff = jnp.where(causal, bcum[:, :, :, None, :] - bcum[:, :, None, :, :], -jnp.inf)
        att = jnp.einsum('bhtd,bhsd,bhtsd->bhts', qc, kc, jnp.exp(diff))
        o_intra = jnp.einsum('bhts,bhse->bhte', att, vc)
        b_last = bcum[:, :, -1:, :]
        S = (jnp.exp(b_last[:, :, 0, :])[..., None] * S
             + jnp.einsum('bhsd,bhse->bhde', kc * jnp.exp(b_last - bcum), vc))
        return S, o_inter + o_intra

    S0 = jnp.zeros((B, H, DK, DV), jnp.float32)
    _, o = lax.scan(step, S0, (chunks(q), chunks(k), chunks(v), chunks(log_f)))
    return o.transpose(1, 0, 3, 2, 4).reshape(B, T, H, DV)


def hgrn2_mixer(h, lb, w_in, w_out, norm_g):
    B, T, _ = h.shape
    proj = h @ w_in
    q, fz, iv, g = jnp.split(proj, [HG_KW, 2 * HG_KW, 2 * HG_KW + HG_VW], axis=-1)
    q = jax.nn.silu(q.astype(jnp.float32)).reshape(B, T, HG_HEADS, HG_DK)
    lb = lb.reshape(HG_HEADS, HG_DK)
    f = lb + (1.0 - lb) * jax.nn.sigmoid(fz.astype(jnp.float32).reshape(B, T, HG_HEADS, HG_DK))
    log_f = jnp.log(jnp.maximum(f, 1e-30))
    v = iv.astype(jnp.float32).reshape(B, T, HG_HEADS, HG_DV)
    o = hgrn2_chunked(q, 1.0 - f, v, log_f)
    o = o * lax.rsqrt(jnp.mean(o * o, axis=-1, keepdims=True) + EPS)
    o = o * norm_g.astype(jnp.float32).reshape(HG_HEADS, HG_DV)
    o = o.reshape(B, T, HG_VW) * jax.nn.silu(g.astype(jnp.float32))
    return o.astype(h.dtype) @ w_out


def hier_moe(h, w_grp, b_grp, w_exp, b_exp, w_gate, w_up, w_down):
    B, T, D = h.shape
    xt = h.reshape(-1, D)
    N = xt.shape[0]
    glog = (xt @ w_grp + b_grp).astype(jnp.float32)
    gprob = jax.nn.softmax(glog, axis=-1)
    grp = jnp.argmax(glog, axis=-1)
    p_grp = jnp.take_along_axis(gprob, grp[:, None], axis=-1)
    elog = (xt @ w_exp + b_exp).astype(jnp.float32).reshape(N, N_GROUPS, EXP_PER_GROUP)
    elog = jnp.take_along_axis(elog, grp[:, None, None], axis=1)[:, 0]
    top_v, top_i = lax.top_k(elog, TOPK_IN_GROUP)
    gate = p_grp * jax.nn.softmax(top_v, axis=-1)
    eid = grp[:, None] * EXP_PER_GROUP + top_i

    A = N * TOPK_IN_GROUP
    e_flat = eid.reshape(-1)
    tok = jnp.repeat(jnp.arange(N, dtype=jnp.int32), TOPK_IN_GROUP)
    w_flat = gate.reshape(-1)
    order = jnp.argsort(e_flat)
    e_s, tok_s, w_s = e_flat[order], tok[order], w_flat[order]
    counts = jnp.zeros((N_EXPERTS,), jnp.int32).at[e_flat].add(1)
    start = jnp.cumsum(counts) - counts
    padded = (counts + MOE_BLOCK - 1) // MOE_BLOCK * MOE_BLOCK
    pad_end = jnp.cumsum(padded)
    pad_start = pad_end - padded
    dest = pad_start[e_s] + jnp.arange(A, dtype=jnp.int32) - start[e_s]
    n_blk = -(-(A + N_EXPERTS * MOE_BLOCK) // MOE_BLOCK)
    n_pad = n_blk * MOE_BLOCK
    buf_tok = jnp.full((n_pad,), N, jnp.int32).at[dest].set(tok_s)
    buf_w = jnp.zeros((n_pad,), jnp.float32).at[dest].set(w_s)
    blk_e = jnp.minimum(jnp.searchsorted(pad_end, jnp.arange(n_blk, dtype=jnp.int32) * MOE_BLOCK,
                                         side='right'), N_EXPERTS - 1)
    xpad = jnp.concatenate([xt, jnp.zeros((1, D), xt.dtype)], axis=0)
    xb = xpad[buf_tok].reshape(n_blk, MOE_BLOCK, D)

    def expert_block(args):
        xblk, e = args
        hid = jax.nn.silu(xblk @ w_gate[e]) * (xblk @ w_up[e])
        return hid @ w_down[e]

    yb = lax.map(expert_block, (xb, blk_e)).reshape(n_pad, D)
    out = jnp.zeros((N + 1, D), yb.dtype).at[buf_tok].add(yb * buf_w[:, None].astype(yb.dtype))
    return out[:N].reshape(B, T, D)


def setup_inputs(seed: int = 0) -> dict:
    key = jax.random.key(seed)
    keys = iter(jax.random.split(key, 40))

    def nrm(shape, scale):
        return jax.random.normal(next(keys), shape, jnp.float32) * scale

    D = D_MODEL
    E = N_EXPERTS
    u = jax.random.uniform(next(keys), (N_EVEN, LRU_WIDTH), jnp.float32, 0.9, 0.999)
    a_base = u ** (1.0 / LRU_C)
    return {
        'x': nrm((BATCH, SEQ, D), 1.0),
        'c': nrm((BATCH, D), 1.0),
        'rel_bias': nrm((N_BUCKETS, NSA_HEADS), 0.3),
        'ada_w': nrm((DEPTH, D, 6 * D), 0.5 * D ** -0.5),
        'ada_b': nrm((DEPTH, 6 * D), 0.01),
        'norm_mix_g': 1.0 + nrm((DEPTH, D), 0.01),
        'norm_ffn_g': 1.0 + nrm((DEPTH, D), 0.01),
        'ev_w_in': nrm((N_EVEN, D, EVEN_COLS), D ** -0.5),
        'ev_w_out': nrm((N_EVEN, EVEN_MIX_OUT, D), EVEN_MIX_OUT ** -0.5),
        'cmp_pe': nrm((N_EVEN, 2, CMP_LEN, HEAD_DIM), 0.1),
        'cmp_w1': nrm((N_EVEN, 2, CMP_LEN * HEAD_DIM, HEAD_DIM), (CMP_LEN * HEAD_DIM) ** -0.5),
        'cmp_b1': nrm((N_EVEN, 2, HEAD_DIM), 0.01),
        'cmp_w2': nrm((N_EVEN, 2, HEAD_DIM, HEAD_DIM), HEAD_DIM ** -0.5),
        'cmp_b2': nrm((N_EVEN, 2, HEAD_DIM), 0.01),
        'lru_conv_w': nrm((N_EVEN, CONV_W, LRU_WIDTH), CONV_W ** -0.5),
        'lru_conv_b': nrm((N_EVEN, LRU_WIDTH), 0.01),
        'lru_wa': nrm((N_EVEN, LRU_BLOCKS, LRU_BW, LRU_BW), LRU_BW ** -0.5),
        'lru_ba': nrm((N_EVEN, LRU_WIDTH), 0.01),
        'lru_wi': nrm((N_EVEN, LRU_BLOCKS, LRU_BW, LRU_BW), LRU_BW ** -0.5),
        'lru_bi': nrm((N_EVEN, LRU_WIDTH), 0.01),
        'lru_lambda': jnp.log(a_base) - jnp.log1p(-a_base),
        'od_w_in': nrm((N_ODD, D, ODD_COLS), D ** -0.5),
        'od_w_out': nrm((N_ODD, HG_VW, D), HG_VW ** -0.5),
        'hg_lb_logits': nrm((DEPTH, HG_KW), 1.0),
        'hg_norm_g': 1.0 + nrm((N_ODD, HG_VW), 0.01),
        'moe_w_grp': nrm((DEPTH, D, N_GROUPS), D ** -0.5),
        'moe_b_grp': nrm((DEPTH, N_GROUPS), 0.01),
        'moe_w_exp': nrm((DEPTH, D, E), D ** -0.5),
        'moe_b_exp': nrm((DEPTH, E), 0.01),
        'moe_w_gate': nrm((DEPTH, E, D, D_EXPERT), D ** -0.5),
        'moe_w_up': nrm((DEPTH, E, D, D_EXPERT), D ** -0.5),
        'moe_w_down': nrm((DEPTH, E, D_EXPERT, D), D_EXPERT ** -0.5),
        'final_g': 1.0 + nrm((D,), 0.01),
    }


def reference(x, c, rel_bias, ada_w, ada_b, norm_mix_g, norm_ffn_g, ev_w_in, ev_w_out,
              cmp_pe, cmp_w1, cmp_b1, cmp_w2, cmp_b2, lru_conv_w, lru_conv_b, lru_wa, lru_ba,
              lru_wi, lru_bi, lru_lambda, od_w_in, od_w_out, hg_lb_logits, hg_norm_g,
              moe_w_grp, moe_b_grp, moe_w_exp, moe_b_exp, moe_w_gate, moe_w_up, moe_w_down,
              final_g):
    lb_all = jnp.cumsum(jax.nn.softmax(hg_lb_logits.astype(jnp.float32), axis=0), axis=0)
    lb_all = lb_all - lb_all[0]
    c_act = jax.nn.silu(c)
    for l in range(DEPTH):
        mod = c_act @ ada_w[l] + ada_b[l]
        sh1, sc1, g1, sh2, sc2, g2 = [m[:, None, :] for m in jnp.split(mod, 6, axis=-1)]
        hmix = rmsnorm(x, norm_mix_g[l]) * (1.0 + sc1) + sh1
        j = l // 2
        if l % 2 == 0:
            y = nsa_rglru_mixer(hmix, rel_bias, ev_w_in[j], ev_w_out[j], cmp_pe[j], cmp_w1[j],
                                cmp_b1[j], cmp_w2[j], cmp_b2[j], lru_conv_w[j], lru_conv_b[j],
                                lru_wa[j], lru_ba[j], lru_wi[j], lru_bi[j], lru_lambda[j])
        else:
            y = hgrn2_mixer(hmix, lb_all[l], od_w_in[j], od_w_out[j], hg_norm_g[j])
        x = x + g1 * y
        hffn = rmsnorm(x, norm_ffn_g[l]) * (1.0 + sc2) + sh2
        x = x + g2 * hier_moe(hffn, moe_w_grp[l], moe_b_grp[l], moe_w_exp[l], moe_b_exp[l],
                              moe_w_gate[l], moe_w_up[l], moe_w_down[l])
    return rmsnorm(x, final_g)
```

```python
import math
import numpy as np
import concourse.bass as bass
import concourse.mybir as mybir

F32 = mybir.dt.float32
BF16 = mybir.dt.bfloat16
I32 = mybir.dt.int32
U32 = mybir.dt.uint32
AF = mybir.ActivationFunctionType
ALU = mybir.AluOpType
AX = mybir.AxisListType

ENGS = ("pe", "act", "dve", "pool", "sp")


class Buf:
    __slots__ = ("name", "w", "r")

    def __init__(self, name):
        self.name = name
        self.w = None
        self.r = []


class Prog:
    NDMA = 24

    def __init__(self):
        nc = bass.Bass("TRN2", target_bir_lowering=False)
        self.nc = nc
        self.eng = {"pe": nc.tensor, "act": nc.scalar, "dve": nc.vector, "pool": nc.gpsimd, "sp": nc.sync}
        self.ops = {e: [] for e in ENGS}
        self.sem = {e: nc.alloc_semaphore("c_" + e) for e in ENGS}
        self.cnt = {e: 0 for e in ENGS}
        self.seen = {e: {} for e in ENGS}
        self.dsem = [nc.alloc_semaphore("d%d" % i) for i in range(self.NDMA)]
        self.dval = [0] * self.NDMA
        self.dnext = 0
        self.nbuf = 0
        self.out_tokens = []

    def sb(self, name, shape, dt):
        return self.nc.alloc_sbuf_tensor("s_" + name, list(shape), dt)

    def ps(self, name, shape, dt=F32):
        return self.nc.alloc_psum_tensor("p_" + name, list(shape), dt)

    def buf(self, name=None):
        self.nbuf += 1
        return Buf(name or "b%d" % self.nbuf)

    def dram_in(self, name, shape, dt=F32):
        return self.nc.dram_tensor(name, list(shape), dt, kind="ExternalInput").ap()

    def dram_out(self, name, shape, dt=F32):
        return self.nc.dram_tensor(name, list(shape), dt, kind="ExternalOutput").ap()

    def dram_tmp(self, name, shape, dt=F32):
        return self.nc.dram_tensor(name, list(shape), dt, kind="Internal").ap()

    def _need(self, e, toks):
        waits = {}
        for t in toks:
            if t is None:
                continue
            key, sem, val = t
            if self.seen[e].get(key, 0) >= val:
                continue
            if key not in waits or waits[key][1] < val:
                waits[key] = (sem, val)
        for key, (sem, val) in waits.items():
            self.seen[e][key] = val
        return list(waits.values())

    def _deps(self, reads, writes):
        toks = []
        for b in reads:
            toks.append(b.w)
        for b in writes:
            toks.append(b.w)
            toks.extend(b.r)
        return toks

    def _commit(self, tok, reads, writes):
        for b in reads:
            b.r.append(tok)
        for b in writes:
            b.w = tok
            b.r = []

    def op(self, e, fn, reads=(), writes=()):
        waits = self._need(e, self._deps(reads, writes))
        self.cnt[e] += 1
        val = self.cnt[e]
        sem = self.sem[e]
        tok = ("c_" + e, sem, val)

        def emit(eng, waits=waits, fn=fn, sem=sem):
            for s, v in waits:
                eng.wait_ge(s, v)
            fn(eng).then_inc(sem, 1)
        self.ops[e].append(emit)
        self._commit(tok, reads, writes)
        return tok

    def dma(self, q, out, in_, reads=(), writes=(), is_output=False, **kw):
        k = self.dnext
        self.dnext = (self.dnext + 1) % self.NDMA
        dsem = self.dsem[k]
        prev = self.dval[k]
        self.dval[k] = prev + 16
        key = "d%d" % k
        toks = self._deps(reads, writes)
        if prev > 0:
            toks.append((key, dsem, prev))
        waits = self._need(q, toks)
        tok = (key, dsem, prev + 16)

        def emit(eng, waits=waits, out=out, in_=in_, dsem=dsem, kw=kw):
            for s, v in waits:
                eng.wait_ge(s, v)
            eng.dma_start(out=out, in_=in_, **kw).then_inc(dsem, 16)
        self.ops[q].append(emit)
        self._commit(tok, reads, writes)
        if is_output:
            self.out_tokens.append(tok)
        return tok

    def raw(self, e, fn):
        self.ops[e].append(fn)

    def mm(self, out, lhsT, rhs, start, stop, reads, writes, **kw):
        return self.op("pe", lambda t: t.matmul(out, lhsT, rhs, start=start, stop=stop, **kw), reads, writes)

    def transpose(self, out, in_, ident, reads, writes):
        return self.op("pe", lambda t: t.transpose(out, in_, ident), reads, writes)

    def act(self, out, in_, func, reads, writes, e="act", **kw):
        return self.op(e, lambda t: t.activation(out=out, in_=in_, func=func, **kw), reads, writes)

    def tt(self, e, out, in0, in1, op, reads, writes):
        return self.op(e, lambda t: t.tensor_tensor(out=out, in0=in0, in1=in1, op=op), reads, writes)

    def ts(self, e, out, in0, s1, s2, op0, op1, reads, writes, **kw):
        if op1 is None:
            return self.op(e, lambda t: t.tensor_scalar(out=out, in0=in0, scalar1=s1, scalar2=None, op0=op0, **kw), reads, writes)
        return self.op(e, lambda t: t.tensor_scalar(out=out, in0=in0, scalar1=s1, scalar2=s2, op0=op0, op1=op1, **kw), reads, writes)

    def copy(self, e, out, in_, reads, writes):
        if e == "act":
            return self.op(e, lambda t: t.copy(out=out, in_=in_), reads, writes)
        return self.op(e, lambda t: t.tensor_copy(out=out, in_=in_), reads, writes)

    def memset(self, e, out, val, writes):
        return self.op(e, lambda t: t.memset(out, val), (), writes)

    def finish(self):
        nc = self.nc
        waits = self._need("sp", self.out_tokens)

        def fin(eng, waits=waits):
            for s, v in waits:
                eng.wait_ge(s, v)
        self.ops["sp"].append(fin)
        ops = self.ops
        with nc.Block() as block:
            @block.tensor
            def _(t):
                for f in ops["pe"]:
                    f(t)

            @block.scalar
            def _(t):
                for f in ops["act"]:
                    f(t)

            @block.vector
            def _(t):
                for f in ops["dve"]:
                    f(t)

            @block.gpsimd
            def _(t):
                for f in ops["pool"]:
                    f(t)

            @block.sync
            def _(t):
                for f in ops["sp"]:
                    f(t)
        return nc

from concourse.bass_utils import run_bass_kernel_spmd
D = 2048
T = 8192
NCORE = 8
TL = T // NCORE
KC = D // 128
EPS = 1e-6
EVEN_COLS = 4632


def fm(v):
    v = np.asarray(v)
    return np.ascontiguousarray(v.reshape(-1, 128).T)


def run_spmd(nc, in_maps):
    res = run_bass_kernel_spmd(nc, in_maps, core_ids=list(range(NCORE)))
    return res.results


def build_L0():
    p = Prog()
    NCOL = 1536
    cT = p.dram_in("cT", [128, KC])
    w = p.dram_in("w", [2, D, NCOL])
    b = p.dram_in("b", [1, 2 * NCOL])
    out = p.dram_out("mod", [1, 2 * NCOL])
    ct = p.sb("ct", [128, KC], F32)
    cact = p.sb("cact", [128, KC], F32)
    bt = p.sb("bt", [1, 2 * NCOL], F32)
    ot = p.sb("ot", [1, 2 * NCOL], F32)
    wt = [p.sb("wt%d" % i, [128, KC, 512], F32) for i in range(2)]
    ps = [p.ps("ps%d" % i, [1, 512]) for i in range(2)]
    b_c, b_b, b_o = p.buf(), p.buf(), p.buf()
    b_w = [p.buf(), p.buf()]
    b_ps = [p.buf(), p.buf()]
    p.dma("sp", ct[:], cT, writes=[b_c])
    p.dma("sp", bt[:], b, writes=[b_b])
    p.act(cact[:], ct[:], AF.Silu, reads=[b_c], writes=[b_c])
    i = 0
    for l in range(2):
        for n in range(3):
            s = i % 2
            p.dma("sp" if s == 0 else "act", wt[s][:], w[l, :, n * 512:(n + 1) * 512].rearrange("(k p) n -> p k n", p=128),
                  writes=[b_w[s]])
            for k in range(KC):
                p.mm(ps[s][:], cact[:, k:k + 1], wt[s][:, k, :], k == 0, k == KC - 1, reads=[b_c, b_w[s]], writes=[b_ps[s]])
            c0 = l * NCOL + n * 512
            p.tt("dve", ot[:, c0:c0 + 512], ps[s][:], bt[:, c0:c0 + 512], ALU.add, reads=[b_ps[s], b_b], writes=[b_o])
            i += 1
    p.dma("sp", out, ot[:], reads=[b_o], is_output=True)
    return p.finish()


def stage_L0(inp):
    nc = build_L0()
    cT = fm(inp["c"][0])
    maps = []
    for c in range(NCORE):
        sl = slice(c * 1536, (c + 1) * 1536)
        maps.append({"cT": cT,
                     "w": np.ascontiguousarray(inp["ada_w"][:, :, sl]),
                     "b": np.ascontiguousarray(inp["ada_b"][:, sl]).reshape(1, -1)})
    res = run_spmd(nc, maps)
    mod = np.concatenate([r["mod"].reshape(2, 1536) for r in res], axis=1)
    return mod


def emit_rms_mod(p, xt, b_x, coef, shift, b_cs, hbf, b_h, ntok, scratch):
    onesf, epst, sq, tmp, rs, ps_ss, b_sq, b_tmp, b_rs, b_ss = scratch
    for tc in range(ntok // 512):
        tsl = slice(tc * 512, (tc + 1) * 512)
        p.act(sq[:], xt[:, :, tsl], AF.Square, reads=[b_x], writes=[b_sq])
        for k in range(KC):
            p.mm(ps_ss[:], onesf[:], sq[:, k, :], k == 0, k == KC - 1, reads=[b_sq], writes=[b_ss])
        p.act(rs[:], ps_ss[:], AF.Sqrt, reads=[b_ss], writes=[b_rs], scale=1.0 / D, bias=epst[:])
        p.op("dve", lambda t: t.reciprocal(out=rs[:], in_=rs[:]), reads=[b_rs], writes=[b_rs])
        for k in range(KC):
            j = k % 2
            p.op("dve", lambda t, k=k, j=j, tsl=tsl: t.scalar_tensor_tensor(out=tmp[j][:], in0=xt[:, k, tsl], scalar=coef[:, k:k + 1],
                                                                  in1=rs[:], op0=ALU.mult, op1=ALU.mult),
                 reads=[b_x, b_cs, b_rs], writes=[b_tmp[j]])
            p.act(hbf[:, k, tsl], tmp[j][:], AF.Identity, reads=[b_tmp[j], b_cs], writes=[b_h], bias=shift[:, k:k + 1])


def alloc_rms_scratch(p):
    onesf = p.sb("onesf", [128, 128], F32)
    epst = p.sb("epst", [128, 1], F32)
    sq = p.sb("sq", [128, KC, 512], F32)
    tmp = [p.sb("rtmp%d" % i, [128, 512], F32) for i in range(2)]
    rs = p.sb("rs", [128, 512], F32)
    ps_ss = p.ps("ps_ss", [128, 512])
    b_c = p.buf()
    p.memset("dve", onesf[:], 1.0, [b_c])
    p.memset("dve", epst[:], EPS, [b_c])
    return (onesf, epst, sq, tmp, rs, ps_ss, p.buf(), [p.buf(), p.buf()], p.buf(), p.buf())


def emit_proj(p, hbf, b_h, w_dram, ncols, ntok, out_dram, wq="pool", evac=None, wbufs=None):
    if wbufs is None:
        wt = [p.sb("pw%d" % i, [128, KC, 512], BF16) for i in range(2)]
        b_w = [p.buf(), p.buf()]
        pp = [p.ps("pp%d" % i, [128, 512]) for i in range(2)]
        b_pp = [p.buf(), p.buf()]
        osb = [p.sb("po%d" % i, [128, 512], F32) for i in range(3)]
        b_o = [p.buf() for _ in range(3)]
    else:
        wt, b_w, pp, b_pp, osb, b_o = wbufs
    ntile = (ncols + 511) // 512
    it = 0
    for j in range(ntile):
        c0 = j * 512
        cw = min(512, ncols - c0)
        s = j % 2
        p.dma(wq, wt[s][:, :, :cw], w_dram[:, c0:c0 + cw].rearrange("(k p) n -> p k n", p=128), writes=[b_w[s]])
        for m in range((cw + 127) // 128):
            mw = min(128, cw - m * 128)
            for tc in range(ntok // 512):
                tsl = slice(tc * 512, (tc + 1) * 512)
                q = it % 2
                o = it % 3
                for k in range(KC):
                    p.mm(pp[q][:mw, :], wt[s][:, k, m * 128:m * 128 + mw], hbf[:, k, tsl], k == 0, k == KC - 1,
                         reads=[b_w[s], b_h], writes=[b_pp[q]])
                if evac is None:
                    if it % 2 == 0:
                        p.act(osb[o][:mw, :], pp[q][:mw, :], AF.Copy, reads=[b_pp[q]], writes=[b_o[o]])
                    else:
                        p.copy("dve", osb[o][:mw, :], pp[q][:mw, :], reads=[b_pp[q]], writes=[b_o[o]])
                    p.dma("sp", out_dram[c0 + m * 128:c0 + m * 128 + mw, tsl], osb[o][:mw, :], reads=[b_o[o]], is_output=True)
                else:
                    evac(c0 + m * 128, mw, tc, pp[q], b_pp[q])
                it += 1


def build_L1():
    p = Prog()
    xT = p.dram_in("xT", [D, TL])
    vecs = p.dram_in("vecs", [128, 3, KC])
    w = p.dram_in("w", [D, EVEN_COLS])
    out = p.dram_out("projT", [EVEN_COLS, TL])
    xt = p.sb("xt", [128, KC, TL], F32)
    vt = p.sb("vt", [128, 3, KC], F32)
    coef = p.sb("coef", [128, KC], F32)
    hbf = p.sb("hbf", [128, KC, TL], BF16)
    b_x, b_v, b_h = p.buf(), p.buf(), p.buf()
    scratch = alloc_rms_scratch(p)
    xv = xT.rearrange("(k p) t -> p k t", p=128)
    for k4 in range(4):
        p.dma("sp" if k4 % 2 == 0 else "act", xt[:, k4 * 4:(k4 + 1) * 4, :], xv[:, k4 * 4:(k4 + 1) * 4, :], writes=[b_x])
    p.dma("sp", vt[:], vecs, writes=[b_v])
    p.ts("dve", coef[:], vt[:, 1, :], 1.0, None, ALU.add, None, reads=[b_v], writes=[b_v])
    p.tt("dve", coef[:], coef[:], vt[:, 0, :], ALU.mult, reads=[b_v], writes=[b_v])
    emit_rms_mod(p, xt, b_x, coef, vt[:, 2, :], b_v, hbf, b_h, TL, scratch)
    emit_proj(p, hbf, b_h, w, EVEN_COLS, TL, out)
    return p.finish()


def stage_L1(inp, mod):
    nc = build_L1()
    x = inp["x"][0]
    sh1, sc1 = mod[0, 0:D], mod[0, D:2 * D]
    vecs = np.ascontiguousarray(np.stack([fm(inp["norm_mix_g"][0]), fm(sc1), fm(sh1)], axis=1))
    maps = []
    for c in range(NCORE):
        maps.append({"xT": np.ascontiguousarray(x[c * TL:(c + 1) * TL].T), "vecs": vecs, "w": inp["ev_w_in"][0]})
    res = run_spmd(nc, maps)
    return np.concatenate([r["projT"] for r in res], axis=1)

HD = 128
QS = HD ** -0.5
NEGM = 30000.0 / QS
FX = 8192
FOFF = 4096
CMP_TILES = [0, 1, 2, 3, 4]
SLC_TILES = [-3, -2, -1, 0, 1]
WIN_TILES = [1, 2, 3, 4]
TINY = 1e-30


def t5_bucket_np(rel):
    n = np.maximum(rel, 0)
    nf = np.maximum(n, 1).astype(np.float32)
    large = 16 + (np.log(nf / 16) / math.log(128 / 16) * 16).astype(np.int32)
    large = np.minimum(large, 31)
    return np.where(n < 16, n, large)


def nsa_consts():
    rel = np.arange(FX) - FOFF
    bk = t5_bucket_np(rel)
    oh = np.zeros((34, FX), np.float32)
    valid = rel >= 0
    oh[bk[valid], np.nonzero(valid)[0]] += 1.0
    oh[31, valid] -= 1.0
    oh[32, ~valid] = 1.0
    oh[33, rel >= 512] = 1.0
    n = np.arange(512)
    j = np.arange(128)
    cover = ((16 * n[:, None] < 64 * j[None, :] + 64) & (16 * n[:, None] + 32 > 64 * j[None, :])).astype(np.float32)
    cover[511, :] = 0.0
    cover1 = np.concatenate([cover, np.ones((512, 1), np.float32)], axis=1)
    cover1 = np.ascontiguousarray(cover1.reshape(4, 128, 129).transpose(1, 0, 2))
    ebig = (np.arange(128)[:, None] == (np.arange(8192)[None, :] // 64)).astype(np.float32)
    ql = np.arange(128)[:, None]
    u = np.arange(256)[None, :] - 128
    fext = np.where(u < ql // 64, 0.0, np.where(u == ql // 64, 1e4, -1e4)).astype(np.float32)
    ident = np.eye(128, dtype=np.float32)
    return oh, cover1, ebig, fext, ident


def build_L2(nqc=T // 512, do_lru=True):
    p = Prog()
    q4T = p.dram_in("q4T", [128, 4, T])
    kcT = p.dram_in("kcT", [128, T]); vcT = p.dram_in("vcT", [128, T])
    ksT = p.dram_in("ksT", [128, T]); kwT = p.dram_in("kwT", [128, T])
    vs_d = p.dram_in("vs", [128, 64, 128]); vw_d = p.dram_in("vw", [128, 64, 128])
    glT = p.dram_in("glT", [3, T])
    tbl_d = p.dram_in("tblrep", [34, 4, 128])
    oh_d = p.dram_in("oh", [34, FX])
    cover_d = p.dram_in("cover1", [128, 4, 129])
    ebig_d = p.dram_in("ebig", [128, T])
    fext_d = p.dram_in("fext", [128, 256])
    ident_d = p.dram_in("ident", [128, 128])
    c31_d = p.dram_in("c31", [128, 4])
    w1_d = p.dram_in("w1", [2, 4096, 128]); peT_d = p.dram_in("peT", [2, 128, 32])
    b1_d = p.dram_in("b1", [128, 2]); w2_d = p.dram_in("w2", [2, 128, 128])
    b2k_d = p.dram_in("b2k", [128, 1]); b2v_d = p.dram_in("b2v", [128, 128])
    nsaT = p.dram_out("nsaT", [128, T])
    R_d = [p.dram_tmp("R%d" % i, [128, FX], BF16) for i in range(5)]

    identb = p.sb("identb", [128, 128], BF16)
    onesb = p.sb("onesb", [128, 128], BF16)
    cover = p.sb("cover", [128, 4, 129], BF16)
    ebig = p.sb("ebig", [128, T], BF16)
    fext = p.sb("fext", [128, 256], F32)
    c31 = p.sb("c31", [128, 4], F32)
    tbl = p.sb("tbl", [34, 4, 128], F32); oh = p.sb("oh", [34, 512], F32)
    b_const = p.buf()
    p.dma("pool", identb[:], ident_d, writes=[b_const])
    p.dma("pool", cover[:], cover_d, writes=[b_const])
    p.dma("pool", ebig[:], ebig_d, writes=[b_const])
    p.dma("sp", fext[:], fext_d, writes=[b_const])
    p.dma("sp", c31[:], c31_d, writes=[b_const])
    p.dma("sp", tbl[:], tbl_d, writes=[b_const])
    p.memset("dve", onesb[:], 1.0, [b_const])

    psA = [p.ps("psA%d" % i, [128, 512]) for i in range(2)]
    b_psA = [p.buf(), p.buf()]
    psO = [p.ps("psO%d" % i, [128, 512]) for i in range(2)]
    psZ = [p.ps("psZ%d" % i, [128, 512]) for i in range(2)]
    b_psO = [p.buf(), p.buf()]; b_psZ = [p.buf(), p.buf()]
    psI = p.ps("psI", [128, 512]); b_psI = p.buf()
    psT = p.ps("psT", [128, 128], BF16); b_psT = p.buf()

    rst = [p.sb("rst%d" % i, [128, 512], BF16) for i in range(2)]
    b_rst = [p.buf(), p.buf()]
    b_oh, b_R = p.buf(), p.buf()
    it = 0
    for xc in range(FX // 512):
        p.dma("sp", oh[:], oh_d[:, xc * 512:(xc + 1) * 512], writes=[b_oh])
        for r in range(5):
            s = it % 2
            hh = r if r < 4 else 0
            nrow = 33 if r < 4 else 34
            p.mm(psA[s][:], tbl[:nrow, hh, :], oh[:nrow, :], True, True, reads=[b_const, b_oh], writes=[b_psA[s]])
            p.act(rst[s][:], psA[s][:], AF.Copy, reads=[b_psA[s]], writes=[b_rst[s]], scale=1.0 / QS)
            p.dma("sp", R_d[r][:, xc * 512:(xc + 1) * 512], rst[s][:], reads=[b_rst[s]], writes=[b_R])
            it += 1

    def toep(Rd, pstride, base):
        return bass.AP(Rd.tensor, base, [[FX - pstride, 128], [1, 512]])

    bt_cmp = p.sb("bt_cmp", [128, 4, 5, 512], BF16)
    bt_slc = p.sb("bt_slc", [128, 5, 512], BF16)
    bt_win = p.sb("bt_win", [128, 4, 512], BF16)
    b_bt = p.buf()
    for hh in range(4):
        for a, d in enumerate(CMP_TILES):
            p.dma("sp", bt_cmp[:, hh, a, :], toep(R_d[hh], 16, FOFF + 512 * d - 31), reads=[b_R], writes=[b_bt])
    for a, d in enumerate(SLC_TILES):
        p.dma("sp", bt_slc[:, a, :], toep(R_d[0], 1, FOFF + 128 * d), reads=[b_R], writes=[b_bt])
    for a, d in enumerate(WIN_TILES):
        p.dma("sp", bt_win[:, a, :], toep(R_d[4], 1, FOFF + 128 * d), reads=[b_R], writes=[b_bt])

    kA = p.sb("kA", [128, T], BF16)
    vA = p.sb("vA", [128, T], BF16)
    ks = p.sb("ks", [128, T], BF16)
    vs = p.sb("vs", [128, 64, 128], BF16)
    b_kA, b_vA, b_ks, b_vs = p.buf(), p.buf(), p.buf(), p.buf()
    p.dma("pool", kA[:], kcT, writes=[b_kA])
    p.dma("pool", vA[:], vcT, writes=[b_vA])
    p.dma("pool", ks[:], ksT, writes=[b_ks])
    p.dma("pool", vs[:], vs_d, writes=[b_vs])

    ecmp = p.sb("ecmp", [128, 4, 4, 512], BF16)
    ecf = ecmp[:].rearrange("p a b c -> p (a b c)")
    w1 = [ecf[:, kv * 4096:(kv + 1) * 4096].rearrange("p (j o) -> p j o", o=128) for kv in range(2)]
    peT = p.sb("peT", [128, 2, 32], BF16)
    b1 = p.sb("b1", [128, 2], F32); w2 = p.sb("w2", [128, 2, 128], BF16)
    b2k = p.sb("b2k", [128, 1], F32); b2v = p.sb("b2v", [128, 128], F32)
    b_cw = p.buf()
    for kv in range(2):
        p.dma("pool", w1[kv], w1_d[kv].rearrange("(j d) o -> d j o", d=128), writes=[b_cw])
        p.dma("pool", peT[:, kv, :], peT_d[kv], writes=[b_cw])
        p.dma("pool", w2[:, kv, :], w2_d[kv], writes=[b_cw])
    p.dma("sp", b1[:], b1_d, writes=[b_cw]); p.dma("sp", b2k[:], b2k_d, writes=[b_cw]); p.dma("sp", b2v[:], b2v_d, writes=[b_cw])
    kcmpT = p.sb("kcmpT", [128, 512], BF16)
    vcmp = p.sb("vcmp", [128, 4, 128], BF16)
    hidT = p.sb("hidT", [128, 512], BF16)
    g1t = p.sb("g1t", [128, 512], F32); g2t = p.sb("g2t", [128, 512], F32); g3t = p.sb("g3t", [128, 512], F32)
    cb = p.sb("cb", [128, 1], F32)
    b_kc, b_vc, b_hid, b_g, b_cb = p.buf(), p.buf(), p.buf(), p.buf(), p.buf()
    p.memset("dve", hidT[:], 0.0, [b_hid])
    p.memset("dve", kcmpT[:], 0.0, [b_kc])

    def gelu_tanh(src, b_src, dst, b_dst, bias_ap, reads_extra, width):
        p.act(g1t[:, :width], src, AF.Identity, reads=[b_src] + reads_extra, writes=[b_g], bias=bias_ap)
        p.tt("dve", g2t[:, :width], g1t[:, :width], g1t[:, :width], ALU.mult, reads=[b_g], writes=[b_g])
        p.ts("dve", g2t[:, :width], g2t[:, :width], 0.044715, 1.0, ALU.mult, ALU.add, reads=[b_g], writes=[b_g])
        p.tt("dve", g2t[:, :width], g2t[:, :width], g1t[:, :width], ALU.mult, reads=[b_g], writes=[b_g])
        p.act(g3t[:, :width], g2t[:, :width], AF.Sigmoid, reads=[b_g], writes=[b_g], scale=1.5957691216)
        p.tt("dve", dst, g3t[:, :width], g1t[:, :width], ALU.mult, reads=[b_g], writes=[b_dst])

    for kv, (src, b_src) in enumerate(((kA, b_kA), (vA, b_vA))):
        sv = src[:].rearrange("p (n s) -> p n s", s=16)
        for j in range(32):
            p.mm(psA[1][:, 0:1], w1[kv][:, j, :], peT[:, kv, j:j + 1], j == 0, j == 31, reads=[b_cw], writes=[b_psA[1]])
        p.tt("dve", cb[:], psA[1][:, 0:1], b1[:, kv:kv + 1], ALU.add, reads=[b_psA[1], b_cw], writes=[b_cb])
        for j in range(32):
            jq, jr = j // 16, j % 16
            p.mm(psA[0][:, :511], w1[kv][:, j, :], sv[:, jq:jq + 511, jr], j == 0, j == 31, reads=[b_cw, b_src], writes=[b_psA[0]])
        gelu_tanh(psA[0][:, :511], b_psA[0], hidT[:, :511], b_hid, cb[:], [b_cb], 511)
        if kv == 0:
            p.mm(psA[1][:, :512], w2[:, 0, :], hidT[:], True, True, reads=[b_cw, b_hid], writes=[b_psA[1]])
            p.act(kcmpT[:, :511], psA[1][:, :511], AF.Identity, reads=[b_psA[1], b_cw], writes=[b_kc], bias=b2k[:])
        else:
            for c in range(4):
                p.mm(psA[1][:, :128], hidT[:, c * 128:(c + 1) * 128], w2[:, 1, :], True, True, reads=[b_cw, b_hid], writes=[b_psA[1]])
                p.tt("dve", vcmp[:, c, :], psA[1][:, :128], b2v[:], ALU.add, reads=[b_psA[1], b_cw], writes=[b_vc])
    vAv = vA[:].rearrange("p (j d) -> p j d", d=128)
    p.dma("pool", kA[:], kwT, writes=[b_kA])
    p.dma("pool", vAv, vw_d, writes=[b_vA])

    q4c = [p.sb("q4c%d" % i, [128, 4, 512], BF16) for i in range(2)]
    b_q = [p.buf(), p.buf()]
    gt0 = p.sb("gt0", [128, 3, 512], F32)
    gt = [gt0, gt0]
    b_gt0 = p.buf()
    b_gt = [b_gt0, b_gt0]
    b_ecmp = [p.buf() for _ in range(4)]
    impacc = p.sb("impacc", [128, 4, 128], F32); b_imp = p.buf()
    rz1 = p.sb("rz1", [128, 1], F32); b_rz1 = p.buf()
    vals = p.sb("vals", [128, 128], F32); work = p.sb("work", [128, 128], F32)
    m8a = p.sb("m8a", [128, 8], F32); m8b = p.sb("m8b", [128, 8], F32)
    selq = p.sb("selq", [128, 128], BF16)
    b_sel = p.buf()
    selT = p.sb("selT", [128, 512], BF16); b_selT = p.buf()
    es = [p.sb("es%d" % i, [128, 512], BF16) for i in range(3)]
    b_es = [p.buf() for _ in range(3)]
    rz = p.sb("rz", [128, 512], F32); wgt = p.sb("wgt", [128, 512], F32); b_rz = p.buf()
    onsa = [p.sb("onsa%d" % i, [128, 512], F32) for i in range(2)]
    b_on = [p.buf(), p.buf()]
    esi = 0
    ozi = 0

    def branch_finish(i, br, oz, first):
        s = i % 2
        p.ts("dve", rz[:], psZ[oz][:], TINY, None, ALU.max, None, reads=[b_psZ[oz]], writes=[b_rz])
        p.op("dve", lambda t: t.reciprocal(out=rz[:], in_=rz[:]), reads=[b_rz], writes=[b_rz])
        p.tt("dve", wgt[:], rz[:], gt[s][:, br, :], ALU.mult, reads=[b_rz, b_gt[s]], writes=[b_rz])
        if first:
            p.tt("dve", onsa[s][:], psO[oz][:], wgt[:], ALU.mult, reads=[b_psO[oz], b_rz], writes=[b_on[s]])
        else:
            p.tt("dve", wgt[:], psO[oz][:], wgt[:], ALU.mult, reads=[b_psO[oz], b_rz], writes=[b_rz])
            p.tt("dve", onsa[s][:], onsa[s][:], wgt[:], ALU.add, reads=[b_rz], writes=[b_on[s]])

    for i in range(nqc):
        s = i % 2
        tsl = slice(i * 512, (i + 1) * 512)
        p.dma("pool", q4c[s][:], q4T[:, :, tsl], writes=[b_q[s]])
        p.dma("sp", gt[s][:], glT[:, tsl].partition_broadcast(128), writes=[b_gt[s]])
        p.act(gt[s][:], gt[s][:], AF.Sigmoid, reads=[b_gt[s]], writes=[b_gt[s]])
        njs = [nj for nj in range(4) if i - 4 * nj >= 0]
        first_imp = True
        for hh in range(4):
            for nj in njs:
                d = i - 4 * nj
                a = esi % 2; esi += 1
                p.mm(psA[a][:], kcmpT[:, nj * 128:(nj + 1) * 128], q4c[s][:, hh, :], True, d > 4, reads=[b_kc, b_q[s]], writes=[b_psA[a]])
                if d <= 4:
                    p.mm(psA[a][:], identb[:], bt_cmp[:, hh, d, :], False, True, reads=[b_const, b_bt], writes=[b_psA[a]])
                p.act(ecmp[:, hh, nj, :], psA[a][:], AF.Exp, reads=[b_psA[a], b_const], writes=[b_ecmp[hh], b_cw], scale=QS, bias=c31[:, hh:hh + 1])
            for qs in range(4):
                for x, nj in enumerate(njs):
                    p.mm(psI[:, :129], ecmp[:, hh, nj, qs * 128:(qs + 1) * 128], cover[:, nj, :], x == 0, x == len(njs) - 1,
                         reads=[b_ecmp[hh], b_const], writes=[b_psI])
                p.ts("dve", rz1[:], psI[:, 128:129], TINY, None, ALU.max, None, reads=[b_psI], writes=[b_rz1])
                p.op("dve", lambda t: t.reciprocal(out=rz1[:], in_=rz1[:]), reads=[b_rz1], writes=[b_rz1])
                if hh == 0:
                    p.ts("dve", impacc[:, qs, :], psI[:, :128], rz1[:], None, ALU.mult, None, reads=[b_psI, b_rz1], writes=[b_imp])
                else:
                    p.op("dve", lambda t, qs=qs: t.scalar_tensor_tensor(out=impacc[:, qs, :], in0=psI[:, :128], scalar=rz1[:],
                                                                      in1=impacc[:, qs, :], op0=ALU.mult, op1=ALU.add),
                         reads=[b_psI, b_rz1, b_imp], writes=[b_imp])
            if hh == 0:
                oz = ozi % 2; ozi += 1
                for x, nj in enumerate(njs):
                    p.mm(psO[oz][:], vcmp[:, nj, :], ecmp[:, 0, nj, :], x == 0, x == len(njs) - 1, reads=[b_vc, b_ecmp[0]], writes=[b_psO[oz]])
                for x, nj in enumerate(njs):
                    p.mm(psZ[oz][:], onesb[:], ecmp[:, 0, nj, :], x == 0, x == len(njs) - 1, reads=[b_const, b_ecmp[0]], writes=[b_psZ[oz]])
                branch_finish(i, 0, oz, True)
        for qs in range(4):
            qb = 4 * i + qs
            p.tt("dve", vals[:], impacc[:, qs, :], fext[:, 128 - 2 * qb:256 - 2 * qb], ALU.add, reads=[b_imp, b_const], writes=[b_sel])
            p.op("dve", lambda t: t.max(out=m8a[:], in_=vals[:]), reads=[b_sel], writes=[b_sel])
            p.op("dve", lambda t: t.match_replace(out=work[:], in_to_replace=m8a[:], in_values=vals[:], imm_value=-1e30), reads=[b_sel], writes=[b_sel])
            p.op("dve", lambda t: t.max(out=m8b[:], in_=work[:]), reads=[b_sel], writes=[b_sel])
            p.ts("dve", work[:], vals[:], m8b[:, 7:8], None, ALU.is_ge, None, reads=[b_sel], writes=[b_sel])
            p.ts("dve", selq[:], work[:], NEGM, -NEGM, ALU.mult, ALU.add, reads=[b_sel], writes=[b_sel])
            p.transpose(psT[:], selq[:], identb[:], reads=[b_sel, b_const], writes=[b_psT])
            p.copy("dve", selT[:, qs * 128:(qs + 1) * 128], psT[:], reads=[b_psT], writes=[b_selT])
        oz = ozi % 2; ozi += 1
        nkb = 4 * i + 4
        for j in range(nkb):
            d = 4 * i - j
            a = esi % 2; esi += 1
            e = esi % 3
            p.mm(psA[a][:], ks[:, j * 128:(j + 1) * 128], q4c[s][:, 0, :], True, False, reads=[b_ks, b_q[s]], writes=[b_psA[a]])
            p.mm(psA[a][:], ebig[:, j * 128:(j + 1) * 128], selT[:], False, d > 1, reads=[b_const, b_selT], writes=[b_psA[a]])
            if d <= 1:
                p.mm(psA[a][:], identb[:], bt_slc[:, d + 3, :], False, True, reads=[b_const, b_bt], writes=[b_psA[a]])
            p.act(es[e][:], psA[a][:], AF.Exp, reads=[b_psA[a], b_const], writes=[b_es[e]], scale=QS, bias=c31[:, 0:1])
            p.mm(psO[oz][:], vs[:, j, :], es[e][:], j == 0, j == nkb - 1, reads=[b_vs, b_es[e]], writes=[b_psO[oz]])
            p.mm(psZ[oz][:], onesb[:], es[e][:], j == 0, j == nkb - 1, reads=[b_const, b_es[e]], writes=[b_psZ[oz]])
        branch_finish(i, 1, oz, False)
        oz = ozi % 2; ozi += 1
        js = list(range(max(0, 4 * i - 4), 4 * i + 4))
        for x, j in enumerate(js):
            d = 4 * i - j
            a = esi % 2; esi += 1
            e = esi % 3
            p.mm(psA[a][:], kA[:, j * 128:(j + 1) * 128], q4c[s][:, 0, :], True, False, reads=[b_kA, b_q[s]], writes=[b_psA[a]])
            btile = bt_slc[:, d + 3, :] if d <= 0 else bt_win[:, d - 1, :]
            p.mm(psA[a][:], identb[:], btile, False, True, reads=[b_const, b_bt], writes=[b_psA[a]])
            p.act(es[e][:], psA[a][:], AF.Exp, reads=[b_psA[a], b_const], writes=[b_es[e]], scale=QS, bias=c31[:, 0:1])
            p.mm(psO[oz][:], vAv[:, j, :], es[e][:], x == 0, x == len(js) - 1, reads=[b_vA, b_es[e]], writes=[b_psO[oz]])
            p.mm(psZ[oz][:], onesb[:], es[e][:], x == 0, x == len(js) - 1, reads=[b_const, b_es[e]], writes=[b_psZ[oz]])
        branch_finish(i, 2, oz, False)
        p.dma("sp", nsaT[:, tsl], onsa[s][:], reads=[b_on[s]], is_output=True)

    return p.finish()


def build_L2b():
    p = Prog()
    CH = 1024
    xbr_d = p.dram_in("xbr", [128, T]); ybr_d = p.dram_in("ybr", [128, T])
    lvec_d = p.dram_in("lvec", [128, 8])
    wa_d = p.dram_in("wa", [128, 128]); wi_d = p.dram_in("wi", [128, 128])
    lruT = p.dram_out("lruT", [128, T])
    lv = p.sb("lv", [128, 8], F32); wa = p.sb("wa", [128, 128], F32); wi = p.sb("wi", [128, 128], F32)
    nsp = p.sb("nsp", [128, 2], F32)
    b_lc = p.buf()
    p.dma("sp", lv[:], lvec_d, writes=[b_lc]); p.dma("sp", wa[:], wa_d, writes=[b_lc]); p.dma("sp", wi[:], wi_d, writes=[b_lc])
    p.act(nsp[:, 0:1], lv[:, 7:8], AF.Exp, reads=[b_lc], writes=[b_lc], scale=-1.0)
    p.ts("dve", nsp[:, 0:1], nsp[:, 0:1], 1.0, None, ALU.add, None, reads=[b_lc], writes=[b_lc])
    p.act(nsp[:, 0:1], nsp[:, 0:1], AF.Ln, reads=[b_lc], writes=[b_lc])
    p.ts("dve", nsp[:, 1:2], nsp[:, 0:1], -16.0, None, ALU.mult, None, reads=[b_lc], writes=[b_lc])
    p.ts("dve", nsp[:, 0:1], nsp[:, 0:1], -8.0, None, ALU.mult, None, reads=[b_lc], writes=[b_lc])
    xb = [p.sb("lxb%d" % i, [128, CH + 3], F32) for i in range(2)]
    b_xb = [p.buf(), p.buf()]
    yb = p.sb("lyb", [128, CH], F32); b_yb = p.buf()
    xc = p.sb("lxc", [128, CH], F32); b_xc = p.buf()
    ra = p.sb("lra", [128, CH], F32); ri = p.sb("lri", [128, CH], F32); b_ra, b_ri = p.buf(), p.buf()
    av = p.sb("lav", [128, CH], F32); bv = p.sb("lbv", [128, CH], F32); b_av, b_bv = p.buf(), p.buf()
    hv = [p.sb("lhv%d" % i, [128, CH], F32) for i in range(2)]; b_hv = [p.buf(), p.buf()]
    t1 = p.sb("lt1", [128, CH], F32); t2 = p.sb("lt2", [128, CH], F32); b_t1, b_t2 = p.buf(), p.buf()
    ov = p.sb("lov", [128, CH], F32); b_ov = p.buf()
    return_ps = [p.ps("psL%d" % i, [128, 512]) for i in range(2)]
    b_lps = [p.buf(), p.buf()]
    p.memset("dve", xb[0][:, 0:3], 0.0, [b_xb[0]])
    for c in range(T // CH):
        s = c % 2
        tsl = slice(c * CH, (c + 1) * CH)
        p.dma("sp", xb[s][:, 3:], xbr_d[:, tsl], writes=[b_xb[s]])
        p.dma("sp", yb[:], ybr_d[:, tsl], writes=[b_yb])
        if c > 0:
            p.copy("dve", xb[s][:, 0:3], xb[1 - s][:, CH:CH + 3], reads=[b_xb[1 - s]], writes=[b_xb[s]])
        p.ts("dve", xc[:], xb[s][:, 0:CH], lv[:, 0:1], lv[:, 4:5], ALU.mult, ALU.add, reads=[b_xb[s], b_lc], writes=[b_xc])
        for k in range(1, 4):
            p.op("dve", lambda t, k=k, s=s: t.scalar_tensor_tensor(out=xc[:], in0=xb[s][:, k:k + CH], scalar=lv[:, k:k + 1], in1=xc[:],
                                                                  op0=ALU.mult, op1=ALU.add), reads=[b_xb[s], b_lc, b_xc], writes=[b_xc])
        for hc in range(CH // 512):
            hs = slice(hc * 512, (hc + 1) * 512)
            p.mm(return_ps[0][:], wa[:], xc[:, hs], True, True, reads=[b_lc, b_xc], writes=[b_lps[0]])
            p.act(ra[:, hs], return_ps[0][:], AF.Sigmoid, reads=[b_lps[0], b_lc], writes=[b_ra], bias=lv[:, 5:6])
            p.mm(return_ps[1][:], wi[:], xc[:, hs], True, True, reads=[b_lc, b_xc], writes=[b_lps[1]])
            p.act(ri[:, hs], return_ps[1][:], AF.Sigmoid, reads=[b_lps[1], b_lc], writes=[b_ri], bias=lv[:, 6:7])
        p.act(av[:], ra[:], AF.Exp, reads=[b_ra, b_lc], writes=[b_av], scale=nsp[:, 0:1])
        p.act(t1[:], ra[:], AF.Exp, reads=[b_ra, b_lc], writes=[b_t1], scale=nsp[:, 1:2])
        p.ts("dve", t1[:], t1[:], -1.0, 1.0, ALU.mult, ALU.add, reads=[b_t1], writes=[b_t1])
        p.ts("dve", t1[:], t1[:], 0.0, None, ALU.max, None, reads=[b_t1], writes=[b_t1])
        p.act(t1[:], t1[:], AF.Sqrt, reads=[b_t1], writes=[b_t1])
        p.tt("dve", bv[:], ri[:], xc[:], ALU.mult, reads=[b_ri, b_xc], writes=[b_bv])
        p.tt("dve", bv[:], bv[:], t1[:], ALU.mult, reads=[b_bv, b_t1], writes=[b_bv])
        init = 0.0 if c == 0 else hv[1 - s][:, CH - 1:CH]
        p.op("dve", lambda t, s=s, init=init: t.tensor_tensor_scan(out=hv[s][:], data0=av[:], data1=bv[:], initial=init,
                                                                   op0=ALU.mult, op1=ALU.add),
             reads=[b_av, b_bv] + ([b_hv[1 - s]] if c > 0 else []), writes=[b_hv[s]])
        p.tt("dve", t2[:], yb[:], yb[:], ALU.mult, reads=[b_yb], writes=[b_t2])
        p.ts("dve", t2[:], t2[:], 0.044715, 1.0, ALU.mult, ALU.add, reads=[b_t2], writes=[b_t2])
        p.tt("dve", t2[:], t2[:], yb[:], ALU.mult, reads=[b_t2, b_yb], writes=[b_t2])
        p.act(t2[:], t2[:], AF.Sigmoid, reads=[b_t2], writes=[b_t2], scale=1.5957691216)
        p.tt("dve", t2[:], t2[:], yb[:], ALU.mult, reads=[b_t2, b_yb], writes=[b_t2])
        p.tt("dve", ov[:], t2[:], hv[s][:], ALU.mult, reads=[b_t2, b_hv[s]], writes=[b_ov])
        p.dma("sp", lruT[:, tsl], ov[:], reads=[b_ov], is_output=True)
    return p.finish()


def stage_L2(inp, projT, nqc=T // 512):
    oh, cover1, ebig, fext, ident = nsa_consts()
    nc = build_L2(nqc)
    rb = inp["rel_bias"]
    maps = []
    for h in range(NCORE):
        g = h // 4
        heads = [h] + [hh for hh in range(4 * g, 4 * g + 4) if hh != h]
        q4 = np.ascontiguousarray(np.stack([projT[hh * 128:(hh + 1) * 128] for hh in heads], axis=1))

        def kv(idx):
            base = 1024 + idx * 256 + g * 128
            return projT[base:base + 128]

        def tokmaj(a):
            return np.ascontiguousarray(a.T.reshape(64, 128, 128).transpose(1, 0, 2))
        tblrep = np.zeros((34, 4, 128), np.float32)
        for a, hh in enumerate(heads):
            tblrep[:32, a, :] = rb[:, hh][:, None]
        tblrep[32:] = -30000.0
        c31 = np.ascontiguousarray(np.stack([np.full(128, rb[31, hh], np.float32) for hh in heads], axis=1))
        maps.append({
            "q4T": q4, "kcT": np.ascontiguousarray(kv(0)), "vcT": np.ascontiguousarray(kv(1)),
            "ksT": np.ascontiguousarray(kv(2)), "vs": tokmaj(kv(3)), "kwT": np.ascontiguousarray(kv(4)), "vw": tokmaj(kv(5)),
            "glT": np.ascontiguousarray(projT[2560 + 3 * h:2560 + 3 * h + 3]),
            "tblrep": tblrep, "oh": oh, "cover1": cover1, "ebig": ebig, "fext": fext, "ident": ident, "c31": c31,
            "w1": inp["cmp_w1"][0], "peT": np.ascontiguousarray(inp["cmp_pe"][0].transpose(0, 2, 1)),
            "b1": np.ascontiguousarray(inp["cmp_b1"][0].T), "w2": inp["cmp_w2"][0],
            "b2k": np.ascontiguousarray(inp["cmp_b2"][0][0].reshape(128, 1)),
            "b2v": np.ascontiguousarray(np.tile(inp["cmp_b2"][0][1][None, :], (128, 1))),
        })
    res = run_spmd(nc, maps)
    return np.concatenate([r["nsaT"] for r in res], axis=0)


def stage_L2b(inp, projT):
    nc = build_L2b()
    maps = []
    for c in range(NCORE):
        sl = slice(c * 128, (c + 1) * 128)
        lvec = np.stack([inp["lru_conv_w"][0][k, sl] for k in range(4)] +
                        [inp["lru_conv_b"][0][sl], inp["lru_ba"][0][sl], inp["lru_bi"][0][sl], inp["lru_lambda"][0][sl]], axis=1)
        maps.append({"xbr": np.ascontiguousarray(projT[3608 + c * 128:3608 + (c + 1) * 128]),
                     "ybr": np.ascontiguousarray(projT[2584 + c * 128:2584 + (c + 1) * 128]),
                     "lvec": np.ascontiguousarray(lvec.astype(np.float32)),
                     "wa": inp["lru_wa"][0][c], "wi": inp["lru_wi"][0][c]})
    res = run_spmd(nc, maps)
    return np.concatenate([r["lruT"] for r in res], axis=0)

NE = 64
DE = 512
ODD_COLS = 8192


def emit_rms_mod2(p, xt, b_x, coef, shift, b_cs, hbf32, b_hf, ntok, scratch, after_chunk):
    onesf, epst, sq, tmp, rs, ps_ss, b_sq, b_tmp, b_rs, b_ss = scratch
    for tc in range(ntok // 512):
        tsl = slice(tc * 512, (tc + 1) * 512)
        p.act(sq[:], xt[:, :, tsl], AF.Square, reads=[b_x], writes=[b_sq])
        for k in range(KC):
            p.mm(ps_ss[:], onesf[:], sq[:, k, :], k == 0, k == KC - 1, reads=[b_sq], writes=[b_ss])
        p.act(rs[:], ps_ss[:], AF.Sqrt, reads=[b_ss], writes=[b_rs], scale=1.0 / D, bias=epst[:])
        p.op("dve", lambda t: t.reciprocal(out=rs[:], in_=rs[:]), reads=[b_rs], writes=[b_rs])
        for k in range(KC):
            j = k % 2
            p.op("dve", lambda t, k=k, j=j, tsl=tsl: t.scalar_tensor_tensor(out=tmp[j][:], in0=xt[:, k, tsl], scalar=coef[:, k:k + 1],
                                                                           in1=rs[:], op0=ALU.mult, op1=ALU.mult),
                 reads=[b_x, b_cs, b_rs], writes=[b_tmp[j]])
            p.act(hbf32[:, k, :], tmp[j][:], AF.Identity, reads=[b_tmp[j], b_cs], writes=[b_hf], bias=shift[:, k:k + 1])
        after_chunk(tc)


def emit_rms_plain(p, xt, b_x, gain, b_g, outt, b_out, ntok, scratch):
    onesf, epst, sq, tmp, rs, ps_ss, b_sq, b_tmp, b_rs, b_ss = scratch
    for tc in range(ntok // 512):
        tsl = slice(tc * 512, (tc + 1) * 512)
        p.act(sq[:], xt[:, :, tsl], AF.Square, reads=[b_x], writes=[b_sq])
        for k in range(KC):
            p.mm(ps_ss[:], onesf[:], sq[:, k, :], k == 0, k == KC - 1, reads=[b_sq], writes=[b_ss])
        p.act(rs[:], ps_ss[:], AF.Sqrt, reads=[b_ss], writes=[b_rs], scale=1.0 / D, bias=epst[:])
        p.op("dve", lambda t: t.reciprocal(out=rs[:], in_=rs[:]), reads=[b_rs], writes=[b_rs])
        for k in range(KC):
            p.op("dve", lambda t, k=k, tsl=tsl: t.scalar_tensor_tensor(out=outt[:, k, tsl], in0=xt[:, k, tsl], scalar=gain[:, k:k + 1],
                                                                      in1=rs[:], op0=ALU.mult, op1=ALU.mult),
                 reads=[b_x, b_g, b_rs], writes=[b_out])


def build_postA(layer):
    last = (layer == 1)
    p = Prog()
    xT = p.dram_in("xT", [D, TL]); mixT = p.dram_in("mixT", [D, TL])
    gateT = p.dram_in("gateT", [D, TL]) if last else None
    w_out = p.dram_in("w_out", [D, D])
    vecs = p.dram_in("vecs", [128, 4, KC])
    wr_d = p.dram_in("wr", [D, 72]); br_d = p.dram_in("br", [128, 72]); ident_d = p.dram_in("ident", [128, 128])
    x1T = p.dram_out("x1T", [D, TL]); hfT = p.dram_out("hfT", [D, TL]); gwTo = p.dram_out("gwT", [64, TL])
    xt = p.sb("xt", [128, KC, TL], F32); b_x = p.buf()
    hbf = p.sb("hbf", [128, KC, TL], BF16); b_h = p.buf()
    vt = p.sb("vt", [128, 4, KC], F32); b_v = p.buf()
    coef = p.sb("coef", [128, KC], F32)
    ident = p.sb("ident", [128, 128], F32); b_c = p.buf()
    scratch = alloc_rms_scratch(p)
    xv = xT.rearrange("(k p) t -> p k t", p=128); mv = mixT.rearrange("(k p) t -> p k t", p=128)
    for k4 in range(4):
        p.dma("sp", xt[:, k4 * 4:(k4 + 1) * 4, :], xv[:, k4 * 4:(k4 + 1) * 4, :], writes=[b_x])
    p.dma("sp", vt[:], vecs, writes=[b_v]); p.dma("sp", ident[:], ident_d, writes=[b_c])
    p.ts("dve", coef[:], vt[:, 2, :], 1.0, None, ALU.add, None, reads=[b_v], writes=[b_v])
    p.tt("dve", coef[:], coef[:], vt[:, 1, :], ALU.mult, reads=[b_v], writes=[b_v])
    hf32 = scratch[2]; b_hf = scratch[6]
    if last:
        gv = gateT.rearrange("(k p) t -> p k t", p=128)
        b_m = p.buf()
        for k4 in range(8):
            ks = slice(k4 * 2, (k4 + 1) * 2)
            mt = hf32[:, 0:2, :].rearrange("p a (b t) -> p (a b) t", b=1) if False else None
            m2 = hf32[:, 0:4, :].rearrange("p (a b) t -> p a (b t)", b=2)
            g2 = hf32[:, 4:8, :].rearrange("p (a b) t -> p a (b t)", b=2)
            p.dma("sp", m2, mv[:, ks, :], writes=[b_hf])
            p.dma("act", g2, gv[:, ks, :], writes=[b_hf])
            p.act(g2, g2, AF.Silu, reads=[b_hf], writes=[b_hf])
            p.tt("dve", hbf[:, ks, :], m2, g2, ALU.mult, reads=[b_hf], writes=[b_h])
    else:
        for k4 in range(4):
            p.dma("pool", hbf[:, k4 * 4:(k4 + 1) * 4, :], mv[:, k4 * 4:(k4 + 1) * 4, :], writes=[b_h])

    def evac_res(c0, mw, tc, ps_t, b_ps):
        k = c0 // 128
        tsl = slice(tc * 512, (tc + 1) * 512)
        p.op("dve", lambda t: t.scalar_tensor_tensor(out=xt[:, k, tsl], in0=ps_t[:], scalar=vt[:, 0, k:k + 1], in1=xt[:, k, tsl],
                                                     op0=ALU.mult, op1=ALU.add), reads=[b_ps, b_v, b_x], writes=[b_x])
    emit_proj(p, hbf, b_h, w_out, D, TL, None, evac=evac_res)
    ov = x1T.rearrange("(k p) t -> p k t", p=128)
    for k4 in range(4):
        p.dma("sp", ov[:, k4 * 4:(k4 + 1) * 4, :], xt[:, k4 * 4:(k4 + 1) * 4, :], reads=[b_x], is_output=True)
    wr = p.sb("wr", [128, KC, 72], F32); br = p.sb("br", [128, 72], F32)
    p.dma("sp", wr[:], wr_d.rearrange("(k p) n -> p k n", p=128), writes=[b_c]); p.dma("sp", br[:], br_d, writes=[b_c])
    gwT = p.sb("gwT", [64, TL], F32); b_gw = p.buf()
    psR = p.ps("psR", [128, 512]); b_psR = p.buf()
    lg = p.sb("lg", [128, 72], F32); gm = p.sb("gm", [128, 8], F32); goh = p.sb("goh", [128, 8], F32)
    ex = p.sb("ex", [128, 8], F32); pg = p.sb("pg", [128, 2], F32); es_ = p.sb("esel", [128, 64], F32)
    t8 = p.sb("t8", [128, 8], F32); w12 = p.sb("w12", [128, 4], F32); gw = p.sb("gw", [128, 64], F32); gw2 = p.sb("gw2", [128, 64], F32)
    b_rt = p.buf()
    hv = hfT.rearrange("(k p) t -> p k t", p=128)

    def router(tc):
        p.dma("sp", hv[:, :, tc * 512:(tc + 1) * 512], hf32[:], reads=[b_hf], is_output=True)
        for sub in range(4):
            tsl = slice(sub * 128, (sub + 1) * 128)
            for k in range(KC):
                p.mm(psR[:, :72], hf32[:, k, tsl], wr[:, k, :], k == 0, k == KC - 1, reads=[b_hf, b_c], writes=[b_psR])
            p.tt("dve", lg[:], psR[:, :72], br[:], ALU.add, reads=[b_psR, b_c], writes=[b_rt])
            p.op("dve", lambda t: t.tensor_reduce(out=gm[:, 0:1], in_=lg[:, 0:8], axis=AX.X, op=ALU.max), reads=[b_rt], writes=[b_rt])
            p.ts("dve", goh[:], lg[:, 0:8], gm[:, 0:1], None, ALU.is_ge, None, reads=[b_rt], writes=[b_rt])
            p.ts("dve", ex[:], lg[:, 0:8], gm[:, 0:1], None, ALU.subtract, None, reads=[b_rt], writes=[b_rt])
            p.act(ex[:], ex[:], AF.Exp, reads=[b_rt], writes=[b_rt])
            p.op("dve", lambda t: t.tensor_reduce(out=pg[:, 0:1], in_=ex[:], axis=AX.X, op=ALU.add), reads=[b_rt], writes=[b_rt])
            p.op("dve", lambda t: t.reciprocal(out=pg[:, 0:1], in_=pg[:, 0:1]), reads=[b_rt], writes=[b_rt])
            p.ts("dve", goh[:], goh[:], 1e9, -1e9, ALU.mult, ALU.add, reads=[b_rt], writes=[b_rt])
            for g in range(8):
                p.ts("dve", es_[:, g * 8:(g + 1) * 8], lg[:, 8 + g * 8:16 + g * 8], goh[:, g:g + 1], None, ALU.add, None, reads=[b_rt], writes=[b_rt])
            p.op("dve", lambda t: t.max(out=t8[:], in_=es_[:]), reads=[b_rt], writes=[b_rt])
            p.tt("dve", w12[:, 0:1], t8[:, 1:2], t8[:, 0:1], ALU.subtract, reads=[b_rt], writes=[b_rt])
            p.act(w12[:, 0:1], w12[:, 0:1], AF.Exp, reads=[b_rt], writes=[b_rt])
            p.ts("dve", w12[:, 1:2], w12[:, 0:1], 1.0, None, ALU.add, None, reads=[b_rt], writes=[b_rt])
            p.op("dve", lambda t: t.reciprocal(out=w12[:, 1:2], in_=w12[:, 1:2]), reads=[b_rt], writes=[b_rt])
            p.tt("dve", w12[:, 2:3], w12[:, 0:1], w12[:, 1:2], ALU.mult, reads=[b_rt], writes=[b_rt])
            p.tt("dve", w12[:, 1:2], w12[:, 1:2], pg[:, 0:1], ALU.mult, reads=[b_rt], writes=[b_rt])
            p.tt("dve", w12[:, 2:3], w12[:, 2:3], pg[:, 0:1], ALU.mult, reads=[b_rt], writes=[b_rt])
            p.ts("dve", gw[:], es_[:], t8[:, 0:1], w12[:, 1:2], ALU.is_equal, ALU.mult, reads=[b_rt], writes=[b_rt])
            p.ts("dve", gw2[:], es_[:], t8[:, 1:2], w12[:, 2:3], ALU.is_equal, ALU.mult, reads=[b_rt], writes=[b_rt])
            p.tt("dve", gw[:], gw[:], gw2[:], ALU.add, reads=[b_rt], writes=[b_rt])
            p.transpose(psR[:64, 128:256], gw[:], ident[:], reads=[b_rt, b_c], writes=[b_psR])
            g0 = tc * 512 + sub * 128
            p.copy("dve", gwT[:, g0:g0 + 128], psR[:64, 128:256], reads=[b_psR], writes=[b_gw])

    emit_rms_mod2(p, xt, b_x, coef, vt[:, 3, :], b_v, hf32, b_hf, TL, scratch, router)
    p.dma("sp", gwTo, gwT[:], reads=[b_gw], is_output=True)
    return p.finish()


def build_moe(nchunk=T // 1024):
    p = Prog()
    hfT = p.dram_in("hfT", [D, T]); gw_d = p.dram_in("gw", [8, T])
    wg_d = p.dram_in("wg", [8, D, DE]); wu_d = p.dram_in("wu", [8, D, DE]); wd_d = p.dram_in("wd", [8, DE, D])
    yT = p.dram_out("yT", [D, T])
    CH = 1024
    HF = 256
    hbf = p.sb("hbf", [128, KC, CH], BF16); b_h = p.buf()
    acc = [p.sb("acc%d" % i, [128, KC, 512], F32) for i in range(2)]; b_acc = [p.buf(), p.buf()]
    wgt = [p.sb("wg%d" % i, [128, KC, HF], BF16) for i in range(2)]
    wut = [p.sb("wu%d" % i, [128, KC, HF], BF16) for i in range(2)]
    wdt = [p.sb("wd%d" % i, [128, HF // 128, D], BF16) for i in range(2)]
    b_we = [p.buf(), p.buf()]
    gwb = [p.sb("gwb%d" % i, [128, 512], F32) for i in range(2)]; b_gwb = [p.buf(), p.buf()]
    psG = p.ps("psG", [128, 512]); psU = p.ps("psU", [128, 512]); b_psG, b_psU = p.buf(), p.buf()
    pp = [p.ps("pp%d" % i, [128, 512]) for i in range(2)]; b_pp = [p.buf(), p.buf()]
    sg = p.sb("sg", [128, 512], F32); b_sg = p.buf()
    hw = [p.sb("hw%d" % i, [128, HF // 128, 512], BF16) for i in range(2)]; b_hw = [p.buf(), p.buf()]
    hv = hfT.rearrange("(k p) t -> p k t", p=128); yv = yT.rearrange("(k p) t -> p k t", p=128)
    unit = 0
    it = 0
    for c in range(nchunk):
        for k4 in range(4):
            p.dma("pool", hbf[:, k4 * 4:(k4 + 1) * 4, :], hv[:, k4 * 4:(k4 + 1) * 4, c * CH:(c + 1) * CH], writes=[b_h])
        for e in range(8):
            for half in range(DE // HF):
                s = unit % 2
                fs = slice(half * HF, (half + 1) * HF)
                p.dma("pool", wgt[s][:], wg_d[e, :, fs].rearrange("(k p) f -> p k f", p=128), writes=[b_we[s]])
                p.dma("pool", wut[s][:], wu_d[e, :, fs].rearrange("(k p) f -> p k f", p=128), writes=[b_we[s]])
                p.dma("pool", wdt[s][:], wd_d[e, fs, :].rearrange("(c p) d -> p c d", p=128), writes=[b_we[s]])
                for tc in range(CH // 512):
                    tsl = slice(tc * 512, (tc + 1) * 512)
                    gsl = slice(c * CH + tc * 512, c * CH + (tc + 1) * 512)
                    hs = it % 2; it += 1
                    p.dma("sp", gwb[hs][:], gw_d[e:e + 1, gsl].partition_broadcast(128), writes=[b_gwb[hs]])
                    for fc in range(HF // 128):
                        fsl = slice(fc * 128, (fc + 1) * 128)
                        for k in range(KC):
                            p.mm(psG[:], wgt[s][:, k, fsl], hbf[:, k, tsl], k == 0, k == KC - 1, reads=[b_we[s], b_h], writes=[b_psG])
                        for k in range(KC):
                            p.mm(psU[:], wut[s][:, k, fsl], hbf[:, k, tsl], k == 0, k == KC - 1, reads=[b_we[s], b_h], writes=[b_psU])
                        p.act(sg[:], psG[:], AF.Silu, reads=[b_psG], writes=[b_sg])
                        p.tt("dve", sg[:], sg[:], psU[:], ALU.mult, reads=[b_sg, b_psU], writes=[b_sg])
                        p.tt("pool", hw[hs][:, fc, :], sg[:], gwb[hs][:], ALU.mult, reads=[b_sg, b_gwb[hs]], writes=[b_hw[hs]])
                    first = (e == 0 and half == 0)
                    for dc in range(KC):
                        q = dc % 2
                        for fc in range(HF // 128):
                            p.mm(pp[q][:], wdt[s][:, fc, dc * 128:(dc + 1) * 128], hw[hs][:, fc, :], fc == 0, fc == HF // 128 - 1,
                                 reads=[b_we[s], b_hw[hs]], writes=[b_pp[q]])
                        if first:
                            p.copy("act", acc[tc][:, dc, :], pp[q][:], reads=[b_pp[q]], writes=[b_acc[tc]])
                        else:
                            p.tt("dve", acc[tc][:, dc, :], acc[tc][:, dc, :], pp[q][:], ALU.add, reads=[b_pp[q], b_acc[tc]], writes=[b_acc[tc]])
                unit += 1
        for tc in range(CH // 512):
            gsl = slice(c * CH + tc * 512, c * CH + (tc + 1) * 512)
            for k4 in range(4):
                p.dma("sp", yv[:, k4 * 4:(k4 + 1) * 4, gsl], acc[tc][:, k4 * 4:(k4 + 1) * 4, :], reads=[b_acc[tc]], is_output=True)
    return p.finish()


def build_postB(layer):
    last = (layer == 1)
    p = Prog()
    x1T = p.dram_in("x1T", [D, TL]); yP = p.dram_in("yP", [8, D, TL])
    vecs = p.dram_in("vecs", [128, 4, KC])
    xt = p.sb("xt", [128, KC, TL], F32); b_x = p.buf()
    vt = p.sb("vt", [128, 4, KC], F32); b_v = p.buf()
    coef = p.sb("coef", [128, KC], F32)
    scratch = alloc_rms_scratch(p)
    KG = 2
    ysum = p.sb("ysum", [128, KG, TL], F32); b_ys = p.buf()
    yt = [p.sb("yt%d" % i, [128, KG, TL], F32) for i in range(2)]; b_yt = [p.buf(), p.buf()]
    xv = x1T.rearrange("(k p) t -> p k t", p=128)
    for k4 in range(4):
        p.dma("sp", xt[:, k4 * 4:(k4 + 1) * 4, :], xv[:, k4 * 4:(k4 + 1) * 4, :], writes=[b_x])
    p.dma("sp", vt[:], vecs, writes=[b_v])
    p.ts("dve", coef[:], vt[:, 2, :], 1.0, None, ALU.add, None, reads=[b_v], writes=[b_v])
    p.tt("dve", coef[:], coef[:], vt[:, 1, :], ALU.mult, reads=[b_v], writes=[b_v])
    it = 0
    for k4 in range(KC // KG):
        ks = slice(k4 * KG, (k4 + 1) * KG)
        for g in range(8):
            s = it % 2; it += 1
            yv = yP[g].rearrange("(k p) t -> p k t", p=128)
            if g == 0:
                p.dma("sp", ysum[:], yv[:, ks, :], writes=[b_ys])
            else:
                p.dma("sp" if s == 0 else "act", yt[s][:], yv[:, ks, :], writes=[b_yt[s]])
                p.tt("dve" if g % 2 else "pool", ysum[:], ysum[:], yt[s][:], ALU.add, reads=[b_yt[s], b_ys], writes=[b_ys])
        for kk in range(KG):
            k = k4 * KG + kk
            p.op("dve", lambda t, k=k, kk=kk: t.scalar_tensor_tensor(out=xt[:, k, :], in0=ysum[:, kk, :], scalar=vt[:, 0, k:k + 1], in1=xt[:, k, :],
                                                                    op0=ALU.mult, op1=ALU.add), reads=[b_ys, b_v, b_x], writes=[b_x])
    if last:
        out = p.dram_out("outT", [D, TL])
        fo = p.sb("fo", [128, KC, TL], F32); b_fo = p.buf()
        emit_rms_plain(p, xt, b_x, vt[:, 1, :], b_v, fo, b_fo, TL, scratch)
        ov = out.rearrange("(k p) t -> p k t", p=128)
        for k4 in range(4):
            p.dma("sp", ov[:, k4 * 4:(k4 + 1) * 4, :], fo[:, k4 * 4:(k4 + 1) * 4, :], reads=[b_fo], is_output=True)
    else:
        x2T = p.dram_out("x2T", [D, TL]); w_next = p.dram_in("w_next", [D, ODD_COLS]); projN = p.dram_out("projN", [ODD_COLS, TL])
        hbf = p.sb("hbf", [128, KC, TL], BF16); b_h = p.buf()
        ov = x2T.rearrange("(k p) t -> p k t", p=128)
        for k4 in range(4):
            p.dma("sp", ov[:, k4 * 4:(k4 + 1) * 4, :], xt[:, k4 * 4:(k4 + 1) * 4, :], reads=[b_x], is_output=True)
        emit_rms_mod(p, xt, b_x, coef, vt[:, 3, :], b_v, hbf, b_h, TL, scratch)
        emit_proj(p, hbf, b_h, w_next, ODD_COLS, TL, projN)
    return p.finish()


def stage_post(inp, mod, layer, xT_full, mixT_full, gateT_full=None, dbg=None):
    ident = np.eye(128, dtype=np.float32)
    m = mod[layer]
    g1, sh2, sc2, g2 = m[2 * D:3 * D], m[3 * D:4 * D], m[4 * D:5 * D], m[5 * D:6 * D]
    z = np.zeros(D, np.float32)
    w_out = inp["ev_w_out"][0] if layer == 0 else inp["od_w_out"][0]
    vecsA = np.ascontiguousarray(np.stack([fm(v) for v in (g1, inp["norm_ffn_g"][layer], sc2, sh2)], axis=1))
    wr = np.ascontiguousarray(np.concatenate([inp["moe_w_grp"][layer], inp["moe_w_exp"][layer]], axis=1))
    br = np.ascontiguousarray(np.tile(np.concatenate([inp["moe_b_grp"][layer], inp["moe_b_exp"][layer]])[None, :], (128, 1)))
    maps = []
    for c in range(NCORE):
        sl = slice(c * TL, (c + 1) * TL)
        mp = {"xT": np.ascontiguousarray(xT_full[:, sl]), "mixT": np.ascontiguousarray(mixT_full[:, sl]), "w_out": w_out, "vecs": vecsA,
              "wr": wr, "br": br, "ident": ident}
        if layer == 1:
            mp["gateT"] = np.ascontiguousarray(gateT_full[:, sl])
        maps.append(mp)
    res = run_spmd(build_postA(layer), maps)
    x1T = np.concatenate([r["x1T"] for r in res], axis=1)
    hfT = np.concatenate([r["hfT"] for r in res], axis=1)
    gwT = np.concatenate([r["gwT"] for r in res], axis=1)
    if dbg is not None:
        dbg["x1T"], dbg["hfT"], dbg["gwT"] = x1T, hfT, gwT
    maps = []
    for g in range(NCORE):
        es = slice(g * 8, (g + 1) * 8)
        maps.append({"hfT": hfT, "gw": np.ascontiguousarray(gwT[es]), "wg": inp["moe_w_gate"][layer][es],
                     "wu": inp["moe_w_up"][layer][es], "wd": inp["moe_w_down"][layer][es]})
    res = run_spmd(build_moe(), maps)
    yP = np.stack([r["yT"] for r in res], axis=0)
    if dbg is not None:
        dbg["yP"] = yP
    if layer == 0:
        mn = mod[1]
        vecsB = np.ascontiguousarray(np.stack([fm(v) for v in (g2, inp["norm_mix_g"][1], mn[D:2 * D], mn[0:D])], axis=1))
    else:
        vecsB = np.ascontiguousarray(np.stack([fm(v) for v in (g2, inp["final_g"], z, z)], axis=1))
    maps = []
    for c in range(NCORE):
        sl = slice(c * TL, (c + 1) * TL)
        mp = {"x1T": np.ascontiguousarray(x1T[:, sl]), "yP": np.ascontiguousarray(yP[:, :, sl]), "vecs": vecsB}
        if layer == 0:
            mp["w_next"] = inp["od_w_in"][0]
        maps.append(mp)
    res = run_spmd(build_postB(layer), maps)
    if layer == 0:
        return (np.concatenate([r["x2T"] for r in res], axis=1), np.concatenate([r["projN"] for r in res], axis=1))
    return np.concatenate([r["outT"] for r in res], axis=1)

HC = 32
SEG = 1024


def build_L4(nseg=T // SEG):
    p = Prog()
    qT = p.dram_in("qT", [2, 128, T]); fT = p.dram_in("fT", [2, 128, T])
    vtok_d = p.dram_in("vtok", [2, HC, T // HC, 128])
    lbl_d = p.dram_in("lbl", [128, 2, 2])
    gn_d = p.dram_in("gn", [128, 2])
    rflag_d = p.dram_in("rflag", [128, SEG]); cmask_d = p.dram_in("cmask", [HC, HC]); ident_d = p.dram_in("ident", [128, 128])
    oT = p.dram_out("oT", [2, 128, T])
    NCH = SEG // HC
    ident = p.sb("ident", [128, 128], F32); rflag = p.sb("rflag", [128, SEG], F32); cmask = p.sb("cmask", [HC, HC], F32)
    onesf = p.sb("onesf", [128, 128], F32); epst = p.sb("epst", [128, 1], F32)
    lbl = p.sb("lbl", [128, 2, 2], F32); lb = p.sb("lb", [128, 2], F32); oml = p.sb("oml", [128, 2], F32); gn = p.sb("gn", [128, 2], F32)
    b_c = p.buf()
    p.dma("sp", ident[:], ident_d, writes=[b_c]); p.dma("sp", rflag[:], rflag_d, writes=[b_c]); p.dma("sp", cmask[:], cmask_d, writes=[b_c])
    p.dma("sp", lbl[:], lbl_d, writes=[b_c]); p.dma("sp", gn[:], gn_d, writes=[b_c])
    p.memset("dve", onesf[:], 1.0, [b_c]); p.memset("dve", epst[:], EPS, [b_c])
    p.tt("dve", lb[:], lbl[:, :, 1], lbl[:, :, 0], ALU.subtract, reads=[b_c], writes=[b_c])
    p.act(lb[:], lb[:], AF.Sigmoid, reads=[b_c], writes=[b_c])
    p.ts("dve", oml[:], lb[:], -1.0, 1.0, ALU.mult, ALU.add, reads=[b_c], writes=[b_c])
    psT = p.ps("psT", [128, 512]); b_psT = p.buf()
    psN = p.ps("psN", [128, 512]); b_psN = p.buf()
    H = []
    for hd in range(2):
        n = "h%d_" % hd
        d = dict(
            A=p.sb(n + "A", [128, SEG], F32), B=p.sb(n + "B", [128, SEG], F32), C=p.sb(n + "C", [128, SEG], F32),
            Bt=p.sb(n + "Bt", [128, SEG], F32), E=p.sb(n + "E", [128, SEG], F32), KA=p.sb(n + "KA", [128, SEG], F32),
            KD=p.sb(n + "KD", [128, SEG], F32), OS=p.sb(n + "OS", [128, SEG], F32),
            qe=p.sb(n + "qe", [128, SEG], BF16), ka=p.sb(n + "ka", [128, SEG], BF16),
            vt=p.sb(n + "vt", [HC, NCH, 128], BF16), kt=p.sb(n + "kt", [HC, NCH, 128], BF16),
            ebl=p.sb(n + "ebl", [128, NCH], F32), S32=p.sb(n + "S32", [128, 128], F32), Sbf=p.sb(n + "Sbf", [128, 128], BF16),
            attm=p.sb(n + "attm", [HC, HC], BF16), rs=p.sb(n + "rs", [128, 512], F32), oo=p.sb(n + "oo", [128, 512], F32),
            psA=p.ps(n + "psA", [128, 512]), psO=p.ps(n + "psO", [128, 512]), psS=p.ps(n + "psS", [128, 512]),
        )
        for k in ("A", "B", "C", "Bt", "E", "KA", "KD", "OS", "qe", "ka", "vt", "kt", "ebl", "S", "attm", "rs", "oo", "psA", "psO", "psS"):
            d["b_" + k] = p.buf()
        p.memset("dve", d["S32"][:], 0.0, [d["b_S"]])
        p.memset("dve", d["Sbf"][:], 0.0, [d["b_S"]])
        H.append(d)
    for sgi in range(nseg):
        tsl = slice(sgi * SEG, (sgi + 1) * SEG)
        for hd in range(2):
            d = H[hd]
            p.dma("sp", d["A"][:], qT[hd, :, tsl], writes=[d["b_A"]])
            p.dma("act", d["B"][:], fT[hd, :, tsl], writes=[d["b_B"]])
            p.dma("pool", d["vt"][:], vtok_d[hd, :, sgi * NCH:(sgi + 1) * NCH, :], writes=[d["b_vt"]])
            p.act(d["B"][:], d["B"][:], AF.Sigmoid, reads=[d["b_B"]], writes=[d["b_B"]])
            p.ts("dve", d["B"][:], d["B"][:], oml[:, hd:hd + 1], lb[:, hd:hd + 1], ALU.mult, ALU.add, reads=[d["b_B"], b_c], writes=[d["b_B"]])
            p.ts("dve", d["B"][:], d["B"][:], 1e-30, None, ALU.max, None, reads=[d["b_B"]], writes=[d["b_B"]])
            p.act(d["C"][:], d["B"][:], AF.Ln, reads=[d["b_B"]], writes=[d["b_C"]])
            p.op("dve", lambda t, d=d: t.tensor_tensor_scan(out=d["Bt"][:], data0=rflag[:], data1=d["C"][:], initial=0.0, op0=ALU.mult, op1=ALU.add),
                 reads=[d["b_C"], b_c], writes=[d["b_Bt"]])
            p.act(d["E"][:], d["Bt"][:], AF.Exp, reads=[d["b_Bt"]], writes=[d["b_E"]])
            p.act(d["A"][:], d["A"][:], AF.Silu, reads=[d["b_A"]], writes=[d["b_A"]])
            p.tt("dve", d["qe"][:], d["A"][:], d["E"][:], ALU.mult, reads=[d["b_A"], d["b_E"]], writes=[d["b_qe"]])
            p.act(d["E"][:], d["Bt"][:], AF.Exp, reads=[d["b_Bt"]], writes=[d["b_E"]], scale=-1.0)
            p.ts("dve", d["B"][:], d["B"][:], -1.0, 1.0, ALU.mult, ALU.add, reads=[d["b_B"]], writes=[d["b_B"]])
            p.tt("dve", d["KA"][:], d["B"][:], d["E"][:], ALU.mult, reads=[d["b_B"], d["b_E"]], writes=[d["b_KA"]])
            p.copy("pool", d["ka"][:], d["KA"][:], reads=[d["b_KA"]], writes=[d["b_ka"]])
            p.act(d["ebl"][:], d["Bt"][:].rearrange("p (c t) -> p c t", t=HC)[:, :, HC - 1], AF.Exp, reads=[d["b_Bt"]], writes=[d["b_ebl"]])
            for c in range(NCH):
                cs = slice(c * HC, (c + 1) * HC)
                p.ts("dve" if c % 2 == 0 else "pool", d["KD"][:, cs], d["KA"][:, cs], d["ebl"][:, c:c + 1], None, ALU.mult, None,
                     reads=[d["b_KA"], d["b_ebl"]], writes=[d["b_KD"]])
            for c in range(NCH):
                cs = slice(c * HC, (c + 1) * HC)
                p.transpose(psT[:HC, :128], d["KD"][:, cs], ident[:], reads=[d["b_KD"], b_c], writes=[b_psT])
                p.copy("act" if c % 2 == 0 else "dve", d["kt"][:, c, :], psT[:HC, :128], reads=[b_psT], writes=[d["b_kt"]])
        for c in range(NCH):
            cs = slice(c * HC, (c + 1) * HC)
            for hd in range(2):
                d = H[hd]
                p.mm(d["psA"][:HC, :HC], d["ka"][:, cs], d["qe"][:, cs], True, True, reads=[d["b_ka"], d["b_qe"]], writes=[d["b_psA"]])
                p.tt("dve", d["attm"][:], d["psA"][:HC, :HC], cmask[:], ALU.mult, reads=[d["b_psA"], b_c], writes=[d["b_attm"]])
                p.mm(d["psO"][:, :HC], d["Sbf"][:], d["qe"][:, cs], True, False, reads=[d["b_S"], d["b_qe"]], writes=[d["b_psO"]])
                p.mm(d["psO"][:, :HC], d["vt"][:, c, :], d["attm"][:], False, True, reads=[d["b_vt"], d["b_attm"]], writes=[d["b_psO"]])
                p.copy("act", d["OS"][:, cs], d["psO"][:, :HC], reads=[d["b_psO"]], writes=[d["b_OS"]])
                p.mm(d["psS"][:, :128], d["kt"][:, c, :], d["vt"][:, c, :], True, True, reads=[d["b_kt"], d["b_vt"]], writes=[d["b_psS"]])
                p.op("dve", lambda t, d=d, c=c: t.scalar_tensor_tensor(out=d["S32"][:], in0=d["S32"][:], scalar=d["ebl"][:, c:c + 1],
                                                                      in1=d["psS"][:, :128], op0=ALU.mult, op1=ALU.add),
                     reads=[d["b_psS"], d["b_ebl"], d["b_S"]], writes=[d["b_S"]])
                p.copy("pool", d["Sbf"][:], d["S32"][:], reads=[d["b_S"]], writes=[d["b_S"]])
        for hd in range(2):
            d = H[hd]
            p.act(d["C"][:], d["OS"][:], AF.Square, reads=[d["b_OS"]], writes=[d["b_C"]])
            for blk in range(SEG // 512):
                bs = slice(blk * 512, (blk + 1) * 512)
                p.mm(psN[:], onesf[:], d["C"][:, bs], True, True, reads=[b_c, d["b_C"]], writes=[b_psN])
                p.act(d["rs"][:], psN[:], AF.Sqrt, reads=[b_psN, b_c], writes=[d["b_rs"]], scale=1.0 / 128, bias=epst[:])
                p.op("dve", lambda t, d=d: t.reciprocal(out=d["rs"][:], in_=d["rs"][:]), reads=[d["b_rs"]], writes=[d["b_rs"]])
                p.op("dve", lambda t, d=d, bs=bs, hd=hd: t.scalar_tensor_tensor(out=d["oo"][:], in0=d["OS"][:, bs], scalar=gn[:, hd:hd + 1],
                                                                               in1=d["rs"][:], op0=ALU.mult, op1=ALU.mult),
                     reads=[d["b_OS"], d["b_rs"], b_c], writes=[d["b_oo"]])
                p.dma("sp", oT[hd, :, sgi * SEG + blk * 512:sgi * SEG + (blk + 1) * 512], d["oo"][:], reads=[d["b_oo"]], is_output=True)
    return p.finish()


def stage_L4(inp, projN, nseg=T // SEG):
    nc = build_L4(nseg)
    rflag = np.ones((128, SEG), np.float32); rflag[:, ::HC] = 0.0
    s = np.arange(HC)
    cmask = (s[:, None] <= s[None, :]).astype(np.float32)
    ident = np.eye(128, dtype=np.float32)
    maps = []
    for c in range(NCORE):
        hs = [2 * c, 2 * c + 1]
        qT = np.ascontiguousarray(np.stack([projN[h * 128:(h + 1) * 128] for h in hs]))
        fT = np.ascontiguousarray(np.stack([projN[2048 + h * 128:2048 + (h + 1) * 128] for h in hs]))
        vt = np.stack([projN[4096 + h * 128:4096 + (h + 1) * 128].T.reshape(T // HC, HC, 128).transpose(1, 0, 2) for h in hs])
        lbl = np.stack([inp["hg_lb_logits"][:, h * 128:(h + 1) * 128].T for h in hs], axis=1)
        gn = np.stack([inp["hg_norm_g"][0][h * 128:(h + 1) * 128] for h in hs], axis=1)
        maps.append({"qT": qT, "fT": fT, "vtok": np.ascontiguousarray(vt), "lbl": np.ascontiguousarray(lbl.astype(np.float32)),
                     "gn": np.ascontiguousarray(gn.astype(np.float32)), "rflag": rflag, "cmask": cmask, "ident": ident})
    res = run_spmd(nc, maps)
    return np.concatenate([r["oT"].reshape(256, T) for r in res], axis=0)


def kernel(**inp):
    import os, time
    dbgdir = os.environ.get("MK_DEBUG_DIR")
    t0 = time.time()

    def dump(name, a):
        print("[mk] %s done at %.0fs" % (name, time.time() - t0), flush=True)
        if dbgdir:
            np.save(os.path.join(dbgdir, "kd_%s.npy" % name), a)
    inp = {k: np.asarray(v) for k, v in inp.items()}
    mod = stage_L0(inp); dump("mod", mod)
    projT = stage_L1(inp, mod); dump("projT", projT)
    nsaT = stage_L2(inp, projT); dump("nsaT", nsaT)
    lruT = stage_L2b(inp, projT); dump("lruT", lruT)
    xT0 = np.ascontiguousarray(inp["x"][0].T)
    mixT = np.concatenate([nsaT, lruT], axis=0)
    dbg = {} if dbgdir else None
    x2T, projN = stage_post(inp, mod, 0, xT0, mixT, dbg=dbg); dump("x2T", x2T); dump("projN", projN)
    if dbg:
        for k, v in dbg.items():
            dump("L0_" + k, v)
    oT = stage_L4(inp, projN); dump("oT", oT)
    dbg = {} if dbgdir else None
    outT = stage_post(inp, mod, 1, x2T, oT, gateT_full=projN[6144:8192], dbg=dbg); dump("outT", outT)
    if dbg:
        for k, v in dbg.items():
            if k != "yP":
                dump("L1_" + k, v)
    return np.ascontiguousarray(outT.T)[None].astype(np.float32)
```

```python
import math
import numpy as np
import concourse.bass as bass
import concourse.mybir as mybir

F32 = mybir.dt.float32
BF16 = mybir.dt.bfloat16
I32 = mybir.dt.int32
U32 = mybir.dt.uint32
AF = mybir.ActivationFunctionType
ALU = mybir.AluOpType
AX = mybir.AxisListType

ENGS = ("pe", "act", "dve", "pool", "sp")


class Buf:
    __slots__ = ("name", "w", "r")

    def __init__(self, name):
        self.name = name
        self.w = None
        self.r = []


class Prog:
    NDMA = 24

    def __init__(self):
        nc = bass.Bass("TRN2", target_bir_lowering=False)
        self.nc = nc
        self.eng = {"pe": nc.tensor, "act": nc.scalar, "dve": nc.vector, "pool": nc.gpsimd, "sp": nc.sync}
        self.ops = {e: [] for e in ENGS}
        self.sem = {e: nc.alloc_semaphore("c_" + e) for e in ENGS}
        self.cnt = {e: 0 for e in ENGS}
        self.seen = {e: {} for e in ENGS}
        self.dsem = [nc.alloc_semaphore("d%d" % i) for i in range(self.NDMA)]
        self.dval = [0] * self.NDMA
        self.dnext = 0
        self.nbuf = 0
        self.out_tokens = []

    def sb(self, name, shape, dt):
        return self.nc.alloc_sbuf_tensor("s_" + name, list(shape), dt)

    def ps(self, name, shape, dt=F32):
        return self.nc.alloc_psum_tensor("p_" + name, list(shape), dt)

    def buf(self, name=None):
        self.nbuf += 1
        return Buf(name or "b%d" % self.nbuf)

    def dram_in(self, name, shape, dt=F32):
        return self.nc.dram_tensor(name, list(shape), dt, kind="ExternalInput").ap()

    def dram_out(self, name, shape, dt=F32):
        return self.nc.dram_tensor(name, list(shape), dt, kind="ExternalOutput").ap()

    def dram_tmp(self, name, shape, dt=F32):
        return self.nc.dram_tensor(name, list(shape), dt, kind="Internal").ap()

    def _need(self, e, toks):
        waits = {}
        for t in toks:
            if t is None:
                continue
            key, sem, val = t
            if self.seen[e].get(key, 0) >= val:
                continue
            if key not in waits or waits[key][1] < val:
                waits[key] = (sem, val)
        for key, (sem, val) in waits.items():
            self.seen[e][key] = val
        return list(waits.values())

    def _deps(self, reads, writes):
        toks = []
        for b in reads:
            toks.append(b.w)
        for b in writes:
            toks.append(b.w)
            toks.extend(b.r)
        return toks

    def _commit(self, tok, reads, writes):
        for b in reads:
            b.r.append(tok)
        for b in writes:
            b.w = tok
            b.r = []

    def op(self, e, fn, reads=(), writes=()):
        waits = self._need(e, self._deps(reads, writes))
        self.cnt[e] += 1
        val = self.cnt[e]
        sem = self.sem[e]
        tok = ("c_" + e, sem, val)

        def emit(eng, waits=waits, fn=fn, sem=sem):
            for s, v in waits:
                eng.wait_ge(s, v)
            fn(eng).then_inc(sem, 1)
        self.ops[e].append(emit)
        self._commit(tok, reads, writes)
        return tok

    def dma(self, q, out, in_, reads=(), writes=(), is_output=False, **kw):
        k = self.dnext
        self.dnext = (self.dnext + 1) % self.NDMA
        dsem = self.dsem[k]
        prev = self.dval[k]
        self.dval[k] = prev + 16
        key = "d%d" % k
        toks = self._deps(reads, writes)
        if prev > 0:
            toks.append((key, dsem, prev))
        waits = self._need(q, toks)
        tok = (key, dsem, prev + 16)

        def emit(eng, waits=waits, out=out, in_=in_, dsem=dsem, kw=kw):
            for s, v in waits:
                eng.wait_ge(s, v)
            eng.dma_start(out=out, in_=in_, **kw).then_inc(dsem, 16)
        self.ops[q].append(emit)
        self._commit(tok, reads, writes)
        if is_output:
            self.out_tokens.append(tok)
        return tok

    def raw(self, e, fn):
        self.ops[e].append(fn)

    def mm(self, out, lhsT, rhs, start, stop, reads, writes, **kw):
        return self.op("pe", lambda t: t.matmul(out, lhsT, rhs, start=start, stop=stop, **kw), reads, writes)

    def transpose(self, out, in_, ident, reads, writes):
        return self.op("pe", lambda t: t.transpose(out, in_, ident), reads, writes)

    def act(self, out, in_, func, reads, writes, e="act", **kw):
        return self.op(e, lambda t: t.activation(out=out, in_=in_, func=func, **kw), reads, writes)

    def tt(self, e, out, in0, in1, op, reads, writes):
        return self.op(e, lambda t: t.tensor_tensor(out=out, in0=in0, in1=in1, op=op), reads, writes)

    def ts(self, e, out, in0, s1, s2, op0, op1, reads, writes, **kw):
        if op1 is None:
            return self.op(e, lambda t: t.tensor_scalar(out=out, in0=in0, scalar1=s1, scalar2=None, op0=op0, **kw), reads, writes)
        return self.op(e, lambda t: t.tensor_scalar(out=out, in0=in0, scalar1=s1, scalar2=s2, op0=op0, op1=op1, **kw), reads, writes)

    def copy(self, e, out, in_, reads, writes):
        if e == "act":
            return self.op(e, lambda t: t.copy(out=out, in_=in_), reads, writes)
        return self.op(e, lambda t: t.tensor_copy(out=out, in_=in_), reads, writes)

    def memset(self, e, out, val, writes):
        return self.op(e, lambda t: t.memset(out, val), (), writes)

    def finish(self):
        nc = self.nc
        waits = self._need("sp", self.out_tokens)

        def fin(eng, waits=waits):
            for s, v in waits:
                eng.wait_ge(s, v)
        self.ops["sp"].append(fin)
        ops = self.ops
        with nc.Block() as block:
            @block.tensor
            def _(t):
                for f in ops["pe"]:
                    f(t)

            @block.scalar
            def _(t):
                for f in ops["act"]:
                    f(t)

            @block.vector
            def _(t):
                for f in ops["dve"]:
                    f(t)

            @block.gpsimd
            def _(t):
                for f in ops["pool"]:
                    f(t)

            @block.sync
            def _(t):
                for f in ops["sp"]:
                    f(t)
        return nc

from concourse.bass_utils import run_bass_kernel_spmd
D = 2048
T = 8192
NCORE = 8
TL = T // NCORE
KC = D // 128
EPS = 1e-6
EVEN_COLS = 4632


def fm(v):
    v = np.asarray(v)
    return np.ascontiguousarray(v.reshape(-1, 128).T)


def run_spmd(nc, in_maps):
    res = run_bass_kernel_spmd(nc, in_maps, core_ids=list(range(NCORE)))
    return res.results


def build_L0():
    p = Prog()
    NCOL = 1536
    cT = p.dram_in("cT", [128, KC])
    w = p.dram_in("w", [2, D, NCOL])
    b = p.dram_in("b", [1, 2 * NCOL])
    out = p.dram_out("mod", [1, 2 * NCOL])
    ct = p.sb("ct", [128, KC], F32)
    cact = p.sb("cact", [128, KC], F32)
    bt = p.sb("bt", [1, 2 * NCOL], F32)
    ot = p.sb("ot", [1, 2 * NCOL], F32)
    wt = [p.sb("wt%d" % i, [128, KC, 512], F32) for i in range(2)]
    ps = [p.ps("ps%d" % i, [1, 512]) for i in range(2)]
    b_c, b_b, b_o = p.buf(), p.buf(), p.buf()
    b_w = [p.buf(), p.buf()]
    b_ps = [p.buf(), p.buf()]
    p.dma("sp", ct[:], cT, writes=[b_c])
    p.dma("sp", bt[:], b, writes=[b_b])
    p.act(cact[:], ct[:], AF.Silu, reads=[b_c], writes=[b_c])
    i = 0
    for l in range(2):
        for n in range(3):
            s = i % 2
            p.dma("sp" if s == 0 else "act", wt[s][:], w[l, :, n * 512:(n + 1) * 512].rearrange("(k p) n -> p k n", p=128),
                  writes=[b_w[s]])
            for k in range(KC):
                p.mm(ps[s][:], cact[:, k:k + 1], wt[s][:, k, :], k == 0, k == KC - 1, reads=[b_c, b_w[s]], writes=[b_ps[s]])
            c0 = l * NCOL + n * 512
            p.tt("dve", ot[:, c0:c0 + 512], ps[s][:], bt[:, c0:c0 + 512], ALU.add, reads=[b_ps[s], b_b], writes=[b_o])
            i += 1
    p.dma("sp", out, ot[:], reads=[b_o], is_output=True)
    return p.finish()


def stage_L0(inp):
    nc = build_L0()
    cT = fm(inp["c"][0])
    maps = []
    for c in range(NCORE):
        sl = slice(c * 1536, (c + 1) * 1536)
        maps.append({"cT": cT,
                     "w": np.ascontiguousarray(inp["ada_w"][:, :, sl]),
                     "b": np.ascontiguousarray(inp["ada_b"][:, sl]).reshape(1, -1)})
    res = run_spmd(nc, maps)
    mod = np.concatenate([r["mod"].reshape(2, 1536) for r in res], axis=1)
    return mod


def emit_rms_mod(p, xt, b_x, coef, shift, b_cs, hbf, b_h, ntok, scratch):
    onesf, epst, sq, tmp, rs, ps_ss, b_sq, b_tmp, b_rs, b_ss = scratch
    for tc in range(ntok // 512):
        tsl = slice(tc * 512, (tc + 1) * 512)
        p.act(sq[:], xt[:, :, tsl], AF.Square, reads=[b_x], writes=[b_sq])
        for k in range(KC):
            p.mm(ps_ss[:], onesf[:], sq[:, k, :], k == 0, k == KC - 1, reads=[b_sq], writes=[b_ss])
        p.act(rs[:], ps_ss[:], AF.Sqrt, reads=[b_ss], writes=[b_rs], scale=1.0 / D, bias=epst[:])
        p.op("dve", lambda t: t.reciprocal(out=rs[:], in_=rs[:]), reads=[b_rs], writes=[b_rs])
        for k in range(KC):
            j = k % 2
            p.op("dve", lambda t, k=k, j=j, tsl=tsl: t.scalar_tensor_tensor(out=tmp[j][:], in0=xt[:, k, tsl], scalar=coef[:, k:k + 1],
                                                                  in1=rs[:], op0=ALU.mult, op1=ALU.mult),
                 reads=[b_x, b_cs, b_rs], writes=[b_tmp[j]])
            p.act(hbf[:, k, tsl], tmp[j][:], AF.Identity, reads=[b_tmp[j], b_cs], writes=[b_h], bias=shift[:, k:k + 1])


def alloc_rms_scratch(p):
    onesf = p.sb("onesf", [128, 128], F32)
    epst = p.sb("epst", [128, 1], F32)
    sq = p.sb("sq", [128, KC, 512], F32)
    tmp = [p.sb("rtmp%d" % i, [128, 512], F32) for i in range(2)]
    rs = p.sb("rs", [128, 512], F32)
    ps_ss = p.ps("ps_ss", [128, 512])
    b_c = p.buf()
    p.memset("dve", onesf[:], 1.0, [b_c])
    p.memset("dve", epst[:], EPS, [b_c])
    return (onesf, epst, sq, tmp, rs, ps_ss, p.buf(), [p.buf(), p.buf()], p.buf(), p.buf())


def emit_proj(p, hbf, b_h, w_dram, ncols, ntok, out_dram, wq="pool", evac=None, wbufs=None, stage=None):
    if wbufs is None:
        wt = [p.sb("pw%d" % i, [128, KC, 512], BF16) for i in range(2)]
        b_w = [p.buf(), p.buf()]
        pp = [p.ps("pp%d" % i, [128, 512]) for i in range(2)]
        b_pp = [p.buf(), p.buf()]
        osb = [p.sb("po%d" % i, [128, 512], F32) for i in range(3)]
        b_o = [p.buf() for _ in range(3)]
    else:
        wt, b_w, pp, b_pp, osb, b_o = wbufs
    ntile = (ncols + 511) // 512
    it = 0
    for j in range(ntile):
        c0 = j * 512
        cw = min(512, ncols - c0)
        s = j % 2
        if stage is None:
            p.dma(wq, wt[s][:, :, :cw], w_dram[:, c0:c0 + cw].rearrange("(k p) n -> p k n", p=128), writes=[b_w[s]])
        else:
            st, b_st = stage
            p.dma("sp" if j % 2 == 0 else "act", st[:, :, :cw], w_dram[:, c0:c0 + cw].rearrange("(k p) n -> p k n", p=128), writes=[b_st])
            p.copy("pool", wt[s][:, :, :cw], st[:, :, :cw], reads=[b_st], writes=[b_w[s]])
        for m in range((cw + 127) // 128):
            mw = min(128, cw - m * 128)
            for tc in range(ntok // 512):
                tsl = slice(tc * 512, (tc + 1) * 512)
                q = it % 2
                o = it % 3
                for k in range(KC):
                    p.mm(pp[q][:mw, :], wt[s][:, k, m * 128:m * 128 + mw], hbf[:, k, tsl], k == 0, k == KC - 1,
                         reads=[b_w[s], b_h], writes=[b_pp[q]])
                if evac is None:
                    if it % 2 == 0:
                        p.act(osb[o][:mw, :], pp[q][:mw, :], AF.Copy, reads=[b_pp[q]], writes=[b_o[o]])
                    else:
                        p.copy("dve", osb[o][:mw, :], pp[q][:mw, :], reads=[b_pp[q]], writes=[b_o[o]])
                    p.dma("sp", out_dram[c0 + m * 128:c0 + m * 128 + mw, tsl], osb[o][:mw, :], reads=[b_o[o]], is_output=True)
                else:
                    evac(c0 + m * 128, mw, tc, pp[q], b_pp[q])
                it += 1


def build_L1():
    p = Prog()
    xT = p.dram_in("xT", [D, TL])
    vecs = p.dram_in("vecs", [128, 3, KC])
    w = p.dram_in("w", [D, EVEN_COLS])
    out = p.dram_out("projT", [EVEN_COLS, TL])
    xt = p.sb("xt", [128, KC, TL], F32)
    vt = p.sb("vt", [128, 3, KC], F32)
    coef = p.sb("coef", [128, KC], F32)
    hbf = p.sb("hbf", [128, KC, TL], BF16)
    b_x, b_v, b_h = p.buf(), p.buf(), p.buf()
    scratch = alloc_rms_scratch(p)
    xv = xT.rearrange("(k p) t -> p k t", p=128)
    for k4 in range(4):
        p.dma("sp" if k4 % 2 == 0 else "act", xt[:, k4 * 4:(k4 + 1) * 4, :], xv[:, k4 * 4:(k4 + 1) * 4, :], writes=[b_x])
    p.dma("sp", vt[:], vecs, writes=[b_v])
    p.ts("dve", coef[:], vt[:, 1, :], 1.0, None, ALU.add, None, reads=[b_v], writes=[b_v])
    p.tt("dve", coef[:], coef[:], vt[:, 0, :], ALU.mult, reads=[b_v], writes=[b_v])
    emit_rms_mod(p, xt, b_x, coef, vt[:, 2, :], b_v, hbf, b_h, TL, scratch)
    emit_proj(p, hbf, b_h, w, EVEN_COLS, TL, out, stage=(scratch[2], scratch[6]))
    return p.finish()


def stage_L1(inp, mod):
    nc = build_L1()
    x = inp["x"][0]
    sh1, sc1 = mod[0, 0:D], mod[0, D:2 * D]
    vecs = np.ascontiguousarray(np.stack([fm(inp["norm_mix_g"][0]), fm(sc1), fm(sh1)], axis=1))
    maps = []
    for c in range(NCORE):
        maps.append({"xT": np.ascontiguousarray(x[c * TL:(c + 1) * TL].T), "vecs": vecs, "w": inp["ev_w_in"][0]})
    res = run_spmd(nc, maps)
    return np.concatenate([r["projT"] for r in res], axis=1)

HD = 128
QS = HD ** -0.5
NEGM = 30000.0 / QS
FX = 8192
FOFF = 4096
CMP_TILES = [0, 1, 2, 3, 4]
SLC_TILES = [-3, -2, -1, 0, 1]
WIN_TILES = [1, 2, 3, 4]
TINY = 1e-30


def t5_bucket_np(rel):
    n = np.maximum(rel, 0)
    nf = np.maximum(n, 1).astype(np.float32)
    large = 16 + (np.log(nf / 16) / math.log(128 / 16) * 16).astype(np.int32)
    large = np.minimum(large, 31)
    return np.where(n < 16, n, large)


def nsa_consts():
    rel = np.arange(FX) - FOFF
    bk = t5_bucket_np(rel)
    oh = np.zeros((34, FX), np.float32)
    valid = rel >= 0
    oh[bk[valid], np.nonzero(valid)[0]] += 1.0
    oh[31, valid] -= 1.0
    oh[32, ~valid] = 1.0
    oh[33, rel >= 512] = 1.0
    n = np.arange(512)
    j = np.arange(128)
    cover = ((16 * n[:, None] < 64 * j[None, :] + 64) & (16 * n[:, None] + 32 > 64 * j[None, :])).astype(np.float32)
    cover[511, :] = 0.0
    cover1 = np.concatenate([cover, np.ones((512, 1), np.float32)], axis=1)
    cover1 = np.ascontiguousarray(cover1.reshape(4, 128, 129).transpose(1, 0, 2))
    ebig = (np.arange(128)[:, None] == (np.arange(8192)[None, :] // 64)).astype(np.float32)
    ql = np.arange(128)[:, None]
    u = np.arange(256)[None, :] - 128
    fext = np.where(u < ql // 64, 0.0, np.where(u == ql // 64, 1e4, -1e4)).astype(np.float32)
    ident = np.eye(128, dtype=np.float32)
    return oh, cover1, ebig, fext, ident


def build_L2(nqc=T // 512, do_lru=True):
    p = Prog()
    q4T = p.dram_in("q4T", [128, 4, T])
    kcT = p.dram_in("kcT", [128, T]); vcT = p.dram_in("vcT", [128, T])
    ksT = p.dram_in("ksT", [128, T]); kwT = p.dram_in("kwT", [128, T])
    vs_d = p.dram_in("vs", [128, 64, 128]); vw_d = p.dram_in("vw", [128, 64, 128])
    glT = p.dram_in("glT", [3, T])
    tbl_d = p.dram_in("tblrep", [34, 4, 128])
    oh_d = p.dram_in("oh", [34, FX])
    cover_d = p.dram_in("cover1", [128, 4, 129])
    ebig_d = p.dram_in("ebig", [128, T])
    fext_d = p.dram_in("fext", [128, 256])
    ident_d = p.dram_in("ident", [128, 128])
    c31_d = p.dram_in("c31", [128, 4])
    w1_d = p.dram_in("w1", [2, 4096, 128]); peT_d = p.dram_in("peT", [2, 128, 32])
    b1_d = p.dram_in("b1", [128, 2]); w2_d = p.dram_in("w2", [2, 128, 128])
    b2k_d = p.dram_in("b2k", [128, 1]); b2v_d = p.dram_in("b2v", [128, 128])
    nsaT = p.dram_out("nsaT", [128, T])
    R_d = [p.dram_tmp("R%d" % i, [128, FX], BF16) for i in range(5)]

    identb = p.sb("identb", [128, 128], BF16)
    onesb = p.sb("onesb", [128, 128], BF16)
    cover = p.sb("cover", [128, 4, 129], BF16)
    ebig = p.sb("ebig", [128, T], BF16)
    fext = p.sb("fext", [128, 256], F32)
    c31 = p.sb("c31", [128, 4], F32)
    tbl = p.sb("tbl", [34, 4, 128], F32); oh = p.sb("oh", [34, 512], F32)
    b_const = p.buf()
    p.dma("pool", identb[:], ident_d, writes=[b_const])
    p.dma("pool", cover[:], cover_d, writes=[b_const])
    p.dma("pool", ebig[:], ebig_d, writes=[b_const])
    p.dma("sp", fext[:], fext_d, writes=[b_const])
    p.dma("sp", c31[:], c31_d, writes=[b_const])
    p.dma("sp", tbl[:], tbl_d, writes=[b_const])
    p.memset("dve", onesb[:], 1.0, [b_const])

    psA = [p.ps("psA%d" % i, [128, 512]) for i in range(2)]
    b_psA = [p.buf(), p.buf()]
    psO = [p.ps("psO%d" % i, [128, 512]) for i in range(2)]
    psZ = [p.ps("psZ%d" % i, [128, 512]) for i in range(2)]
    b_psO = [p.buf(), p.buf()]; b_psZ = [p.buf(), p.buf()]
    psI = p.ps("psI", [128, 512]); b_psI = p.buf()
    psT = p.ps("psT", [128, 128], BF16); b_psT = p.buf()

    rst = [p.sb("rst%d" % i, [128, 512], BF16) for i in range(2)]
    b_rst = [p.buf(), p.buf()]
    b_oh, b_R = p.buf(), p.buf()
    it = 0
    for xc in range(FX // 512):
        p.dma("sp", oh[:], oh_d[:, xc * 512:(xc + 1) * 512], writes=[b_oh])
        for r in range(5):
            s = it % 2
            hh = r if r < 4 else 0
            nrow = 33 if r < 4 else 34
            p.mm(psA[s][:], tbl[:nrow, hh, :], oh[:nrow, :], True, True, reads=[b_const, b_oh], writes=[b_psA[s]])
            p.act(rst[s][:], psA[s][:], AF.Copy, reads=[b_psA[s]], writes=[b_rst[s]], scale=1.0 / QS)
            p.dma("sp", R_d[r][:, xc * 512:(xc + 1) * 512], rst[s][:], reads=[b_rst[s]], writes=[b_R])
            it += 1

    def toep(Rd, pstride, base):
        return bass.AP(Rd.tensor, base, [[FX - pstride, 128], [1, 512]])

    bt_cmp = p.sb("bt_cmp", [128, 4, 5, 512], BF16)
    bt_slc = p.sb("bt_slc", [128, 5, 512], BF16)
    bt_win = p.sb("bt_win", [128, 4, 512], BF16)
    b_bt = p.buf()
    for hh in range(4):
        for a, d in enumerate(CMP_TILES):
            p.dma("sp", bt_cmp[:, hh, a, :], toep(R_d[hh], 16, FOFF + 512 * d - 31), reads=[b_R], writes=[b_bt])
    for a, d in enumerate(SLC_TILES):
        p.dma("sp", bt_slc[:, a, :], toep(R_d[0], 1, FOFF + 128 * d), reads=[b_R], writes=[b_bt])
    for a, d in enumerate(WIN_TILES):
        p.dma("sp", bt_win[:, a, :], toep(R_d[4], 1, FOFF + 128 * d), reads=[b_R], writes=[b_bt])

    kA = p.sb("kA", [128, T], BF16)
    vA = p.sb("vA", [128, T], BF16)
    ks = p.sb("ks", [128, T], BF16)
    vs = p.sb("vs", [128, 64, 128], BF16)
    b_kA, b_vA, b_ks, b_vs = p.buf(), p.buf(), p.buf(), p.buf()
    p.dma("pool", kA[:], kcT, writes=[b_kA])
    p.dma("pool", vA[:], vcT, writes=[b_vA])
    p.dma("pool", ks[:], ksT, writes=[b_ks])
    p.dma("pool", vs[:], vs_d, writes=[b_vs])

    ecmp = p.sb("ecmp", [128, 4, 4, 512], BF16)
    ecf = ecmp[:].rearrange("p a b c -> p (a b c)")
    w1 = [ecf[:, kv * 4096:(kv + 1) * 4096].rearrange("p (j o) -> p j o", o=128) for kv in range(2)]
    peT = p.sb("peT", [128, 2, 32], BF16)
    b1 = p.sb("b1", [128, 2], F32); w2 = p.sb("w2", [128, 2, 128], BF16)
    b2k = p.sb("b2k", [128, 1], F32); b2v = p.sb("b2v", [128, 128], F32)
    b_cw = p.buf()
    for kv in range(2):
        p.dma("pool", w1[kv], w1_d[kv].rearrange("(j d) o -> d j o", d=128), writes=[b_cw])
        p.dma("pool", peT[:, kv, :], peT_d[kv], writes=[b_cw])
        p.dma("pool", w2[:, kv, :], w2_d[kv], writes=[b_cw])
    p.dma("sp", b1[:], b1_d, writes=[b_cw]); p.dma("sp", b2k[:], b2k_d, writes=[b_cw]); p.dma("sp", b2v[:], b2v_d, writes=[b_cw])
    kcmpT = p.sb("kcmpT", [128, 512], BF16)
    vcmp = p.sb("vcmp", [128, 4, 128], BF16)
    hidT = p.sb("hidT", [128, 512], BF16)
    g1t = p.sb("g1t", [128, 512], F32); g2t = p.sb("g2t", [128, 512], F32); g3t = p.sb("g3t", [128, 512], F32)
    cb = p.sb("cb", [128, 1], F32)
    b_kc, b_vc, b_hid, b_g, b_cb = p.buf(), p.buf(), p.buf(), p.buf(), p.buf()
    p.memset("dve", hidT[:], 0.0, [b_hid])
    p.memset("dve", kcmpT[:], 0.0, [b_kc])

    def gelu_tanh(src, b_src, dst, b_dst, bias_ap, reads_extra, width):
        p.act(g1t[:, :width], src, AF.Identity, reads=[b_src] + reads_extra, writes=[b_g], bias=bias_ap)
        p.tt("dve", g2t[:, :width], g1t[:, :width], g1t[:, :width], ALU.mult, reads=[b_g], writes=[b_g])
        p.ts("dve", g2t[:, :width], g2t[:, :width], 0.044715, 1.0, ALU.mult, ALU.add, reads=[b_g], writes=[b_g])
        p.tt("dve", g2t[:, :width], g2t[:, :width], g1t[:, :width], ALU.mult, reads=[b_g], writes=[b_g])
        p.act(g3t[:, :width], g2t[:, :width], AF.Sigmoid, reads=[b_g], writes=[b_g], scale=1.5957691216)
        p.tt("dve", dst, g3t[:, :width], g1t[:, :width], ALU.mult, reads=[b_g], writes=[b_dst])

    for kv, (src, b_src) in enumerate(((kA, b_kA), (vA, b_vA))):
        sv = src[:].rearrange("p (n s) -> p n s", s=16)
        for j in range(32):
            p.mm(psA[1][:, 0:1], w1[kv][:, j, :], peT[:, kv, j:j + 1], j == 0, j == 31, reads=[b_cw], writes=[b_psA[1]])
        p.tt("dve", cb[:], psA[1][:, 0:1], b1[:, kv:kv + 1], ALU.add, reads=[b_psA[1], b_cw], writes=[b_cb])
        for j in range(32):
            jq, jr = j // 16, j % 16
            p.mm(psA[0][:, :511], w1[kv][:, j, :], sv[:, jq:jq + 511, jr], j == 0, j == 31, reads=[b_cw, b_src], writes=[b_psA[0]])
        gelu_tanh(psA[0][:, :511], b_psA[0], hidT[:, :511], b_hid, cb[:], [b_cb], 511)
        if kv == 0:
            p.mm(psA[1][:, :512], w2[:, 0, :], hidT[:], True, True, reads=[b_cw, b_hid], writes=[b_psA[1]])
            p.act(kcmpT[:, :511], psA[1][:, :511], AF.Identity, reads=[b_psA[1], b_cw], writes=[b_kc], bias=b2k[:])
        else:
            for c in range(4):
                p.mm(psA[1][:, :128], hidT[:, c * 128:(c + 1) * 128], w2[:, 1, :], True, True, reads=[b_cw, b_hid], writes=[b_psA[1]])
                p.tt("dve", vcmp[:, c, :], psA[1][:, :128], b2v[:], ALU.add, reads=[b_psA[1], b_cw], writes=[b_vc])
    vAv = vA[:].rearrange("p (j d) -> p j d", d=128)
    p.dma("pool", kA[:], kwT, writes=[b_kA])
    p.dma("pool", vAv, vw_d, writes=[b_vA])

    q4c = [p.sb("q4c%d" % i, [128, 4, 512], BF16) for i in range(2)]
    b_q = [p.buf(), p.buf()]
    gt0 = p.sb("gt0", [128, 3, 512], F32)
    gt = [gt0, gt0]
    b_gt0 = p.buf()
    b_gt = [b_gt0, b_gt0]
    b_ecmp = [p.buf() for _ in range(4)]
    impacc = p.sb("impacc", [128, 4, 128], F32); b_imp = p.buf()
    rz1 = p.sb("rz1", [128, 1], F32); b_rz1 = p.buf()
    vals = p.sb("vals", [128, 128], F32); work = p.sb("work", [128, 128], F32)
    m8a = p.sb("m8a", [128, 8], F32); m8b = p.sb("m8b", [128, 8], F32)
    selq = p.sb("selq", [128, 128], BF16)
    b_sel = p.buf()
    selT = p.sb("selT", [128, 512], BF16); b_selT = p.buf()
    es = [p.sb("es%d" % i, [128, 512], BF16) for i in range(3)]
    b_es = [p.buf() for _ in range(3)]
    rz = p.sb("rz", [128, 512], F32); wgt = p.sb("wgt", [128, 512], F32); b_rz = p.buf()
    onsa = [p.sb("onsa%d" % i, [128, 512], F32) for i in range(2)]
    b_on = [p.buf(), p.buf()]
    esi = 0
    ozi = 0

    def branch_finish(i, br, oz, first):
        s = i % 2
        p.ts("dve", rz[:], psZ[oz][:], TINY, None, ALU.max, None, reads=[b_psZ[oz]], writes=[b_rz])
        p.op("dve", lambda t: t.reciprocal(out=rz[:], in_=rz[:]), reads=[b_rz], writes=[b_rz])
        p.tt("dve", wgt[:], rz[:], gt[s][:, br, :], ALU.mult, reads=[b_rz, b_gt[s]], writes=[b_rz])
        if first:
            p.tt("dve", onsa[s][:], psO[oz][:], wgt[:], ALU.mult, reads=[b_psO[oz], b_rz], writes=[b_on[s]])
        else:
            p.tt("dve", wgt[:], psO[oz][:], wgt[:], ALU.mult, reads=[b_psO[oz], b_rz], writes=[b_rz])
            p.tt("dve", onsa[s][:], onsa[s][:], wgt[:], ALU.add, reads=[b_rz], writes=[b_on[s]])

    for i in range(nqc):
        s = i % 2
        tsl = slice(i * 512, (i + 1) * 512)
        p.dma("pool", q4c[s][:], q4T[:, :, tsl], writes=[b_q[s]])
        p.dma("sp", gt[s][:], glT[:, tsl].partition_broadcast(128), writes=[b_gt[s]])
        p.act(gt[s][:], gt[s][:], AF.Sigmoid, reads=[b_gt[s]], writes=[b_gt[s]])
        njs = [nj for nj in range(4) if i - 4 * nj >= 0]
        first_imp = True
        for hh in range(4):
            for nj in njs:
                d = i - 4 * nj
                a = esi % 2; esi += 1
                p.mm(psA[a][:], kcmpT[:, nj * 128:(nj + 1) * 128], q4c[s][:, hh, :], True, d > 4, reads=[b_kc, b_q[s]], writes=[b_psA[a]])
                if d <= 4:
                    p.mm(psA[a][:], identb[:], bt_cmp[:, hh, d, :], False, True, reads=[b_const, b_bt], writes=[b_psA[a]])
                p.act(ecmp[:, hh, nj, :], psA[a][:], AF.Exp, reads=[b_psA[a], b_const], writes=[b_ecmp[hh], b_cw], scale=QS, bias=c31[:, hh:hh + 1])
            for qs in range(4):
                for x, nj in enumerate(njs):
                    p.mm(psI[:, :129], ecmp[:, hh, nj, qs * 128:(qs + 1) * 128], cover[:, nj, :], x == 0, x == len(njs) - 1,
                         reads=[b_ecmp[hh], b_const], writes=[b_psI])
                p.ts("dve", rz1[:], psI[:, 128:129], TINY, None, ALU.max, None, reads=[b_psI], writes=[b_rz1])
                p.op("dve", lambda t: t.reciprocal(out=rz1[:], in_=rz1[:]), reads=[b_rz1], writes=[b_rz1])
                if hh == 0:
                    p.ts("dve", impacc[:, qs, :], psI[:, :128], rz1[:], None, ALU.mult, None, reads=[b_psI, b_rz1], writes=[b_imp])
                else:
                    p.op("dve", lambda t, qs=qs: t.scalar_tensor_tensor(out=impacc[:, qs, :], in0=psI[:, :128], scalar=rz1[:],
                                                                      in1=impacc[:, qs, :], op0=ALU.mult, op1=ALU.add),
                         reads=[b_psI, b_rz1, b_imp], writes=[b_imp])
            if hh == 0:
                oz = ozi % 2; ozi += 1
                for x, nj in enumerate(njs):
                    p.mm(psO[oz][:], vcmp[:, nj, :], ecmp[:, 0, nj, :], x == 0, x == len(njs) - 1, reads=[b_vc, b_ecmp[0]], writes=[b_psO[oz]])
                for x, nj in enumerate(njs):
                    p.mm(psZ[oz][:], onesb[:], ecmp[:, 0, nj, :], x == 0, x == len(njs) - 1, reads=[b_const, b_ecmp[0]], writes=[b_psZ[oz]])
                branch_finish(i, 0, oz, True)
        for qs in range(4):
            qb = 4 * i + qs
            p.tt("dve", vals[:], impacc[:, qs, :], fext[:, 128 - 2 * qb:256 - 2 * qb], ALU.add, reads=[b_imp, b_const], writes=[b_sel])
            p.op("dve", lambda t: t.max(out=m8a[:], in_=vals[:]), reads=[b_sel], writes=[b_sel])
            p.op("dve", lambda t: t.match_replace(out=work[:], in_to_replace=m8a[:], in_values=vals[:], imm_value=-1e30), reads=[b_sel], writes=[b_sel])
            p.op("dve", lambda t: t.max(out=m8b[:], in_=work[:]), reads=[b_sel], writes=[b_sel])
            p.ts("dve", work[:], vals[:], m8b[:, 7:8], None, ALU.is_ge, None, reads=[b_sel], writes=[b_sel])
            p.ts("dve", selq[:], work[:], NEGM, -NEGM, ALU.mult, ALU.add, reads=[b_sel], writes=[b_sel])
            p.transpose(psT[:], selq[:], identb[:], reads=[b_sel, b_const], writes=[b_psT])
            p.copy("dve", selT[:, qs * 128:(qs + 1) * 128], psT[:], reads=[b_psT], writes=[b_selT])
        oz = ozi % 2; ozi += 1
        nkb = 4 * i + 4
        for j in range(nkb):
            d = 4 * i - j
            a = esi % 2; esi += 1
            e = esi % 3
            p.mm(psA[a][:], ks[:, j * 128:(j + 1) * 128], q4c[s][:, 0, :], True, False, reads=[b_ks, b_q[s]], writes=[b_psA[a]])
            p.mm(psA[a][:], ebig[:, j * 128:(j + 1) * 128], selT[:], False, d > 1, reads=[b_const, b_selT], writes=[b_psA[a]])
            if d <= 1:
                p.mm(psA[a][:], identb[:], bt_slc[:, d + 3, :], False, True, reads=[b_const, b_bt], writes=[b_psA[a]])
            p.act(es[e][:], psA[a][:], AF.Exp, reads=[b_psA[a], b_const], writes=[b_es[e]], scale=QS, bias=c31[:, 0:1])
            p.mm(psO[oz][:], vs[:, j, :], es[e][:], j == 0, j == nkb - 1, reads=[b_vs, b_es[e]], writes=[b_psO[oz]])
            p.mm(psZ[oz][:], onesb[:], es[e][:], j == 0, j == nkb - 1, reads=[b_const, b_es[e]], writes=[b_psZ[oz]])
        branch_finish(i, 1, oz, False)
        oz = ozi % 2; ozi += 1
        js = list(range(max(0, 4 * i - 4), 4 * i + 4))
        for x, j in enumerate(js):
            d = 4 * i - j
            a = esi % 2; esi += 1
            e = esi % 3
            p.mm(psA[a][:], kA[:, j * 128:(j + 1) * 128], q4c[s][:, 0, :], True, False, reads=[b_kA, b_q[s]], writes=[b_psA[a]])
            btile = bt_slc[:, d + 3, :] if d <= 0 else bt_win[:, d - 1, :]
            p.mm(psA[a][:], identb[:], btile, False, True, reads=[b_const, b_bt], writes=[b_psA[a]])
            p.act(es[e][:], psA[a][:], AF.Exp, reads=[b_psA[a], b_const], writes=[b_es[e]], scale=QS, bias=c31[:, 0:1])
            p.mm(psO[oz][:], vAv[:, j, :], es[e][:], x == 0, x == len(js) - 1, reads=[b_vA, b_es[e]], writes=[b_psO[oz]])
            p.mm(psZ[oz][:], onesb[:], es[e][:], x == 0, x == len(js) - 1, reads=[b_const, b_es[e]], writes=[b_psZ[oz]])
        branch_finish(i, 2, oz, False)
        p.dma("sp", nsaT[:, tsl], onsa[s][:], reads=[b_on[s]], is_output=True)

    return p.finish()


def build_L2b():
    p = Prog()
    CH = 1024
    xbr_d = p.dram_in("xbr", [128, T]); ybr_d = p.dram_in("ybr", [128, T])
    lvec_d = p.dram_in("lvec", [128, 8])
    wa_d = p.dram_in("wa", [128, 128]); wi_d = p.dram_in("wi", [128, 128])
    lruT = p.dram_out("lruT", [128, T])
    lv = p.sb("lv", [128, 8], F32); wa = p.sb("wa", [128, 128], F32); wi = p.sb("wi", [128, 128], F32)
    nsp = p.sb("nsp", [128, 2], F32)
    b_lc = p.buf()
    p.dma("sp", lv[:], lvec_d, writes=[b_lc]); p.dma("sp", wa[:], wa_d, writes=[b_lc]); p.dma("sp", wi[:], wi_d, writes=[b_lc])
    p.act(nsp[:, 0:1], lv[:, 7:8], AF.Exp, reads=[b_lc], writes=[b_lc], scale=-1.0)
    p.ts("dve", nsp[:, 0:1], nsp[:, 0:1], 1.0, None, ALU.add, None, reads=[b_lc], writes=[b_lc])
    p.act(nsp[:, 0:1], nsp[:, 0:1], AF.Ln, reads=[b_lc], writes=[b_lc])
    p.ts("dve", nsp[:, 1:2], nsp[:, 0:1], -16.0, None, ALU.mult, None, reads=[b_lc], writes=[b_lc])
    p.ts("dve", nsp[:, 0:1], nsp[:, 0:1], -8.0, None, ALU.mult, None, reads=[b_lc], writes=[b_lc])
    xb = [p.sb("lxb%d" % i, [128, CH + 3], F32) for i in range(2)]
    b_xb = [p.buf(), p.buf()]
    yb = p.sb("lyb", [128, CH], F32); b_yb = p.buf()
    xc = p.sb("lxc", [128, CH], F32); b_xc = p.buf()
    ra = p.sb("lra", [128, CH], F32); ri = p.sb("lri", [128, CH], F32); b_ra, b_ri = p.buf(), p.buf()
    av = p.sb("lav", [128, CH], F32); bv = p.sb("lbv", [128, CH], F32); b_av, b_bv = p.buf(), p.buf()
    hv = [p.sb("lhv%d" % i, [128, CH], F32) for i in range(2)]; b_hv = [p.buf(), p.buf()]
    t1 = p.sb("lt1", [128, CH], F32); t2 = p.sb("lt2", [128, CH], F32); b_t1, b_t2 = p.buf(), p.buf()
    ov = p.sb("lov", [128, CH], F32); b_ov = p.buf()
    return_ps = [p.ps("psL%d" % i, [128, 512]) for i in range(2)]
    b_lps = [p.buf(), p.buf()]
    p.memset("dve", xb[0][:, 0:3], 0.0, [b_xb[0]])
    for c in range(T // CH):
        s = c % 2
        tsl = slice(c * CH, (c + 1) * CH)
        p.dma("sp", xb[s][:, 3:], xbr_d[:, tsl], writes=[b_xb[s]])
        p.dma("sp", yb[:], ybr_d[:, tsl], writes=[b_yb])
        if c > 0:
            p.copy("dve", xb[s][:, 0:3], xb[1 - s][:, CH:CH + 3], reads=[b_xb[1 - s]], writes=[b_xb[s]])
        p.ts("dve", xc[:], xb[s][:, 0:CH], lv[:, 0:1], lv[:, 4:5], ALU.mult, ALU.add, reads=[b_xb[s], b_lc], writes=[b_xc])
        for k in range(1, 4):
            p.op("dve", lambda t, k=k, s=s: t.scalar_tensor_tensor(out=xc[:], in0=xb[s][:, k:k + CH], scalar=lv[:, k:k + 1], in1=xc[:],
                                                                  op0=ALU.mult, op1=ALU.add), reads=[b_xb[s], b_lc, b_xc], writes=[b_xc])
        for hc in range(CH // 512):
            hs = slice(hc * 512, (hc + 1) * 512)
            p.mm(return_ps[0][:], wa[:], xc[:, hs], True, True, reads=[b_lc, b_xc], writes=[b_lps[0]])
            p.act(ra[:, hs], return_ps[0][:], AF.Sigmoid, reads=[b_lps[0], b_lc], writes=[b_ra], bias=lv[:, 5:6])
            p.mm(return_ps[1][:], wi[:], xc[:, hs], True, True, reads=[b_lc, b_xc], writes=[b_lps[1]])
            p.act(ri[:, hs], return_ps[1][:], AF.Sigmoid, reads=[b_lps[1], b_lc], writes=[b_ri], bias=lv[:, 6:7])
        p.act(av[:], ra[:], AF.Exp, reads=[b_ra, b_lc], writes=[b_av], scale=nsp[:, 0:1])
        p.act(t1[:], ra[:], AF.Exp, reads=[b_ra, b_lc], writes=[b_t1], scale=nsp[:, 1:2])
        p.ts("dve", t1[:], t1[:], -1.0, 1.0, ALU.mult, ALU.add, reads=[b_t1], writes=[b_t1])
        p.ts("dve", t1[:], t1[:], 0.0, None, ALU.max, None, reads=[b_t1], writes=[b_t1])
        p.act(t1[:], t1[:], AF.Sqrt, reads=[b_t1], writes=[b_t1])
        p.tt("dve", bv[:], ri[:], xc[:], ALU.mult, reads=[b_ri, b_xc], writes=[b_bv])
        p.tt("dve", bv[:], bv[:], t1[:], ALU.mult, reads=[b_bv, b_t1], writes=[b_bv])
        init = 0.0 if c == 0 else hv[1 - s][:, CH - 1:CH]
        p.op("dve", lambda t, s=s, init=init: t.tensor_tensor_scan(out=hv[s][:], data0=av[:], data1=bv[:], initial=init,
                                                                   op0=ALU.mult, op1=ALU.add),
             reads=[b_av, b_bv] + ([b_hv[1 - s]] if c > 0 else []), writes=[b_hv[s]])
        p.tt("dve", t2[:], yb[:], yb[:], ALU.mult, reads=[b_yb], writes=[b_t2])
        p.ts("dve", t2[:], t2[:], 0.044715, 1.0, ALU.mult, ALU.add, reads=[b_t2], writes=[b_t2])
        p.tt("dve", t2[:], t2[:], yb[:], ALU.mult, reads=[b_t2, b_yb], writes=[b_t2])
        p.act(t2[:], t2[:], AF.Sigmoid, reads=[b_t2], writes=[b_t2], scale=1.5957691216)
        p.tt("dve", t2[:], t2[:], yb[:], ALU.mult, reads=[b_t2, b_yb], writes=[b_t2])
        p.tt("dve", ov[:], t2[:], hv[s][:], ALU.mult, reads=[b_t2, b_hv[s]], writes=[b_ov])
        p.dma("sp", lruT[:, tsl], ov[:], reads=[b_ov], is_output=True)
    return p.finish()


def stage_L2(inp, projT, nqc=T // 512):
    oh, cover1, ebig, fext, ident = nsa_consts()
    nc = build_L2(nqc)
    rb = inp["rel_bias"]
    maps = []
    for h in range(NCORE):
        g = h // 4
        heads = [h] + [hh for hh in range(4 * g, 4 * g + 4) if hh != h]
        q4 = np.ascontiguousarray(np.stack([projT[hh * 128:(hh + 1) * 128] for hh in heads], axis=1))

        def kv(idx):
            base = 1024 + idx * 256 + g * 128
            return projT[base:base + 128]

        def tokmaj(a):
            return np.ascontiguousarray(a.T.reshape(64, 128, 128).transpose(1, 0, 2))
        tblrep = np.zeros((34, 4, 128), np.float32)
        for a, hh in enumerate(heads):
            tblrep[:32, a, :] = rb[:, hh][:, None]
        tblrep[32:] = -30000.0
        c31 = np.ascontiguousarray(np.stack([np.full(128, rb[31, hh], np.float32) for hh in heads], axis=1))
        maps.append({
            "q4T": q4, "kcT": np.ascontiguousarray(kv(0)), "vcT": np.ascontiguousarray(kv(1)),
            "ksT": np.ascontiguousarray(kv(2)), "vs": tokmaj(kv(3)), "kwT": np.ascontiguousarray(kv(4)), "vw": tokmaj(kv(5)),
            "glT": np.ascontiguousarray(projT[2560 + 3 * h:2560 + 3 * h + 3]),
            "tblrep": tblrep, "oh": oh, "cover1": cover1, "ebig": ebig, "fext": fext, "ident": ident, "c31": c31,
            "w1": inp["cmp_w1"][0], "peT": np.ascontiguousarray(inp["cmp_pe"][0].transpose(0, 2, 1)),
            "b1": np.ascontiguousarray(inp["cmp_b1"][0].T), "w2": inp["cmp_w2"][0],
            "b2k": np.ascontiguousarray(inp["cmp_b2"][0][0].reshape(128, 1)),
            "b2v": np.ascontiguousarray(np.tile(inp["cmp_b2"][0][1][None, :], (128, 1))),
        })
    res = run_spmd(nc, maps)
    return np.concatenate([r["nsaT"] for r in res], axis=0)


def stage_L2b(inp, projT):
    nc = build_L2b()
    maps = []
    for c in range(NCORE):
        sl = slice(c * 128, (c + 1) * 128)
        lvec = np.stack([inp["lru_conv_w"][0][k, sl] for k in range(4)] +
                        [inp["lru_conv_b"][0][sl], inp["lru_ba"][0][sl], inp["lru_bi"][0][sl], inp["lru_lambda"][0][sl]], axis=1)
        maps.append({"xbr": np.ascontiguousarray(projT[3608 + c * 128:3608 + (c + 1) * 128]),
                     "ybr": np.ascontiguousarray(projT[2584 + c * 128:2584 + (c + 1) * 128]),
                     "lvec": np.ascontiguousarray(lvec.astype(np.float32)),
                     "wa": inp["lru_wa"][0][c], "wi": inp["lru_wi"][0][c]})
    res = run_spmd(nc, maps)
    return np.concatenate([r["lruT"] for r in res], axis=0)

NE = 64
DE = 512
ODD_COLS = 8192


def emit_rms_mod2(p, xt, b_x, coef, shift, b_cs, hbf32, b_hf, ntok, scratch, after_chunk):
    onesf, epst, sq, tmp, rs, ps_ss, b_sq, b_tmp, b_rs, b_ss = scratch
    for tc in range(ntok // 512):
        tsl = slice(tc * 512, (tc + 1) * 512)
        p.act(sq[:], xt[:, :, tsl], AF.Square, reads=[b_x], writes=[b_sq])
        for k in range(KC):
            p.mm(ps_ss[:], onesf[:], sq[:, k, :], k == 0, k == KC - 1, reads=[b_sq], writes=[b_ss])
        p.act(rs[:], ps_ss[:], AF.Sqrt, reads=[b_ss], writes=[b_rs], scale=1.0 / D, bias=epst[:])
        p.op("dve", lambda t: t.reciprocal(out=rs[:], in_=rs[:]), reads=[b_rs], writes=[b_rs])
        for k in range(KC):
            j = k % 2
            p.op("dve", lambda t, k=k, j=j, tsl=tsl: t.scalar_tensor_tensor(out=tmp[j][:], in0=xt[:, k, tsl], scalar=coef[:, k:k + 1],
                                                                           in1=rs[:], op0=ALU.mult, op1=ALU.mult),
                 reads=[b_x, b_cs, b_rs], writes=[b_tmp[j]])
            p.act(hbf32[:, k, :], tmp[j][:], AF.Identity, reads=[b_tmp[j], b_cs], writes=[b_hf], bias=shift[:, k:k + 1])
        after_chunk(tc)


def emit_rms_plain(p, xt, b_x, gain, b_g, outt, b_out, ntok, scratch):
    onesf, epst, sq, tmp, rs, ps_ss, b_sq, b_tmp, b_rs, b_ss = scratch
    for tc in range(ntok // 512):
        tsl = slice(tc * 512, (tc + 1) * 512)
        p.act(sq[:], xt[:, :, tsl], AF.Square, reads=[b_x], writes=[b_sq])
        for k in range(KC):
            p.mm(ps_ss[:], onesf[:], sq[:, k, :], k == 0, k == KC - 1, reads=[b_sq], writes=[b_ss])
        p.act(rs[:], ps_ss[:], AF.Sqrt, reads=[b_ss], writes=[b_rs], scale=1.0 / D, bias=epst[:])
        p.op("dve", lambda t: t.reciprocal(out=rs[:], in_=rs[:]), reads=[b_rs], writes=[b_rs])
        for k in range(KC):
            p.op("dve", lambda t, k=k, tsl=tsl: t.scalar_tensor_tensor(out=outt[:, k, tsl], in0=xt[:, k, tsl], scalar=gain[:, k:k + 1],
                                                                      in1=rs[:], op0=ALU.mult, op1=ALU.mult),
                 reads=[b_x, b_g, b_rs], writes=[b_out])


def build_postA(layer):
    last = (layer == 1)
    p = Prog()
    xT = p.dram_in("xT", [D, TL]); mixT = p.dram_in("mixT", [D, TL])
    gateT = p.dram_in("gateT", [D, TL]) if last else None
    w_out = p.dram_in("w_out", [D, D])
    vecs = p.dram_in("vecs", [128, 4, KC])
    wr_d = p.dram_in("wr", [D, 72]); br_d = p.dram_in("br", [128, 72]); ident_d = p.dram_in("ident", [128, 128])
    x1T = p.dram_out("x1T", [D, TL]); hfT = p.dram_out("hfT", [D, TL]); gwTo = p.dram_out("gwT", [64, TL])
    xt = p.sb("xt", [128, KC, TL], F32); b_x = p.buf()
    hbf = p.sb("hbf", [128, KC, TL], BF16); b_h = p.buf()
    vt = p.sb("vt", [128, 4, KC], F32); b_v = p.buf()
    coef = p.sb("coef", [128, KC], F32)
    ident = p.sb("ident", [128, 128], F32); b_c = p.buf()
    scratch = alloc_rms_scratch(p)
    xv = xT.rearrange("(k p) t -> p k t", p=128); mv = mixT.rearrange("(k p) t -> p k t", p=128)
    for k4 in range(4):
        p.dma("sp", xt[:, k4 * 4:(k4 + 1) * 4, :], xv[:, k4 * 4:(k4 + 1) * 4, :], writes=[b_x])
    p.dma("sp", vt[:], vecs, writes=[b_v]); p.dma("sp", ident[:], ident_d, writes=[b_c])
    p.ts("dve", coef[:], vt[:, 2, :], 1.0, None, ALU.add, None, reads=[b_v], writes=[b_v])
    p.tt("dve", coef[:], coef[:], vt[:, 1, :], ALU.mult, reads=[b_v], writes=[b_v])
    hf32 = scratch[2]; b_hf = scratch[6]
    if last:
        gv = gateT.rearrange("(k p) t -> p k t", p=128)
        b_m = p.buf()
        for k4 in range(8):
            ks = slice(k4 * 2, (k4 + 1) * 2)
            mt = hf32[:, 0:2, :].rearrange("p a (b t) -> p (a b) t", b=1) if False else None
            m2 = hf32[:, 0:4, :].rearrange("p (a b) t -> p a (b t)", b=2)
            g2 = hf32[:, 4:8, :].rearrange("p (a b) t -> p a (b t)", b=2)
            p.dma("sp", m2, mv[:, ks, :], writes=[b_hf])
            p.dma("act", g2, gv[:, ks, :], writes=[b_hf])
            p.act(g2, g2, AF.Silu, reads=[b_hf], writes=[b_hf])
            p.tt("dve", hbf[:, ks, :], m2, g2, ALU.mult, reads=[b_hf], writes=[b_h])
    else:
        for k4 in range(4):
            p.dma("pool", hbf[:, k4 * 4:(k4 + 1) * 4, :], mv[:, k4 * 4:(k4 + 1) * 4, :], writes=[b_h])

    def evac_res(c0, mw, tc, ps_t, b_ps):
        k = c0 // 128
        tsl = slice(tc * 512, (tc + 1) * 512)
        p.op("dve", lambda t: t.scalar_tensor_tensor(out=xt[:, k, tsl], in0=ps_t[:], scalar=vt[:, 0, k:k + 1], in1=xt[:, k, tsl],
                                                     op0=ALU.mult, op1=ALU.add), reads=[b_ps, b_v, b_x], writes=[b_x])
    emit_proj(p, hbf, b_h, w_out, D, TL, None, evac=evac_res, stage=(scratch[2], scratch[6]))
    ov = x1T.rearrange("(k p) t -> p k t", p=128)
    for k4 in range(4):
        p.dma("sp", ov[:, k4 * 4:(k4 + 1) * 4, :], xt[:, k4 * 4:(k4 + 1) * 4, :], reads=[b_x], is_output=True)
    wr = p.sb("wr", [128, KC, 72], F32); br = p.sb("br", [128, 72], F32)
    p.dma("sp", wr[:], wr_d.rearrange("(k p) n -> p k n", p=128), writes=[b_c]); p.dma("sp", br[:], br_d, writes=[b_c])
    gwT = p.sb("gwT", [64, TL], F32); b_gw = p.buf()
    psR = p.ps("psR", [128, 512]); b_psR = p.buf()
    lg = p.sb("lg", [128, 72], F32); gm = p.sb("gm", [128, 8], F32); goh = p.sb("goh", [128, 8], F32)
    ex = p.sb("ex", [128, 8], F32); pg = p.sb("pg", [128, 2], F32); es_ = p.sb("esel", [128, 64], F32)
    t8 = p.sb("t8", [128, 8], F32); w12 = p.sb("w12", [128, 4], F32); gw = p.sb("gw", [128, 64], F32); gw2 = p.sb("gw2", [128, 64], F32)
    b_rt = p.buf()
    hv = hfT.rearrange("(k p) t -> p k t", p=128)

    def router(tc):
        p.dma("sp", hv[:, :, tc * 512:(tc + 1) * 512], hf32[:], reads=[b_hf], is_output=True)
        for sub in range(4):
            tsl = slice(sub * 128, (sub + 1) * 128)
            for k in range(KC):
                p.mm(psR[:, :72], hf32[:, k, tsl], wr[:, k, :], k == 0, k == KC - 1, reads=[b_hf, b_c], writes=[b_psR])
            p.tt("dve", lg[:], psR[:, :72], br[:], ALU.add, reads=[b_psR, b_c], writes=[b_rt])
            p.op("dve", lambda t: t.tensor_reduce(out=gm[:, 0:1], in_=lg[:, 0:8], axis=AX.X, op=ALU.max), reads=[b_rt], writes=[b_rt])
            p.ts("dve", goh[:], lg[:, 0:8], gm[:, 0:1], None, ALU.is_ge, None, reads=[b_rt], writes=[b_rt])
            p.ts("dve", ex[:], lg[:, 0:8], gm[:, 0:1], None, ALU.subtract, None, reads=[b_rt], writes=[b_rt])
            p.act(ex[:], ex[:], AF.Exp, reads=[b_rt], writes=[b_rt])
            p.op("dve", lambda t: t.tensor_reduce(out=pg[:, 0:1], in_=ex[:], axis=AX.X, op=ALU.add), reads=[b_rt], writes=[b_rt])
            p.op("dve", lambda t: t.reciprocal(out=pg[:, 0:1], in_=pg[:, 0:1]), reads=[b_rt], writes=[b_rt])
            p.ts("dve", goh[:], goh[:], 1e9, -1e9, ALU.mult, ALU.add, reads=[b_rt], writes=[b_rt])
            for g in range(8):
                p.ts("dve", es_[:, g * 8:(g + 1) * 8], lg[:, 8 + g * 8:16 + g * 8], goh[:, g:g + 1], None, ALU.add, None, reads=[b_rt], writes=[b_rt])
            p.op("dve", lambda t: t.max(out=t8[:], in_=es_[:]), reads=[b_rt], writes=[b_rt])
            p.tt("dve", w12[:, 0:1], t8[:, 1:2], t8[:, 0:1], ALU.subtract, reads=[b_rt], writes=[b_rt])
            p.act(w12[:, 0:1], w12[:, 0:1], AF.Exp, reads=[b_rt], writes=[b_rt])
            p.ts("dve", w12[:, 1:2], w12[:, 0:1], 1.0, None, ALU.add, None, reads=[b_rt], writes=[b_rt])
            p.op("dve", lambda t: t.reciprocal(out=w12[:, 1:2], in_=w12[:, 1:2]), reads=[b_rt], writes=[b_rt])
            p.tt("dve", w12[:, 2:3], w12[:, 0:1], w12[:, 1:2], ALU.mult, reads=[b_rt], writes=[b_rt])
            p.tt("dve", w12[:, 1:2], w12[:, 1:2], pg[:, 0:1], ALU.mult, reads=[b_rt], writes=[b_rt])
            p.tt("dve", w12[:, 2:3], w12[:, 2:3], pg[:, 0:1], ALU.mult, reads=[b_rt], writes=[b_rt])
            p.ts("dve", gw[:], es_[:], t8[:, 0:1], w12[:, 1:2], ALU.is_equal, ALU.mult, reads=[b_rt], writes=[b_rt])
            p.ts("dve", gw2[:], es_[:], t8[:, 1:2], w12[:, 2:3], ALU.is_equal, ALU.mult, reads=[b_rt], writes=[b_rt])
            p.tt("dve", gw[:], gw[:], gw2[:], ALU.add, reads=[b_rt], writes=[b_rt])
            p.transpose(psR[:64, 128:256], gw[:], ident[:], reads=[b_rt, b_c], writes=[b_psR])
            g0 = tc * 512 + sub * 128
            p.copy("dve", gwT[:, g0:g0 + 128], psR[:64, 128:256], reads=[b_psR], writes=[b_gw])

    emit_rms_mod2(p, xt, b_x, coef, vt[:, 3, :], b_v, hf32, b_hf, TL, scratch, router)
    p.dma("sp", gwTo, gwT[:], reads=[b_gw], is_output=True)
    return p.finish()


def build_moe(nchunk=T // 1024):
    p = Prog()
    hfT = p.dram_in("hfT", [D, T]); gw_d = p.dram_in("gw", [8, T])
    wg_d = p.dram_in("wg", [8, D, DE]); wu_d = p.dram_in("wu", [8, D, DE]); wd_d = p.dram_in("wd", [8, DE, D])
    yT = p.dram_out("yT", [D, T])
    CH = 1024
    HF = 256
    hbf = p.sb("hbf", [128, KC, CH], BF16); b_h = p.buf()
    acc = [p.sb("acc%d" % i, [128, KC, 512], F32) for i in range(2)]; b_acc = [p.buf(), p.buf()]
    wgt = [p.sb("wg%d" % i, [128, KC, HF], BF16) for i in range(2)]
    wut = [p.sb("wu%d" % i, [128, KC, HF], BF16) for i in range(2)]
    wdt = [p.sb("wd%d" % i, [128, HF // 128, D], BF16) for i in range(2)]
    b_we = [p.buf(), p.buf()]
    stg = [p.sb("stg%d" % i, [128, KC, HF], F32) for i in range(3)]; b_stg = [p.buf() for _ in range(3)]
    gwb = [p.sb("gwb%d" % i, [128, 512], F32) for i in range(2)]; b_gwb = [p.buf(), p.buf()]
    psG = p.ps("psG", [128, 512]); psU = p.ps("psU", [128, 512]); b_psG, b_psU = p.buf(), p.buf()
    pp = [p.ps("pp%d" % i, [128, 512]) for i in range(2)]; b_pp = [p.buf(), p.buf()]
    sg = p.sb("sg", [128, 512], F32); b_sg = p.buf()
    hw = [p.sb("hw%d" % i, [128, HF // 128, 512], BF16) for i in range(2)]; b_hw = [p.buf(), p.buf()]
    hv = hfT.rearrange("(k p) t -> p k t", p=128); yv = yT.rearrange("(k p) t -> p k t", p=128)
    unit = 0
    it = 0
    for c in range(nchunk):
        for k4 in range(4):
            p.dma("pool", hbf[:, k4 * 4:(k4 + 1) * 4, :], hv[:, k4 * 4:(k4 + 1) * 4, c * CH:(c + 1) * CH], writes=[b_h])
        for e in range(8):
            for half in range(DE // HF):
                s = unit % 2
                fs = slice(half * HF, (half + 1) * HF)
                p.dma("sp", stg[0][:], wg_d[e, :, fs].rearrange("(k p) f -> p k f", p=128), writes=[b_stg[0]])
                p.dma("act", stg[1][:], wu_d[e, :, fs].rearrange("(k p) f -> p k f", p=128), writes=[b_stg[1]])
                p.dma("sp", stg[2][:].rearrange("p k f -> p (k f)").rearrange("p (c d) -> p c d", d=D),
                      wd_d[e, fs, :].rearrange("(c p) d -> p c d", p=128), writes=[b_stg[2]])
                p.copy("act", wgt[s][:], stg[0][:], reads=[b_stg[0]], writes=[b_we[s]])
                p.copy("act", wut[s][:], stg[1][:], reads=[b_stg[1]], writes=[b_we[s]])
                p.copy("pool", wdt[s][:], stg[2][:].rearrange("p k f -> p (k f)").rearrange("p (c d) -> p c d", d=D),
                       reads=[b_stg[2]], writes=[b_we[s]])
                for tc in range(CH // 512):
                    tsl = slice(tc * 512, (tc + 1) * 512)
                    gsl = slice(c * CH + tc * 512, c * CH + (tc + 1) * 512)
                    hs = it % 2; it += 1
                    p.dma("sp", gwb[hs][:], gw_d[e:e + 1, gsl].partition_broadcast(128), writes=[b_gwb[hs]])
                    for fc in range(HF // 128):
                        fsl = slice(fc * 128, (fc + 1) * 128)
                        for k in range(KC):
                            p.mm(psG[:], wgt[s][:, k, fsl], hbf[:, k, tsl], k == 0, k == KC - 1, reads=[b_we[s], b_h], writes=[b_psG])
                        for k in range(KC):
                            p.mm(psU[:], wut[s][:, k, fsl], hbf[:, k, tsl], k == 0, k == KC - 1, reads=[b_we[s], b_h], writes=[b_psU])
                        p.act(sg[:], psG[:], AF.Silu, reads=[b_psG], writes=[b_sg])
                        p.tt("dve", sg[:], sg[:], psU[:], ALU.mult, reads=[b_sg, b_psU], writes=[b_sg])
                        p.tt("pool", hw[hs][:, fc, :], sg[:], gwb[hs][:], ALU.mult, reads=[b_sg, b_gwb[hs]], writes=[b_hw[hs]])
                    first = (e == 0 and half == 0)
                    for dc in range(KC):
                        q = dc % 2
                        for fc in range(HF // 128):
                            p.mm(pp[q][:], wdt[s][:, fc, dc * 128:(dc + 1) * 128], hw[hs][:, fc, :], fc == 0, fc == HF // 128 - 1,
                                 reads=[b_we[s], b_hw[hs]], writes=[b_pp[q]])
                        if first:
                            p.copy("act", acc[tc][:, dc, :], pp[q][:], reads=[b_pp[q]], writes=[b_acc[tc]])
                        else:
                            p.tt("dve", acc[tc][:, dc, :], acc[tc][:, dc, :], pp[q][:], ALU.add, reads=[b_pp[q], b_acc[tc]], writes=[b_acc[tc]])
                unit += 1
        for tc in range(CH // 512):
            gsl = slice(c * CH + tc * 512, c * CH + (tc + 1) * 512)
            for k4 in range(4):
                p.dma("sp", yv[:, k4 * 4:(k4 + 1) * 4, gsl], acc[tc][:, k4 * 4:(k4 + 1) * 4, :], reads=[b_acc[tc]], is_output=True)
    return p.finish()


def build_postB(layer):
    last = (layer == 1)
    p = Prog()
    x1T = p.dram_in("x1T", [D, TL]); yP = p.dram_in("yP", [8, D, TL])
    vecs = p.dram_in("vecs", [128, 4, KC])
    xt = p.sb("xt", [128, KC, TL], F32); b_x = p.buf()
    vt = p.sb("vt", [128, 4, KC], F32); b_v = p.buf()
    coef = p.sb("coef", [128, KC], F32)
    scratch = alloc_rms_scratch(p)
    KG = 2
    ysum = p.sb("ysum", [128, KG, TL], F32); b_ys = p.buf()
    yt = [p.sb("yt%d" % i, [128, KG, TL], F32) for i in range(2)]; b_yt = [p.buf(), p.buf()]
    xv = x1T.rearrange("(k p) t -> p k t", p=128)
    for k4 in range(4):
        p.dma("sp", xt[:, k4 * 4:(k4 + 1) * 4, :], xv[:, k4 * 4:(k4 + 1) * 4, :], writes=[b_x])
    p.dma("sp", vt[:], vecs, writes=[b_v])
    p.ts("dve", coef[:], vt[:, 2, :], 1.0, None, ALU.add, None, reads=[b_v], writes=[b_v])
    p.tt("dve", coef[:], coef[:], vt[:, 1, :], ALU.mult, reads=[b_v], writes=[b_v])
    it = 0
    for k4 in range(KC // KG):
        ks = slice(k4 * KG, (k4 + 1) * KG)
        for g in range(8):
            s = it % 2; it += 1
            yv = yP[g].rearrange("(k p) t -> p k t", p=128)
            if g == 0:
                p.dma("sp", ysum[:], yv[:, ks, :], writes=[b_ys])
            else:
                p.dma("sp" if s == 0 else "act", yt[s][:], yv[:, ks, :], writes=[b_yt[s]])
                p.tt("dve" if g % 2 else "pool", ysum[:], ysum[:], yt[s][:], ALU.add, reads=[b_yt[s], b_ys], writes=[b_ys])
        for kk in range(KG):
            k = k4 * KG + kk
            p.op("dve", lambda t, k=k, kk=kk: t.scalar_tensor_tensor(out=xt[:, k, :], in0=ysum[:, kk, :], scalar=vt[:, 0, k:k + 1], in1=xt[:, k, :],
                                                                    op0=ALU.mult, op1=ALU.add), reads=[b_ys, b_v, b_x], writes=[b_x])
    if last:
        out = p.dram_out("outT", [D, TL])
        fo = p.sb("fo", [128, KC, TL], F32); b_fo = p.buf()
        emit_rms_plain(p, xt, b_x, vt[:, 1, :], b_v, fo, b_fo, TL, scratch)
        ov = out.rearrange("(k p) t -> p k t", p=128)
        for k4 in range(4):
            p.dma("sp", ov[:, k4 * 4:(k4 + 1) * 4, :], fo[:, k4 * 4:(k4 + 1) * 4, :], reads=[b_fo], is_output=True)
    else:
        x2T = p.dram_out("x2T", [D, TL]); w_next = p.dram_in("w_next", [D, ODD_COLS]); projN = p.dram_out("projN", [ODD_COLS, TL])
        hbf = p.sb("hbf", [128, KC, TL], BF16); b_h = p.buf()
        ov = x2T.rearrange("(k p) t -> p k t", p=128)
        for k4 in range(4):
            p.dma("sp", ov[:, k4 * 4:(k4 + 1) * 4, :], xt[:, k4 * 4:(k4 + 1) * 4, :], reads=[b_x], is_output=True)
        emit_rms_mod(p, xt, b_x, coef, vt[:, 3, :], b_v, hbf, b_h, TL, scratch)
        emit_proj(p, hbf, b_h, w_next, ODD_COLS, TL, projN, stage=(scratch[2], scratch[6]))
    return p.finish()


def stage_post(inp, mod, layer, xT_full, mixT_full, gateT_full=None, dbg=None):
    ident = np.eye(128, dtype=np.float32)
    m = mod[layer]
    g1, sh2, sc2, g2 = m[2 * D:3 * D], m[3 * D:4 * D], m[4 * D:5 * D], m[5 * D:6 * D]
    z = np.zeros(D, np.float32)
    w_out = inp["ev_w_out"][0] if layer == 0 else inp["od_w_out"][0]
    vecsA = np.ascontiguousarray(np.stack([fm(v) for v in (g1, inp["norm_ffn_g"][layer], sc2, sh2)], axis=1))
    wr = np.ascontiguousarray(np.concatenate([inp["moe_w_grp"][layer], inp["moe_w_exp"][layer]], axis=1))
    br = np.ascontiguousarray(np.tile(np.concatenate([inp["moe_b_grp"][layer], inp["moe_b_exp"][layer]])[None, :], (128, 1)))
    maps = []
    for c in range(NCORE):
        sl = slice(c * TL, (c + 1) * TL)
        mp = {"xT": np.ascontiguousarray(xT_full[:, sl]), "mixT": np.ascontiguousarray(mixT_full[:, sl]), "w_out": w_out, "vecs": vecsA,
              "wr": wr, "br": br, "ident": ident}
        if layer == 1:
            mp["gateT"] = np.ascontiguousarray(gateT_full[:, sl])
        maps.append(mp)
    res = run_spmd(build_postA(layer), maps)
    x1T = np.concatenate([r["x1T"] for r in res], axis=1)
    hfT = np.concatenate([r["hfT"] for r in res], axis=1)
    gwT = np.concatenate([r["gwT"] for r in res], axis=1)
    if dbg is not None:
        dbg["x1T"], dbg["hfT"], dbg["gwT"] = x1T, hfT, gwT
    maps = []
    for g in range(NCORE):
        es = slice(g * 8, (g + 1) * 8)
        maps.append({"hfT": hfT, "gw": np.ascontiguousarray(gwT[es]), "wg": inp["moe_w_gate"][layer][es],
                     "wu": inp["moe_w_up"][layer][es], "wd": inp["moe_w_down"][layer][es]})
    res = run_spmd(build_moe(), maps)
    yP = np.stack([r["yT"] for r in res], axis=0)
    if dbg is not None:
        dbg["yP"] = yP
    if layer == 0:
        mn = mod[1]
        vecsB = np.ascontiguousarray(np.stack([fm(v) for v in (g2, inp["norm_mix_g"][1], mn[D:2 * D], mn[0:D])], axis=1))
    else:
        vecsB = np.ascontiguousarray(np.stack([fm(v) for v in (g2, inp["final_g"], z, z)], axis=1))
    maps = []
    for c in range(NCORE):
        sl = slice(c * TL, (c + 1) * TL)
        mp = {"x1T": np.ascontiguousarray(x1T[:, sl]), "yP": np.ascontiguousarray(yP[:, :, sl]), "vecs": vecsB}
        if layer == 0:
            mp["w_next"] = inp["od_w_in"][0]
        maps.append(mp)
    res = run_spmd(build_postB(layer), maps)
    if layer == 0:
        return (np.concatenate([r["x2T"] for r in res], axis=1), np.concatenate([r["projN"] for r in res], axis=1))
    return np.concatenate([r["outT"] for r in res], axis=1)

HC = 32
SEG = 1024


def build_L4(nseg=T // SEG):
    p = Prog()
    qT = p.dram_in("qT", [2, 128, T]); fT = p.dram_in("fT", [2, 128, T])
    vtok_d = p.dram_in("vtok", [2, HC, T // HC, 128])
    lbl_d = p.dram_in("lbl", [128, 2, 2])
    gn_d = p.dram_in("gn", [128, 2])
    rflag_d = p.dram_in("rflag", [128, SEG]); cmask_d = p.dram_in("cmask", [HC, HC]); ident_d = p.dram_in("ident", [128, 128])
    oT = p.dram_out("oT", [2, 128, T])
    NCH = SEG // HC
    ident = p.sb("ident", [128, 128], F32); rflag = p.sb("rflag", [128, SEG], F32); cmask = p.sb("cmask", [HC, HC], F32)
    onesf = p.sb("onesf", [128, 128], F32); epst = p.sb("epst", [128, 1], F32)
    lbl = p.sb("lbl", [128, 2, 2], F32); lb = p.sb("lb", [128, 2], F32); oml = p.sb("oml", [128, 2], F32); gn = p.sb("gn", [128, 2], F32)
    b_c = p.buf()
    p.dma("sp", ident[:], ident_d, writes=[b_c]); p.dma("sp", rflag[:], rflag_d, writes=[b_c]); p.dma("sp", cmask[:], cmask_d, writes=[b_c])
    p.dma("sp", lbl[:], lbl_d, writes=[b_c]); p.dma("sp", gn[:], gn_d, writes=[b_c])
    p.memset("dve", onesf[:], 1.0, [b_c]); p.memset("dve", epst[:], EPS, [b_c])
    p.tt("dve", lb[:], lbl[:, :, 1], lbl[:, :, 0], ALU.subtract, reads=[b_c], writes=[b_c])
    p.act(lb[:], lb[:], AF.Sigmoid, reads=[b_c], writes=[b_c])
    p.ts("dve", oml[:], lb[:], -1.0, 1.0, ALU.mult, ALU.add, reads=[b_c], writes=[b_c])
    psT = p.ps("psT", [128, 512]); b_psT = p.buf()
    psN = p.ps("psN", [128, 512]); b_psN = p.buf()
    H = []
    for hd in range(2):
        n = "h%d_" % hd
        d = dict(
            A=p.sb(n + "A", [128, SEG], F32), B=p.sb(n + "B", [128, SEG], F32), C=p.sb(n + "C", [128, SEG], F32),
            Bt=p.sb(n + "Bt", [128, SEG], F32), E=p.sb(n + "E", [128, SEG], F32), KA=p.sb(n + "KA", [128, SEG], F32),
            KD=p.sb(n + "KD", [128, SEG], F32), OS=p.sb(n + "OS", [128, SEG], F32),
            qe=p.sb(n + "qe", [128, SEG], BF16), ka=p.sb(n + "ka", [128, SEG], BF16),
            vt=p.sb(n + "vt", [HC, NCH, 128], BF16), kt=p.sb(n + "kt", [HC, NCH, 128], BF16),
            ebl=p.sb(n + "ebl", [128, NCH], F32), S32=p.sb(n + "S32", [128, 128], F32), Sbf=p.sb(n + "Sbf", [128, 128], BF16),
            attm=p.sb(n + "attm", [HC, HC], BF16), rs=p.sb(n + "rs", [128, 512], F32), oo=p.sb(n + "oo", [128, 512], F32),
            psA=p.ps(n + "psA", [128, 512]), psO=p.ps(n + "psO", [128, 512]), psS=p.ps(n + "psS", [128, 512]),
        )
        for k in ("A", "B", "C", "Bt", "E", "KA", "KD", "OS", "qe", "ka", "vt", "kt", "ebl", "S", "attm", "rs", "oo", "psA", "psO", "psS"):
            d["b_" + k] = p.buf()
        p.memset("dve", d["S32"][:], 0.0, [d["b_S"]])
        p.memset("dve", d["Sbf"][:], 0.0, [d["b_S"]])
        H.append(d)
    for sgi in range(nseg):
        tsl = slice(sgi * SEG, (sgi + 1) * SEG)
        for hd in range(2):
            d = H[hd]
            p.dma("sp", d["A"][:], qT[hd, :, tsl], writes=[d["b_A"]])
            p.dma("act", d["B"][:], fT[hd, :, tsl], writes=[d["b_B"]])
            p.dma("pool", d["vt"][:], vtok_d[hd, :, sgi * NCH:(sgi + 1) * NCH, :], writes=[d["b_vt"]])
            p.act(d["B"][:], d["B"][:], AF.Sigmoid, reads=[d["b_B"]], writes=[d["b_B"]])
            p.ts("dve", d["B"][:], d["B"][:], oml[:, hd:hd + 1], lb[:, hd:hd + 1], ALU.mult, ALU.add, reads=[d["b_B"], b_c], writes=[d["b_B"]])
            p.ts("dve", d["B"][:], d["B"][:], 1e-30, None, ALU.max, None, reads=[d["b_B"]], writes=[d["b_B"]])
            p.act(d["C"][:], d["B"][:], AF.Ln, reads=[d["b_B"]], writes=[d["b_C"]])
            p.op("dve", lambda t, d=d: t.tensor_tensor_scan(out=d["Bt"][:], data0=rflag[:], data1=d["C"][:], initial=0.0, op0=ALU.mult, op1=ALU.add),
                 reads=[d["b_C"], b_c], writes=[d["b_Bt"]])
            p.act(d["E"][:], d["Bt"][:], AF.Exp, reads=[d["b_Bt"]], writes=[d["b_E"]])
            p.act(d["A"][:], d["A"][:], AF.Silu, reads=[d["b_A"]], writes=[d["b_A"]])
            p.tt("dve", d["qe"][:], d["A"][:], d["E"][:], ALU.mult, reads=[d["b_A"], d["b_E"]], writes=[d["b_qe"]])
            p.act(d["E"][:], d["Bt"][:], AF.Exp, reads=[d["b_Bt"]], writes=[d["b_E"]], scale=-1.0)
            p.ts("dve", d["B"][:], d["B"][:], -1.0, 1.0, ALU.mult, ALU.add, reads=[d["b_B"]], writes=[d["b_B"]])
            p.tt("dve", d["KA"][:], d["B"][:], d["E"][:], ALU.mult, reads=[d["b_B"], d["b_E"]], writes=[d["b_KA"]])
            p.copy("pool", d["ka"][:], d["KA"][:], reads=[d["b_KA"]], writes=[d["b_ka"]])
            p.act(d["ebl"][:], d["Bt"][:].rearrange("p (c t) -> p c t", t=HC)[:, :, HC - 1], AF.Exp, reads=[d["b_Bt"]], writes=[d["b_ebl"]])
            for c in range(NCH):
                cs = slice(c * HC, (c + 1) * HC)
                p.ts("dve" if c % 2 == 0 else "pool", d["KD"][:, cs], d["KA"][:, cs], d["ebl"][:, c:c + 1], None, ALU.mult, None,
                     reads=[d["b_KA"], d["b_ebl"]], writes=[d["b_KD"]])
            for c in range(NCH):
                cs = slice(c * HC, (c + 1) * HC)
                p.transpose(psT[:HC, :128], d["KD"][:, cs], ident[:], reads=[d["b_KD"], b_c], writes=[b_psT])
                p.copy("act" if c % 2 == 0 else "dve", d["kt"][:, c, :], psT[:HC, :128], reads=[b_psT], writes=[d["b_kt"]])
        for c in range(NCH):
            cs = slice(c * HC, (c + 1) * HC)
            for hd in range(2):
                d = H[hd]
                p.mm(d["psA"][:HC, :HC], d["ka"][:, cs], d["qe"][:, cs], True, True, reads=[d["b_ka"], d["b_qe"]], writes=[d["b_psA"]])
                p.tt("dve", d["attm"][:], d["psA"][:HC, :HC], cmask[:], ALU.mult, reads=[d["b_psA"], b_c], writes=[d["b_attm"]])
                p.mm(d["psO"][:, :HC], d["Sbf"][:], d["qe"][:, cs], True, False, reads=[d["b_S"], d["b_qe"]], writes=[d["b_psO"]])
                p.mm(d["psO"][:, :HC], d["vt"][:, c, :], d["attm"][:], False, True, reads=[d["b_vt"], d["b_attm"]], writes=[d["b_psO"]])
                p.copy("act", d["OS"][:, cs], d["psO"][:, :HC], reads=[d["b_psO"]], writes=[d["b_OS"]])
                p.mm(d["psS"][:, :128], d["kt"][:, c, :], d["vt"][:, c, :], True, True, reads=[d["b_kt"], d["b_vt"]], writes=[d["b_psS"]])
                p.op("dve", lambda t, d=d, c=c: t.scalar_tensor_tensor(out=d["S32"][:], in0=d["S32"][:], scalar=d["ebl"][:, c:c + 1],
                                                                      in1=d["psS"][:, :128], op0=ALU.mult, op1=ALU.add),
                     reads=[d["b_psS"], d["b_ebl"], d["b_S"]], writes=[d["b_S"]])
                p.copy("pool", d["Sbf"][:], d["S32"][:], reads=[d["b_S"]], writes=[d["b_S"]])
        for hd in range(2):
            d = H[hd]
            p.act(d["C"][:], d["OS"][:], AF.Square, reads=[d["b_OS"]], writes=[d["b_C"]])
            for blk in range(SEG // 512):
                bs = slice(blk * 512, (blk + 1) * 512)
                p.mm(psN[:], onesf[:], d["C"][:, bs], True, True, reads=[b_c, d["b_C"]], writes=[b_psN])
                p.act(d["rs"][:], psN[:], AF.Sqrt, reads=[b_psN, b_c], writes=[d["b_rs"]], scale=1.0 / 128, bias=epst[:])
                p.op("dve", lambda t, d=d: t.reciprocal(out=d["rs"][:], in_=d["rs"][:]), reads=[d["b_rs"]], writes=[d["b_rs"]])
                p.op("dve", lambda t, d=d, bs=bs, hd=hd: t.scalar_tensor_tensor(out=d["oo"][:], in0=d["OS"][:, bs], scalar=gn[:, hd:hd + 1],
                                                                               in1=d["rs"][:], op0=ALU.mult, op1=ALU.mult),
                     reads=[d["b_OS"], d["b_rs"], b_c], writes=[d["b_oo"]])
                p.dma("sp", oT[hd, :, sgi * SEG + blk * 512:sgi * SEG + (blk + 1) * 512], d["oo"][:], reads=[d["b_oo"]], is_output=True)
    return p.finish()


def stage_L4(inp, projN, nseg=T // SEG):
    nc = build_L4(nseg)
    rflag = np.ones((128, SEG), np.float32); rflag[:, ::HC] = 0.0
    s = np.arange(HC)
    cmask = (s[:, None] <= s[None, :]).astype(np.float32)
    ident = np.eye(128, dtype=np.float32)
    maps = []
    for c in range(NCORE):
        hs = [2 * c, 2 * c + 1]
        qT = np.ascontiguousarray(np.stack([projN[h * 128:(h + 1) * 128] for h in hs]))
        fT = np.ascontiguousarray(np.stack([projN[2048 + h * 128:2048 + (h + 1) * 128] for h in hs]))
        vt = np.stack([projN[4096 + h * 128:4096 + (h + 1) * 128].T.reshape(T // HC, HC, 128).transpose(1, 0, 2) for h in hs])
        lbl = np.stack([inp["hg_lb_logits"][:, h * 128:(h + 1) * 128].T for h in hs], axis=1)
        gn = np.stack([inp["hg_norm_g"][0][h * 128:(h + 1) * 128] for h in hs], axis=1)
        maps.append({"qT": qT, "fT": fT, "vtok": np.ascontiguousarray(vt), "lbl": np.ascontiguousarray(lbl.astype(np.float32)),
                     "gn": np.ascontiguousarray(gn.astype(np.float32)), "rflag": rflag, "cmask": cmask, "ident": ident})
    res = run_spmd(nc, maps)
    return np.concatenate([r["oT"].reshape(256, T) for r in res], axis=0)


def kernel(**inp):
    import os, time
    dbgdir = os.environ.get("MK_DEBUG_DIR")
    t0 = time.time()

    def dump(name, a):
        print("[mk] %s done at %.0fs" % (name, time.time() - t0), flush=True)
        if dbgdir:
            np.save(os.path.join(dbgdir, "kd_%s.npy" % name), a)
    inp = {k: np.asarray(v) for k, v in inp.items()}
    mod = stage_L0(inp); dump("mod", mod)
    projT = stage_L1(inp, mod); dump("projT", projT)
    nsaT = stage_L2(inp, projT); dump("nsaT", nsaT)
    lruT = stage_L2b(inp, projT); dump("lruT", lruT)
    xT0 = np.ascontiguousarray(inp["x"][0].T)
    mixT = np.concatenate([nsaT, lruT], axis=0)
    dbg = {} if dbgdir else None
    x2T, projN = stage_post(inp, mod, 0, xT0, mixT, dbg=dbg); dump("x2T", x2T); dump("projN", projN)
    if dbg:
        for k, v in dbg.items():
            dump("L0_" + k, v)
    oT = stage_L4(inp, projN); dump("oT", oT)
    dbg = {} if dbgdir else None
    outT = stage_post(inp, mod, 1, x2T, oT, gateT_full=projN[6144:8192], dbg=dbg); dump("outT", outT)
    if dbg:
        for k, v in dbg.items():
            if k != "yP":
                dump("L1_" + k, v)
    return np.ascontiguousarray(outT.T)[None].astype(np.float32)
```

```python
import math
import numpy as np
import concourse.bass as bass
import concourse.mybir as mybir

F32 = mybir.dt.float32
BF16 = mybir.dt.bfloat16
I32 = mybir.dt.int32
U32 = mybir.dt.uint32
AF = mybir.ActivationFunctionType
ALU = mybir.AluOpType
AX = mybir.AxisListType

ENGS = ("pe", "act", "dve", "pool", "sp")


class Buf:
    __slots__ = ("name", "w", "r")

    def __init__(self, name):
        self.name = name
        self.w = None
        self.r = []


class Prog:
    NDMA = 24

    def __init__(self):
        nc = bass.Bass("TRN2", target_bir_lowering=False)
        self.nc = nc
        self.eng = {"pe": nc.tensor, "act": nc.scalar, "dve": nc.vector, "pool": nc.gpsimd, "sp": nc.sync}
        self.ops = {e: [] for e in ENGS}
        self.sem = {e: nc.alloc_semaphore("c_" + e) for e in ENGS}
        self.cnt = {e: 0 for e in ENGS}
        self.seen = {e: {} for e in ENGS}
        self.dsem = [nc.alloc_semaphore("d%d" % i) for i in range(self.NDMA)]
        self.dval = [0] * self.NDMA
        self.dnext = 0
        self.nbuf = 0
        self.out_tokens = []

    def sb(self, name, shape, dt):
        return self.nc.alloc_sbuf_tensor("s_" + name, list(shape), dt)

    def ps(self, name, shape, dt=F32):
        return self.nc.alloc_psum_tensor("p_" + name, list(shape), dt)

    def buf(self, name=None):
        self.nbuf += 1
        return Buf(name or "b%d" % self.nbuf)

    def dram_in(self, name, shape, dt=F32):
        return self.nc.dram_tensor(name, list(shape), dt, kind="ExternalInput").ap()

    def dram_out(self, name, shape, dt=F32):
        return self.nc.dram_tensor(name, list(shape), dt, kind="ExternalOutput").ap()

    def dram_tmp(self, name, shape, dt=F32):
        return self.nc.dram_tensor(name, list(shape), dt, kind="Internal").ap()

    def _need(self, e, toks):
        waits = {}
        for t in toks:
            if t is None:
                continue
            key, sem, val = t
            if e == "pe" and key == "c_pe":
                continue
            if self.seen[e].get(key, 0) >= val:
                continue
            if key not in waits or waits[key][1] < val:
                waits[key] = (sem, val)
        for key, (sem, val) in waits.items():
            self.seen[e][key] = val
        return list(waits.values())

    def _deps(self, reads, writes):
        toks = []
        for b in reads:
            toks.append(b.w)
        for b in writes:
            toks.append(b.w)
            toks.extend(b.r)
        return toks

    def _commit(self, tok, reads, writes):
        for b in reads:
            b.r.append(tok)
        for b in writes:
            b.w = tok
            b.r = []

    def op(self, e, fn, reads=(), writes=()):
        waits = self._need(e, self._deps(reads, writes))
        self.cnt[e] += 1
        val = self.cnt[e]
        sem = self.sem[e]
        tok = ("c_" + e, sem, val)

        def emit(eng, waits=waits, fn=fn, sem=sem):
            for s, v in waits:
                eng.wait_ge(s, v)
            fn(eng).then_inc(sem, 1)
        self.ops[e].append(emit)
        self._commit(tok, reads, writes)
        return tok

    def dma(self, q, out, in_, reads=(), writes=(), is_output=False, **kw):
        k = self.dnext
        self.dnext = (self.dnext + 1) % self.NDMA
        dsem = self.dsem[k]
        prev = self.dval[k]
        self.dval[k] = prev + 16
        key = "d%d" % k
        toks = self._deps(reads, writes)
        if prev > 0:
            toks.append((key, dsem, prev))
        waits = self._need(q, toks)
        tok = (key, dsem, prev + 16)

        def emit(eng, waits=waits, out=out, in_=in_, dsem=dsem, kw=kw):
            for s, v in waits:
                eng.wait_ge(s, v)
            eng.dma_start(out=out, in_=in_, **kw).then_inc(dsem, 16)
        self.ops[q].append(emit)
        self._commit(tok, reads, writes)
        if is_output:
            self.out_tokens.append(tok)
        return tok

    def raw(self, e, fn):
        self.ops[e].append(fn)

    def mm(self, out, lhsT, rhs, start, stop, reads, writes, **kw):
        return self.op("pe", lambda t: t.matmul(out, lhsT, rhs, start=start, stop=stop, **kw), reads, writes)

    def transpose(self, out, in_, ident, reads, writes):
        return self.op("pe", lambda t: t.transpose(out, in_, ident), reads, writes)

    def act(self, out, in_, func, reads, writes, e="act", **kw):
        return self.op(e, lambda t: t.activation(out=out, in_=in_, func=func, **kw), reads, writes)

    def tt(self, e, out, in0, in1, op, reads, writes):
        return self.op(e, lambda t: t.tensor_tensor(out=out, in0=in0, in1=in1, op=op), reads, writes)

    def ts(self, e, out, in0, s1, s2, op0, op1, reads, writes, **kw):
        if op1 is None:
            return self.op(e, lambda t: t.tensor_scalar(out=out, in0=in0, scalar1=s1, scalar2=None, op0=op0, **kw), reads, writes)
        return self.op(e, lambda t: t.tensor_scalar(out=out, in0=in0, scalar1=s1, scalar2=s2, op0=op0, op1=op1, **kw), reads, writes)

    def copy(self, e, out, in_, reads, writes):
        if e == "act":
            return self.op(e, lambda t: t.copy(out=out, in_=in_), reads, writes)
        return self.op(e, lambda t: t.tensor_copy(out=out, in_=in_), reads, writes)

    def memset(self, e, out, val, writes):
        return self.op(e, lambda t: t.memset(out, val), (), writes)

    def finish(self):
        nc = self.nc
        waits = self._need("sp", self.out_tokens)

        def fin(eng, waits=waits):
            for s, v in waits:
                eng.wait_ge(s, v)
        self.ops["sp"].append(fin)
        ops = self.ops
        with nc.Block() as block:
            @block.tensor
            def _(t):
                for f in ops["pe"]:
                    f(t)

            @block.scalar
            def _(t):
                for f in ops["act"]:
                    f(t)

            @block.vector
            def _(t):
                for f in ops["dve"]:
                    f(t)

            @block.gpsimd
            def _(t):
                for f in ops["pool"]:
                    f(t)

            @block.sync
            def _(t):
                for f in ops["sp"]:
                    f(t)
        return nc

from concourse.bass_utils import run_bass_kernel_spmd
D = 2048
T = 8192
NCORE = 8
TL = T // NCORE
KC = D // 128
EPS = 1e-6
EVEN_COLS = 4632


def fm(v):
    v = np.asarray(v)
    return np.ascontiguousarray(v.reshape(-1, 128).T)


def run_spmd(nc, in_maps):
    res = run_bass_kernel_spmd(nc, in_maps, core_ids=list(range(NCORE)))
    return res.results


def build_L0():
    p = Prog()
    NCOL = 1536
    cT = p.dram_in("cT", [128, KC])
    w = p.dram_in("w", [2, D, NCOL])
    b = p.dram_in("b", [1, 2 * NCOL])
    out = p.dram_out("mod", [1, 2 * NCOL])
    ct = p.sb("ct", [128, KC], F32)
    cact = p.sb("cact", [128, KC], F32)
    bt = p.sb("bt", [1, 2 * NCOL], F32)
    ot = p.sb("ot", [1, 2 * NCOL], F32)
    wt = [p.sb("wt%d" % i, [128, KC, 512], F32) for i in range(2)]
    ps = [p.ps("ps%d" % i, [1, 512]) for i in range(2)]
    b_c, b_b, b_o = p.buf(), p.buf(), p.buf()
    b_w = [p.buf(), p.buf()]
    b_ps = [p.buf(), p.buf()]
    p.dma("sp", ct[:], cT, writes=[b_c])
    p.dma("sp", bt[:], b, writes=[b_b])
    p.act(cact[:], ct[:], AF.Silu, reads=[b_c], writes=[b_c])
    i = 0
    for l in range(2):
        for n in range(3):
            s = i % 2
            p.dma("sp" if s == 0 else "act", wt[s][:], w[l, :, n * 512:(n + 1) * 512].rearrange("(k p) n -> p k n", p=128),
                  writes=[b_w[s]])
            for k in range(KC):
                p.mm(ps[s][:], cact[:, k:k + 1], wt[s][:, k, :], k == 0, k == KC - 1, reads=[b_c, b_w[s]], writes=[b_ps[s]])
            c0 = l * NCOL + n * 512
            p.tt("dve", ot[:, c0:c0 + 512], ps[s][:], bt[:, c0:c0 + 512], ALU.add, reads=[b_ps[s], b_b], writes=[b_o])
            i += 1
    p.dma("sp", out, ot[:], reads=[b_o], is_output=True)
    return p.finish()


def stage_L0(inp):
    nc = build_L0()
    cT = fm(inp["c"][0])
    maps = []
    for c in range(NCORE):
        sl = slice(c * 1536, (c + 1) * 1536)
        maps.append({"cT": cT,
                     "w": np.ascontiguousarray(inp["ada_w"][:, :, sl]),
                     "b": np.ascontiguousarray(inp["ada_b"][:, sl]).reshape(1, -1)})
    res = run_spmd(nc, maps)
    mod = np.concatenate([r["mod"].reshape(2, 1536) for r in res], axis=1)
    return mod


def emit_rms_mod(p, xt, b_x, coef, shift, b_cs, hbf, b_h, ntok, scratch):
    onesf, epst, sq, tmp, rs, ps_ss, b_sq, b_tmp, b_rs, b_ss = scratch
    for tc in range(ntok // 512):
        tsl = slice(tc * 512, (tc + 1) * 512)
        p.act(sq[:], xt[:, :, tsl], AF.Square, reads=[b_x], writes=[b_sq])
        for k in range(KC):
            p.mm(ps_ss[:], onesf[:], sq[:, k, :], k == 0, k == KC - 1, reads=[b_sq], writes=[b_ss])
        p.act(rs[:], ps_ss[:], AF.Sqrt, reads=[b_ss], writes=[b_rs], scale=1.0 / D, bias=epst[:])
        p.op("dve", lambda t: t.reciprocal(out=rs[:], in_=rs[:]), reads=[b_rs], writes=[b_rs])
        for k in range(KC):
            j = k % 2
            p.op("dve", lambda t, k=k, j=j, tsl=tsl: t.scalar_tensor_tensor(out=tmp[j][:], in0=xt[:, k, tsl], scalar=coef[:, k:k + 1],
                                                                  in1=rs[:], op0=ALU.mult, op1=ALU.mult),
                 reads=[b_x, b_cs, b_rs], writes=[b_tmp[j]])
            p.act(hbf[:, k, tsl], tmp[j][:], AF.Identity, reads=[b_tmp[j], b_cs], writes=[b_h], bias=shift[:, k:k + 1])


def alloc_rms_scratch(p):
    onesf = p.sb("onesf", [128, 128], F32)
    epst = p.sb("epst", [128, 1], F32)
    sq = p.sb("sq", [128, KC, 512], F32)
    tmp = [p.sb("rtmp%d" % i, [128, 512], F32) for i in range(2)]
    rs = p.sb("rs", [128, 512], F32)
    ps_ss = p.ps("ps_ss", [128, 512])
    b_c = p.buf()
    p.memset("dve", onesf[:], 1.0, [b_c])
    p.memset("dve", epst[:], EPS, [b_c])
    return (onesf, epst, sq, tmp, rs, ps_ss, p.buf(), [p.buf(), p.buf()], p.buf(), p.buf())


def emit_proj(p, hbf, b_h, w_dram, ncols, ntok, out_dram, wq="pool", evac=None, wbufs=None, stage=None):
    if wbufs is None:
        wt = [p.sb("pw%d" % i, [128, KC, 512], BF16) for i in range(2)]
        b_w = [p.buf(), p.buf()]
        pp = [p.ps("pp%d" % i, [128, 512]) for i in range(2)]
        b_pp = [p.buf(), p.buf()]
        osb = [p.sb("po%d" % i, [128, 512], F32) for i in range(3)]
        b_o = [p.buf() for _ in range(3)]
    else:
        wt, b_w, pp, b_pp, osb, b_o = wbufs
    ntile = (ncols + 511) // 512
    it = 0
    for j in range(ntile):
        c0 = j * 512
        cw = min(512, ncols - c0)
        s = j % 2
        if stage is None:
            p.dma(wq, wt[s][:, :, :cw], w_dram[:, c0:c0 + cw].rearrange("(k p) n -> p k n", p=128), writes=[b_w[s]])
        else:
            st, b_st = stage
            p.dma("sp" if j % 2 == 0 else "act", st[:, :, :cw], w_dram[:, c0:c0 + cw].rearrange("(k p) n -> p k n", p=128), writes=[b_st])
            p.copy("pool", wt[s][:, :, :cw], st[:, :, :cw], reads=[b_st], writes=[b_w[s]])
        for m in range((cw + 127) // 128):
            mw = min(128, cw - m * 128)
            for tc in range(ntok // 512):
                tsl = slice(tc * 512, (tc + 1) * 512)
                q = it % 2
                o = it % 3
                for k in range(KC):
                    p.mm(pp[q][:mw, :], wt[s][:, k, m * 128:m * 128 + mw], hbf[:, k, tsl], k == 0, k == KC - 1,
                         reads=[b_w[s], b_h], writes=[b_pp[q]])
                if evac is None:
                    if it % 2 == 0:
                        p.act(osb[o][:mw, :], pp[q][:mw, :], AF.Copy, reads=[b_pp[q]], writes=[b_o[o]])
                    else:
                        p.copy("dve", osb[o][:mw, :], pp[q][:mw, :], reads=[b_pp[q]], writes=[b_o[o]])
                    p.dma("sp", out_dram[c0 + m * 128:c0 + m * 128 + mw, tsl], osb[o][:mw, :], reads=[b_o[o]], is_output=True)
                else:
                    evac(c0 + m * 128, mw, tc, pp[q], b_pp[q])
                it += 1


def build_L1():
    p = Prog()
    xT = p.dram_in("xT", [D, TL])
    vecs = p.dram_in("vecs", [128, 3, KC])
    w = p.dram_in("w", [D, EVEN_COLS])
    out = p.dram_out("projT", [EVEN_COLS, TL])
    xt = p.sb("xt", [128, KC, TL], F32)
    vt = p.sb("vt", [128, 3, KC], F32)
    coef = p.sb("coef", [128, KC], F32)
    hbf = p.sb("hbf", [128, KC, TL], BF16)
    b_x, b_v, b_h = p.buf(), p.buf(), p.buf()
    scratch = alloc_rms_scratch(p)
    xv = xT.rearrange("(k p) t -> p k t", p=128)
    for k4 in range(4):
        p.dma("sp" if k4 % 2 == 0 else "act", xt[:, k4 * 4:(k4 + 1) * 4, :], xv[:, k4 * 4:(k4 + 1) * 4, :], writes=[b_x])
    p.dma("sp", vt[:], vecs, writes=[b_v])
    p.ts("dve", coef[:], vt[:, 1, :], 1.0, None, ALU.add, None, reads=[b_v], writes=[b_v])
    p.tt("dve", coef[:], coef[:], vt[:, 0, :], ALU.mult, reads=[b_v], writes=[b_v])
    emit_rms_mod(p, xt, b_x, coef, vt[:, 2, :], b_v, hbf, b_h, TL, scratch)
    emit_proj(p, hbf, b_h, w, EVEN_COLS, TL, out, stage=(scratch[2], scratch[6]))
    return p.finish()


def stage_L1(inp, mod):
    nc = build_L1()
    x = inp["x"][0]
    sh1, sc1 = mod[0, 0:D], mod[0, D:2 * D]
    vecs = np.ascontiguousarray(np.stack([fm(inp["norm_mix_g"][0]), fm(sc1), fm(sh1)], axis=1))
    maps = []
    for c in range(NCORE):
        maps.append({"xT": np.ascontiguousarray(x[c * TL:(c + 1) * TL].T), "vecs": vecs, "w": inp["ev_w_in"][0]})
    res = run_spmd(nc, maps)
    return np.concatenate([r["projT"] for r in res], axis=1)

HD = 128
QS = HD ** -0.5
NEGM = 30000.0 / QS
FX = 8192
FOFF = 4096
CMP_TILES = [0, 1, 2, 3, 4]
SLC_TILES = [-3, -2, -1, 0, 1]
WIN_TILES = [1, 2, 3, 4]
TINY = 1e-30


def t5_bucket_np(rel):
    n = np.maximum(rel, 0)
    nf = np.maximum(n, 1).astype(np.float32)
    large = 16 + (np.log(nf / 16) / math.log(128 / 16) * 16).astype(np.int32)
    large = np.minimum(large, 31)
    return np.where(n < 16, n, large)


def nsa_consts():
    rel = np.arange(FX) - FOFF
    bk = t5_bucket_np(rel)
    oh = np.zeros((34, FX), np.float32)
    valid = rel >= 0
    oh[bk[valid], np.nonzero(valid)[0]] += 1.0
    oh[31, valid] -= 1.0
    oh[32, ~valid] = 1.0
    oh[33, rel >= 512] = 1.0
    n = np.arange(512)
    j = np.arange(128)
    cover = ((16 * n[:, None] < 64 * j[None, :] + 64) & (16 * n[:, None] + 32 > 64 * j[None, :])).astype(np.float32)
    cover[511, :] = 0.0
    cover1 = np.concatenate([cover, np.ones((512, 1), np.float32)], axis=1)
    cover1 = np.ascontiguousarray(cover1.reshape(4, 128, 129).transpose(1, 0, 2))
    ebig = (np.arange(128)[:, None] == (np.arange(8192)[None, :] // 64)).astype(np.float32)
    ql = np.arange(128)[:, None]
    u = np.arange(256)[None, :] - 128
    fext = np.where(u < ql // 64, 0.0, np.where(u == ql // 64, 1e4, -1e4)).astype(np.float32)
    ident = np.eye(128, dtype=np.float32)
    return oh, cover1, ebig, fext, ident


def build_L2(nqc=T // 512, do_lru=True):
    p = Prog()
    q4T = p.dram_in("q4T", [128, 4, T])
    kcT = p.dram_in("kcT", [128, T]); vcT = p.dram_in("vcT", [128, T])
    ksT = p.dram_in("ksT", [128, T]); kwT = p.dram_in("kwT", [128, T])
    vs_d = p.dram_in("vs", [128, 64, 128]); vw_d = p.dram_in("vw", [128, 64, 128])
    glT = p.dram_in("glT", [3, T])
    tbl_d = p.dram_in("tblrep", [34, 4, 128])
    oh_d = p.dram_in("oh", [34, FX])
    cover_d = p.dram_in("cover1", [128, 4, 129])
    ebig_d = p.dram_in("ebig", [128, T])
    fext_d = p.dram_in("fext", [128, 256])
    ident_d = p.dram_in("ident", [128, 128])
    c31_d = p.dram_in("c31", [128, 4])
    w1_d = p.dram_in("w1", [2, 4096, 128]); peT_d = p.dram_in("peT", [2, 128, 32])
    b1_d = p.dram_in("b1", [128, 2]); w2_d = p.dram_in("w2", [2, 128, 128])
    b2k_d = p.dram_in("b2k", [128, 1]); b2v_d = p.dram_in("b2v", [128, 128])
    nsaT = p.dram_out("nsaT", [128, T])
    R_d = [p.dram_tmp("R%d" % i, [128, FX], BF16) for i in range(5)]

    identb = p.sb("identb", [128, 128], BF16)
    onesb = p.sb("onesb", [128, 128], BF16)
    cover = p.sb("cover", [128, 4, 129], BF16)
    ebig = p.sb("ebig", [128, T], BF16)
    fext = p.sb("fext", [128, 256], F32)
    c31 = p.sb("c31", [128, 4], F32)
    tbl = p.sb("tbl", [34, 4, 128], F32); oh = p.sb("oh", [34, 512], F32)
    b_const = p.buf()
    p.dma("pool", identb[:], ident_d, writes=[b_const])
    p.dma("pool", cover[:], cover_d, writes=[b_const])
    p.dma("pool", ebig[:], ebig_d, writes=[b_const])
    p.dma("sp", fext[:], fext_d, writes=[b_const])
    p.dma("sp", c31[:], c31_d, writes=[b_const])
    p.dma("sp", tbl[:], tbl_d, writes=[b_const])
    p.memset("dve", onesb[:], 1.0, [b_const])

    psA = [p.ps("psA%d" % i, [128, 512]) for i in range(2)]
    b_psA = [p.buf(), p.buf()]
    psO = [p.ps("psO%d" % i, [128, 512]) for i in range(2)]
    psZ = [p.ps("psZ%d" % i, [128, 512]) for i in range(2)]
    b_psO = [p.buf(), p.buf()]; b_psZ = [p.buf(), p.buf()]
    psI = p.ps("psI", [128, 512]); b_psI = p.buf()
    psT = p.ps("psT", [128, 128], BF16); b_psT = p.buf()

    rst = [p.sb("rst%d" % i, [128, 512], BF16) for i in range(2)]
    b_rst = [p.buf(), p.buf()]
    b_oh, b_R = p.buf(), p.buf()
    it = 0
    for xc in range(FX // 512):
        p.dma("sp", oh[:], oh_d[:, xc * 512:(xc + 1) * 512], writes=[b_oh])
        for r in range(5):
            s = it % 2
            hh = r if r < 4 else 0
            nrow = 33 if r < 4 else 34
            p.mm(psA[s][:], tbl[:nrow, hh, :], oh[:nrow, :], True, True, reads=[b_const, b_oh], writes=[b_psA[s]])
            p.act(rst[s][:], psA[s][:], AF.Copy, reads=[b_psA[s]], writes=[b_rst[s]], scale=1.0 / QS)
            p.dma("sp", R_d[r][:, xc * 512:(xc + 1) * 512], rst[s][:], reads=[b_rst[s]], writes=[b_R])
            it += 1

    def toep(Rd, pstride, base):
        return bass.AP(Rd.tensor, base, [[FX - pstride, 128], [1, 512]])

    bt_cmp = p.sb("bt_cmp", [128, 4, 5, 512], BF16)
    bt_slc = p.sb("bt_slc", [128, 5, 512], BF16)
    bt_win = p.sb("bt_win", [128, 4, 512], BF16)
    b_bt = p.buf()
    for hh in range(4):
        for a, d in enumerate(CMP_TILES):
            p.dma("sp", bt_cmp[:, hh, a, :], toep(R_d[hh], 16, FOFF + 512 * d - 31), reads=[b_R], writes=[b_bt])
    for a, d in enumerate(SLC_TILES):
        p.dma("sp", bt_slc[:, a, :], toep(R_d[0], 1, FOFF + 128 * d), reads=[b_R], writes=[b_bt])
    for a, d in enumerate(WIN_TILES):
        p.dma("sp", bt_win[:, a, :], toep(R_d[4], 1, FOFF + 128 * d), reads=[b_R], writes=[b_bt])

    kA = p.sb("kA", [128, T], BF16)
    vA = p.sb("vA", [128, T], BF16)
    ks = p.sb("ks", [128, T], BF16)
    vs = p.sb("vs", [128, 64, 128], BF16)
    b_kA, b_vA, b_ks, b_vs = p.buf(), p.buf(), p.buf(), p.buf()
    p.dma("pool", kA[:], kcT, writes=[b_kA])
    p.dma("pool", vA[:], vcT, writes=[b_vA])
    p.dma("pool", ks[:], ksT, writes=[b_ks])
    p.dma("pool", vs[:], vs_d, writes=[b_vs])

    ecmp = p.sb("ecmp", [128, 4, 4, 512], BF16)
    ecf = ecmp[:].rearrange("p a b c -> p (a b c)")
    w1 = [ecf[:, kv * 4096:(kv + 1) * 4096].rearrange("p (j o) -> p j o", o=128) for kv in range(2)]
    peT = p.sb("peT", [128, 2, 32], BF16)
    b1 = p.sb("b1", [128, 2], F32); w2 = p.sb("w2", [128, 2, 128], BF16)
    b2k = p.sb("b2k", [128, 1], F32); b2v = p.sb("b2v", [128, 128], F32)
    b_cw = p.buf()
    for kv in range(2):
        p.dma("pool", w1[kv], w1_d[kv].rearrange("(j d) o -> d j o", d=128), writes=[b_cw])
        p.dma("pool", peT[:, kv, :], peT_d[kv], writes=[b_cw])
        p.dma("pool", w2[:, kv, :], w2_d[kv], writes=[b_cw])
    p.dma("sp", b1[:], b1_d, writes=[b_cw]); p.dma("sp", b2k[:], b2k_d, writes=[b_cw]); p.dma("sp", b2v[:], b2v_d, writes=[b_cw])
    kcmpT = p.sb("kcmpT", [128, 512], BF16)
    vcmp = p.sb("vcmp", [128, 4, 128], BF16)
    hidT = p.sb("hidT", [128, 512], BF16)
    g1t = p.sb("g1t", [128, 512], F32); g2t = p.sb("g2t", [128, 512], F32); g3t = p.sb("g3t", [128, 512], F32)
    cb = p.sb("cb", [128, 1], F32)
    b_kc, b_vc, b_hid, b_g, b_cb = p.buf(), p.buf(), p.buf(), p.buf(), p.buf()
    p.memset("dve", hidT[:], 0.0, [b_hid])
    p.memset("dve", kcmpT[:], 0.0, [b_kc])

    def gelu_tanh(src, b_src, dst, b_dst, bias_ap, reads_extra, width):
        p.act(g1t[:, :width], src, AF.Identity, reads=[b_src] + reads_extra, writes=[b_g], bias=bias_ap)
        p.tt("dve", g2t[:, :width], g1t[:, :width], g1t[:, :width], ALU.mult, reads=[b_g], writes=[b_g])
        p.ts("dve", g2t[:, :width], g2t[:, :width], 0.044715, 1.0, ALU.mult, ALU.add, reads=[b_g], writes=[b_g])
        p.tt("dve", g2t[:, :width], g2t[:, :width], g1t[:, :width], ALU.mult, reads=[b_g], writes=[b_g])
        p.act(g3t[:, :width], g2t[:, :width], AF.Sigmoid, reads=[b_g], writes=[b_g], scale=1.5957691216)
        p.tt("dve", dst, g3t[:, :width], g1t[:, :width], ALU.mult, reads=[b_g], writes=[b_dst])

    for kv, (src, b_src) in enumerate(((kA, b_kA), (vA, b_vA))):
        sv = src[:].rearrange("p (n s) -> p n s", s=16)
        for j in range(32):
            p.mm(psA[1][:, 0:1], w1[kv][:, j, :], peT[:, kv, j:j + 1], j == 0, j == 31, reads=[b_cw], writes=[b_psA[1]])
        p.tt("dve", cb[:], psA[1][:, 0:1], b1[:, kv:kv + 1], ALU.add, reads=[b_psA[1], b_cw], writes=[b_cb])
        for j in range(32):
            jq, jr = j // 16, j % 16
            p.mm(psA[0][:, :511], w1[kv][:, j, :], sv[:, jq:jq + 511, jr], j == 0, j == 31, reads=[b_cw, b_src], writes=[b_psA[0]])
        gelu_tanh(psA[0][:, :511], b_psA[0], hidT[:, :511], b_hid, cb[:], [b_cb], 511)
        if kv == 0:
            p.mm(psA[1][:, :512], w2[:, 0, :], hidT[:], True, True, reads=[b_cw, b_hid], writes=[b_psA[1]])
            p.act(kcmpT[:, :511], psA[1][:, :511], AF.Identity, reads=[b_psA[1], b_cw], writes=[b_kc], bias=b2k[:])
        else:
            for c in range(4):
                p.mm(psA[1][:, :128], hidT[:, c * 128:(c + 1) * 128], w2[:, 1, :], True, True, reads=[b_cw, b_hid], writes=[b_psA[1]])
                p.tt("dve", vcmp[:, c, :], psA[1][:, :128], b2v[:], ALU.add, reads=[b_psA[1], b_cw], writes=[b_vc])
    vAv = vA[:].rearrange("p (j d) -> p j d", d=128)
    p.dma("pool", kA[:], kwT, writes=[b_kA])
    p.dma("pool", vAv, vw_d, writes=[b_vA])

    q4c = [p.sb("q4c%d" % i, [128, 4, 512], BF16) for i in range(2)]
    b_q = [p.buf(), p.buf()]
    gt0 = p.sb("gt0", [128, 3, 512], F32)
    gt = [gt0, gt0]
    b_gt0 = p.buf()
    b_gt = [b_gt0, b_gt0]
    b_ecmp = [p.buf() for _ in range(4)]
    impacc = p.sb("impacc", [128, 4, 128], F32); b_imp = p.buf()
    rz1 = p.sb("rz1", [128, 1], F32); b_rz1 = p.buf()
    vals = p.sb("vals", [128, 128], F32); work = p.sb("work", [128, 128], F32)
    m8a = p.sb("m8a", [128, 8], F32); m8b = p.sb("m8b", [128, 8], F32)
    selq = p.sb("selq", [128, 128], BF16)
    b_sel = p.buf()
    selT = p.sb("selT", [128, 512], BF16); b_selT = p.buf()
    es = [p.sb("es%d" % i, [128, 512], BF16) for i in range(3)]
    b_es = [p.buf() for _ in range(3)]
    rz = p.sb("rz", [128, 512], F32); wgt = p.sb("wgt", [128, 512], F32); b_rz = p.buf()
    onsa = [p.sb("onsa%d" % i, [128, 512], F32) for i in range(2)]
    b_on = [p.buf(), p.buf()]
    esi = 0
    ozi = 0

    def branch_finish(i, br, oz, first):
        s = i % 2
        p.ts("dve", rz[:], psZ[oz][:], TINY, None, ALU.max, None, reads=[b_psZ[oz]], writes=[b_rz])
        p.op("dve", lambda t: t.reciprocal(out=rz[:], in_=rz[:]), reads=[b_rz], writes=[b_rz])
        p.tt("dve", wgt[:], rz[:], gt[s][:, br, :], ALU.mult, reads=[b_rz, b_gt[s]], writes=[b_rz])
        if first:
            p.tt("dve", onsa[s][:], psO[oz][:], wgt[:], ALU.mult, reads=[b_psO[oz], b_rz], writes=[b_on[s]])
        else:
            p.tt("dve", wgt[:], psO[oz][:], wgt[:], ALU.mult, reads=[b_psO[oz], b_rz], writes=[b_rz])
            p.tt("dve", onsa[s][:], onsa[s][:], wgt[:], ALU.add, reads=[b_rz], writes=[b_on[s]])

    for i in range(nqc):
        s = i % 2
        tsl = slice(i * 512, (i + 1) * 512)
        p.dma("pool", q4c[s][:], q4T[:, :, tsl], writes=[b_q[s]])
        p.dma("sp", gt[s][:], glT[:, tsl].partition_broadcast(128), writes=[b_gt[s]])
        p.act(gt[s][:], gt[s][:], AF.Sigmoid, reads=[b_gt[s]], writes=[b_gt[s]])
        njs = [nj for nj in range(4) if i - 4 * nj >= 0]
        first_imp = True
        for hh in range(4):
            for nj in njs:
                d = i - 4 * nj
                a = esi % 2; esi += 1
                p.mm(psA[a][:], kcmpT[:, nj * 128:(nj + 1) * 128], q4c[s][:, hh, :], True, d > 4, reads=[b_kc, b_q[s]], writes=[b_psA[a]])
                if d <= 4:
                    p.mm(psA[a][:], identb[:], bt_cmp[:, hh, d, :], False, True, reads=[b_const, b_bt], writes=[b_psA[a]])
                p.act(ecmp[:, hh, nj, :], psA[a][:], AF.Exp, reads=[b_psA[a], b_const], writes=[b_ecmp[hh], b_cw], scale=QS, bias=c31[:, hh:hh + 1])
            for qs in range(4):
                for x, nj in enumerate(njs):
                    p.mm(psI[:, :129], ecmp[:, hh, nj, qs * 128:(qs + 1) * 128], cover[:, nj, :], x == 0, x == len(njs) - 1,
                         reads=[b_ecmp[hh], b_const], writes=[b_psI])
                p.ts("dve", rz1[:], psI[:, 128:129], TINY, None, ALU.max, None, reads=[b_psI], writes=[b_rz1])
                p.op("dve", lambda t: t.reciprocal(out=rz1[:], in_=rz1[:]), reads=[b_rz1], writes=[b_rz1])
                if hh == 0:
                    p.ts("dve", impacc[:, qs, :], psI[:, :128], rz1[:], None, ALU.mult, None, reads=[b_psI, b_rz1], writes=[b_imp])
                else:
                    p.op("dve", lambda t, qs=qs: t.scalar_tensor_tensor(out=impacc[:, qs, :], in0=psI[:, :128], scalar=rz1[:],
                                                                      in1=impacc[:, qs, :], op0=ALU.mult, op1=ALU.add),
                         reads=[b_psI, b_rz1, b_imp], writes=[b_imp])
            if hh == 0:
                oz = ozi % 2; ozi += 1
                for x, nj in enumerate(njs):
                    p.mm(psO[oz][:], vcmp[:, nj, :], ecmp[:, 0, nj, :], x == 0, x == len(njs) - 1, reads=[b_vc, b_ecmp[0]], writes=[b_psO[oz]])
                for x, nj in enumerate(njs):
                    p.mm(psZ[oz][:], onesb[:], ecmp[:, 0, nj, :], x == 0, x == len(njs) - 1, reads=[b_const, b_ecmp[0]], writes=[b_psZ[oz]])
                branch_finish(i, 0, oz, True)
        for qs in range(4):
            qb = 4 * i + qs
            p.tt("dve", vals[:], impacc[:, qs, :], fext[:, 128 - 2 * qb:256 - 2 * qb], ALU.add, reads=[b_imp, b_const], writes=[b_sel])
            p.op("dve", lambda t: t.max(out=m8a[:], in_=vals[:]), reads=[b_sel], writes=[b_sel])
            p.op("dve", lambda t: t.match_replace(out=work[:], in_to_replace=m8a[:], in_values=vals[:], imm_value=-1e30), reads=[b_sel], writes=[b_sel])
            p.op("dve", lambda t: t.max(out=m8b[:], in_=work[:]), reads=[b_sel], writes=[b_sel])
            p.ts("dve", work[:], vals[:], m8b[:, 7:8], None, ALU.is_ge, None, reads=[b_sel], writes=[b_sel])
            p.ts("dve", selq[:], work[:], NEGM, -NEGM, ALU.mult, ALU.add, reads=[b_sel], writes=[b_sel])
            p.transpose(psT[:], selq[:], identb[:], reads=[b_sel, b_const], writes=[b_psT])
            p.copy("dve", selT[:, qs * 128:(qs + 1) * 128], psT[:], reads=[b_psT], writes=[b_selT])
        oz = ozi % 2; ozi += 1
        nkb = 4 * i + 4
        for j in range(nkb):
            d = 4 * i - j
            a = esi % 2; esi += 1
            e = esi % 3
            p.mm(psA[a][:], ks[:, j * 128:(j + 1) * 128], q4c[s][:, 0, :], True, False, reads=[b_ks, b_q[s]], writes=[b_psA[a]])
            p.mm(psA[a][:], ebig[:, j * 128:(j + 1) * 128], selT[:], False, d > 1, reads=[b_const, b_selT], writes=[b_psA[a]])
            if d <= 1:
                p.mm(psA[a][:], identb[:], bt_slc[:, d + 3, :], False, True, reads=[b_const, b_bt], writes=[b_psA[a]])
            p.act(es[e][:], psA[a][:], AF.Exp, reads=[b_psA[a], b_const], writes=[b_es[e]], scale=QS, bias=c31[:, 0:1])
            p.mm(psO[oz][:], vs[:, j, :], es[e][:], j == 0, j == nkb - 1, reads=[b_vs, b_es[e]], writes=[b_psO[oz]])
            p.mm(psZ[oz][:], onesb[:], es[e][:], j == 0, j == nkb - 1, reads=[b_const, b_es[e]], writes=[b_psZ[oz]])
        branch_finish(i, 1, oz, False)
        oz = ozi % 2; ozi += 1
        js = list(range(max(0, 4 * i - 4), 4 * i + 4))
        for x, j in enumerate(js):
            d = 4 * i - j
            a = esi % 2; esi += 1
            e = esi % 3
            p.mm(psA[a][:], kA[:, j * 128:(j + 1) * 128], q4c[s][:, 0, :], True, False, reads=[b_kA, b_q[s]], writes=[b_psA[a]])
            btile = bt_slc[:, d + 3, :] if d <= 0 else bt_win[:, d - 1, :]
            p.mm(psA[a][:], identb[:], btile, False, True, reads=[b_const, b_bt], writes=[b_psA[a]])
            p.act(es[e][:], psA[a][:], AF.Exp, reads=[b_psA[a], b_const], writes=[b_es[e]], scale=QS, bias=c31[:, 0:1])
            p.mm(psO[oz][:], vAv[:, j, :], es[e][:], x == 0, x == len(js) - 1, reads=[b_vA, b_es[e]], writes=[b_psO[oz]])
            p.mm(psZ[oz][:], onesb[:], es[e][:], x == 0, x == len(js) - 1, reads=[b_const, b_es[e]], writes=[b_psZ[oz]])
        branch_finish(i, 2, oz, False)
        p.dma("sp", nsaT[:, tsl], onsa[s][:], reads=[b_on[s]], is_output=True)

    return p.finish()


def build_L2b():
    p = Prog()
    CH = 1024
    xbr_d = p.dram_in("xbr", [128, T]); ybr_d = p.dram_in("ybr", [128, T])
    lvec_d = p.dram_in("lvec", [128, 8])
    wa_d = p.dram_in("wa", [128, 128]); wi_d = p.dram_in("wi", [128, 128])
    lruT = p.dram_out("lruT", [128, T])
    lv = p.sb("lv", [128, 8], F32); wa = p.sb("wa", [128, 128], F32); wi = p.sb("wi", [128, 128], F32)
    nsp = p.sb("nsp", [128, 2], F32)
    b_lc = p.buf()
    p.dma("sp", lv[:], lvec_d, writes=[b_lc]); p.dma("sp", wa[:], wa_d, writes=[b_lc]); p.dma("sp", wi[:], wi_d, writes=[b_lc])
    p.act(nsp[:, 0:1], lv[:, 7:8], AF.Exp, reads=[b_lc], writes=[b_lc], scale=-1.0)
    p.ts("dve", nsp[:, 0:1], nsp[:, 0:1], 1.0, None, ALU.add, None, reads=[b_lc], writes=[b_lc])
    p.act(nsp[:, 0:1], nsp[:, 0:1], AF.Ln, reads=[b_lc], writes=[b_lc])
    p.ts("dve", nsp[:, 1:2], nsp[:, 0:1], -16.0, None, ALU.mult, None, reads=[b_lc], writes=[b_lc])
    p.ts("dve", nsp[:, 0:1], nsp[:, 0:1], -8.0, None, ALU.mult, None, reads=[b_lc], writes=[b_lc])
    xb = [p.sb("lxb%d" % i, [128, CH + 3], F32) for i in range(2)]
    b_xb = [p.buf(), p.buf()]
    yb = p.sb("lyb", [128, CH], F32); b_yb = p.buf()
    xc = p.sb("lxc", [128, CH], F32); b_xc = p.buf()
    ra = p.sb("lra", [128, CH], F32); ri = p.sb("lri", [128, CH], F32); b_ra, b_ri = p.buf(), p.buf()
    av = p.sb("lav", [128, CH], F32); bv = p.sb("lbv", [128, CH], F32); b_av, b_bv = p.buf(), p.buf()
    hv = [p.sb("lhv%d" % i, [128, CH], F32) for i in range(2)]; b_hv = [p.buf(), p.buf()]
    t1 = p.sb("lt1", [128, CH], F32); t2 = p.sb("lt2", [128, CH], F32); b_t1, b_t2 = p.buf(), p.buf()
    ov = p.sb("lov", [128, CH], F32); b_ov = p.buf()
    return_ps = [p.ps("psL%d" % i, [128, 512]) for i in range(2)]
    b_lps = [p.buf(), p.buf()]
    p.memset("dve", xb[0][:, 0:3], 0.0, [b_xb[0]])
    for c in range(T // CH):
        s = c % 2
        tsl = slice(c * CH, (c + 1) * CH)
        p.dma("sp", xb[s][:, 3:], xbr_d[:, tsl], writes=[b_xb[s]])
        p.dma("sp", yb[:], ybr_d[:, tsl], writes=[b_yb])
        if c > 0:
            p.copy("dve", xb[s][:, 0:3], xb[1 - s][:, CH:CH + 3], reads=[b_xb[1 - s]], writes=[b_xb[s]])
        p.ts("dve", xc[:], xb[s][:, 0:CH], lv[:, 0:1], lv[:, 4:5], ALU.mult, ALU.add, reads=[b_xb[s], b_lc], writes=[b_xc])
        for k in range(1, 4):
            p.op("dve", lambda t, k=k, s=s: t.scalar_tensor_tensor(out=xc[:], in0=xb[s][:, k:k + CH], scalar=lv[:, k:k + 1], in1=xc[:],
                                                                  op0=ALU.mult, op1=ALU.add), reads=[b_xb[s], b_lc, b_xc], writes=[b_xc])
        for hc in range(CH // 512):
            hs = slice(hc * 512, (hc + 1) * 512)
            p.mm(return_ps[0][:], wa[:], xc[:, hs], True, True, reads=[b_lc, b_xc], writes=[b_lps[0]])
            p.act(ra[:, hs], return_ps[0][:], AF.Sigmoid, reads=[b_lps[0], b_lc], writes=[b_ra], bias=lv[:, 5:6])
            p.mm(return_ps[1][:], wi[:], xc[:, hs], True, True, reads=[b_lc, b_xc], writes=[b_lps[1]])
            p.act(ri[:, hs], return_ps[1][:], AF.Sigmoid, reads=[b_lps[1], b_lc], writes=[b_ri], bias=lv[:, 6:7])
        p.act(av[:], ra[:], AF.Exp, reads=[b_ra, b_lc], writes=[b_av], scale=nsp[:, 0:1])
        p.act(t1[:], ra[:], AF.Exp, reads=[b_ra, b_lc], writes=[b_t1], scale=nsp[:, 1:2])
        p.ts("dve", t1[:], t1[:], -1.0, 1.0, ALU.mult, ALU.add, reads=[b_t1], writes=[b_t1])
        p.ts("dve", t1[:], t1[:], 0.0, None, ALU.max, None, reads=[b_t1], writes=[b_t1])
        p.act(t1[:], t1[:], AF.Sqrt, reads=[b_t1], writes=[b_t1])
        p.tt("dve", bv[:], ri[:], xc[:], ALU.mult, reads=[b_ri, b_xc], writes=[b_bv])
        p.tt("dve", bv[:], bv[:], t1[:], ALU.mult, reads=[b_bv, b_t1], writes=[b_bv])
        init = 0.0 if c == 0 else hv[1 - s][:, CH - 1:CH]
        p.op("dve", lambda t, s=s, init=init: t.tensor_tensor_scan(out=hv[s][:], data0=av[:], data1=bv[:], initial=init,
                                                                   op0=ALU.mult, op1=ALU.add),
             reads=[b_av, b_bv] + ([b_hv[1 - s]] if c > 0 else []), writes=[b_hv[s]])
        p.tt("dve", t2[:], yb[:], yb[:], ALU.mult, reads=[b_yb], writes=[b_t2])
        p.ts("dve", t2[:], t2[:], 0.044715, 1.0, ALU.mult, ALU.add, reads=[b_t2], writes=[b_t2])
        p.tt("dve", t2[:], t2[:], yb[:], ALU.mult, reads=[b_t2, b_yb], writes=[b_t2])
        p.act(t2[:], t2[:], AF.Sigmoid, reads=[b_t2], writes=[b_t2], scale=1.5957691216)
        p.tt("dve", t2[:], t2[:], yb[:], ALU.mult, reads=[b_t2, b_yb], writes=[b_t2])
        p.tt("dve", ov[:], t2[:], hv[s][:], ALU.mult, reads=[b_t2, b_hv[s]], writes=[b_ov])
        p.dma("sp", lruT[:, tsl], ov[:], reads=[b_ov], is_output=True)
    return p.finish()


def stage_L2(inp, projT, nqc=T // 512):
    oh, cover1, ebig, fext, ident = nsa_consts()
    nc = build_L2(nqc)
    rb = inp["rel_bias"]
    maps = []
    for h in range(NCORE):
        g = h // 4
        heads = [h] + [hh for hh in range(4 * g, 4 * g + 4) if hh != h]
        q4 = np.ascontiguousarray(np.stack([projT[hh * 128:(hh + 1) * 128] for hh in heads], axis=1))

        def kv(idx):
            base = 1024 + idx * 256 + g * 128
            return projT[base:base + 128]

        def tokmaj(a):
            return np.ascontiguousarray(a.T.reshape(64, 128, 128).transpose(1, 0, 2))
        tblrep = np.zeros((34, 4, 128), np.float32)
        for a, hh in enumerate(heads):
            tblrep[:32, a, :] = rb[:, hh][:, None]
        tblrep[32:] = -30000.0
        c31 = np.ascontiguousarray(np.stack([np.full(128, rb[31, hh], np.float32) for hh in heads], axis=1))
        maps.append({
            "q4T": q4, "kcT": np.ascontiguousarray(kv(0)), "vcT": np.ascontiguousarray(kv(1)),
            "ksT": np.ascontiguousarray(kv(2)), "vs": tokmaj(kv(3)), "kwT": np.ascontiguousarray(kv(4)), "vw": tokmaj(kv(5)),
            "glT": np.ascontiguousarray(projT[2560 + 3 * h:2560 + 3 * h + 3]),
            "tblrep": tblrep, "oh": oh, "cover1": cover1, "ebig": ebig, "fext": fext, "ident": ident, "c31": c31,
            "w1": inp["cmp_w1"][0], "peT": np.ascontiguousarray(inp["cmp_pe"][0].transpose(0, 2, 1)),
            "b1": np.ascontiguousarray(inp["cmp_b1"][0].T), "w2": inp["cmp_w2"][0],
            "b2k": np.ascontiguousarray(inp["cmp_b2"][0][0].reshape(128, 1)),
            "b2v": np.ascontiguousarray(np.tile(inp["cmp_b2"][0][1][None, :], (128, 1))),
        })
    res = run_spmd(nc, maps)
    return np.concatenate([r["nsaT"] for r in res], axis=0)


def stage_L2b(inp, projT):
    nc = build_L2b()
    maps = []
    for c in range(NCORE):
        sl = slice(c * 128, (c + 1) * 128)
        lvec = np.stack([inp["lru_conv_w"][0][k, sl] for k in range(4)] +
                        [inp["lru_conv_b"][0][sl], inp["lru_ba"][0][sl], inp["lru_bi"][0][sl], inp["lru_lambda"][0][sl]], axis=1)
        maps.append({"xbr": np.ascontiguousarray(projT[3608 + c * 128:3608 + (c + 1) * 128]),
                     "ybr": np.ascontiguousarray(projT[2584 + c * 128:2584 + (c + 1) * 128]),
                     "lvec": np.ascontiguousarray(lvec.astype(np.float32)),
                     "wa": inp["lru_wa"][0][c], "wi": inp["lru_wi"][0][c]})
    res = run_spmd(nc, maps)
    return np.concatenate([r["lruT"] for r in res], axis=0)

NE = 64
DE = 512
ODD_COLS = 8192


def emit_rms_mod2(p, xt, b_x, coef, shift, b_cs, hbf32, b_hf, ntok, scratch, after_chunk):
    onesf, epst, sq, tmp, rs, ps_ss, b_sq, b_tmp, b_rs, b_ss = scratch
    for tc in range(ntok // 512):
        tsl = slice(tc * 512, (tc + 1) * 512)
        p.act(sq[:], xt[:, :, tsl], AF.Square, reads=[b_x], writes=[b_sq])
        for k in range(KC):
            p.mm(ps_ss[:], onesf[:], sq[:, k, :], k == 0, k == KC - 1, reads=[b_sq], writes=[b_ss])
        p.act(rs[:], ps_ss[:], AF.Sqrt, reads=[b_ss], writes=[b_rs], scale=1.0 / D, bias=epst[:])
        p.op("dve", lambda t: t.reciprocal(out=rs[:], in_=rs[:]), reads=[b_rs], writes=[b_rs])
        for k in range(KC):
            j = k % 2
            p.op("dve", lambda t, k=k, j=j, tsl=tsl: t.scalar_tensor_tensor(out=tmp[j][:], in0=xt[:, k, tsl], scalar=coef[:, k:k + 1],
                                                                           in1=rs[:], op0=ALU.mult, op1=ALU.mult),
                 reads=[b_x, b_cs, b_rs], writes=[b_tmp[j]])
            p.act(hbf32[:, k, :], tmp[j][:], AF.Identity, reads=[b_tmp[j], b_cs], writes=[b_hf], bias=shift[:, k:k + 1])
        after_chunk(tc)


def emit_rms_plain(p, xt, b_x, gain, b_g, outt, b_out, ntok, scratch):
    onesf, epst, sq, tmp, rs, ps_ss, b_sq, b_tmp, b_rs, b_ss = scratch
    for tc in range(ntok // 512):
        tsl = slice(tc * 512, (tc + 1) * 512)
        p.act(sq[:], xt[:, :, tsl], AF.Square, reads=[b_x], writes=[b_sq])
        for k in range(KC):
            p.mm(ps_ss[:], onesf[:], sq[:, k, :], k == 0, k == KC - 1, reads=[b_sq], writes=[b_ss])
        p.act(rs[:], ps_ss[:], AF.Sqrt, reads=[b_ss], writes=[b_rs], scale=1.0 / D, bias=epst[:])
        p.op("dve", lambda t: t.reciprocal(out=rs[:], in_=rs[:]), reads=[b_rs], writes=[b_rs])
        for k in range(KC):
            p.op("dve", lambda t, k=k, tsl=tsl: t.scalar_tensor_tensor(out=outt[:, k, tsl], in0=xt[:, k, tsl], scalar=gain[:, k:k + 1],
                                                                      in1=rs[:], op0=ALU.mult, op1=ALU.mult),
                 reads=[b_x, b_g, b_rs], writes=[b_out])


def build_postA(layer):
    last = (layer == 1)
    p = Prog()
    xT = p.dram_in("xT", [D, TL]); mixT = p.dram_in("mixT", [D, TL])
    gateT = p.dram_in("gateT", [D, TL]) if last else None
    w_out = p.dram_in("w_out", [D, D])
    vecs = p.dram_in("vecs", [128, 4, KC])
    wr_d = p.dram_in("wr", [D, 72]); br_d = p.dram_in("br", [128, 72]); ident_d = p.dram_in("ident", [128, 128])
    x1T = p.dram_out("x1T", [D, TL]); hfT = p.dram_out("hfT", [D, TL]); gwTo = p.dram_out("gwT", [64, TL])
    xt = p.sb("xt", [128, KC, TL], F32); b_x = p.buf()
    hbf = p.sb("hbf", [128, KC, TL], BF16); b_h = p.buf()
    vt = p.sb("vt", [128, 4, KC], F32); b_v = p.buf()
    coef = p.sb("coef", [128, KC], F32)
    ident = p.sb("ident", [128, 128], F32); b_c = p.buf()
    scratch = alloc_rms_scratch(p)
    xv = xT.rearrange("(k p) t -> p k t", p=128); mv = mixT.rearrange("(k p) t -> p k t", p=128)
    for k4 in range(4):
        p.dma("sp", xt[:, k4 * 4:(k4 + 1) * 4, :], xv[:, k4 * 4:(k4 + 1) * 4, :], writes=[b_x])
    p.dma("sp", vt[:], vecs, writes=[b_v]); p.dma("sp", ident[:], ident_d, writes=[b_c])
    p.ts("dve", coef[:], vt[:, 2, :], 1.0, None, ALU.add, None, reads=[b_v], writes=[b_v])
    p.tt("dve", coef[:], coef[:], vt[:, 1, :], ALU.mult, reads=[b_v], writes=[b_v])
    hf32 = scratch[2]; b_hf = scratch[6]
    if last:
        gv = gateT.rearrange("(k p) t -> p k t", p=128)
        b_m = p.buf()
        for k4 in range(8):
            ks = slice(k4 * 2, (k4 + 1) * 2)
            mt = hf32[:, 0:2, :].rearrange("p a (b t) -> p (a b) t", b=1) if False else None
            m2 = hf32[:, 0:4, :].rearrange("p (a b) t -> p a (b t)", b=2)
            g2 = hf32[:, 4:8, :].rearrange("p (a b) t -> p a (b t)", b=2)
            p.dma("sp", m2, mv[:, ks, :], writes=[b_hf])
            p.dma("act", g2, gv[:, ks, :], writes=[b_hf])
            p.act(g2, g2, AF.Silu, reads=[b_hf], writes=[b_hf])
            p.tt("dve", hbf[:, ks, :], m2, g2, ALU.mult, reads=[b_hf], writes=[b_h])
    else:
        for k4 in range(4):
            p.dma("pool", hbf[:, k4 * 4:(k4 + 1) * 4, :], mv[:, k4 * 4:(k4 + 1) * 4, :], writes=[b_h])

    def evac_res(c0, mw, tc, ps_t, b_ps):
        k = c0 // 128
        tsl = slice(tc * 512, (tc + 1) * 512)
        p.op("dve", lambda t: t.scalar_tensor_tensor(out=xt[:, k, tsl], in0=ps_t[:], scalar=vt[:, 0, k:k + 1], in1=xt[:, k, tsl],
                                                     op0=ALU.mult, op1=ALU.add), reads=[b_ps, b_v, b_x], writes=[b_x])
    emit_proj(p, hbf, b_h, w_out, D, TL, None, evac=evac_res, stage=(scratch[2], scratch[6]))
    ov = x1T.rearrange("(k p) t -> p k t", p=128)
    for k4 in range(4):
        p.dma("sp", ov[:, k4 * 4:(k4 + 1) * 4, :], xt[:, k4 * 4:(k4 + 1) * 4, :], reads=[b_x], is_output=True)
    wr = p.sb("wr", [128, KC, 72], F32); br = p.sb("br", [128, 72], F32)
    p.dma("sp", wr[:], wr_d.rearrange("(k p) n -> p k n", p=128), writes=[b_c]); p.dma("sp", br[:], br_d, writes=[b_c])
    gwT = p.sb("gwT", [64, TL], F32); b_gw = p.buf()
    psR = p.ps("psR", [128, 512]); b_psR = p.buf()
    lg = p.sb("lg", [128, 72], F32); gm = p.sb("gm", [128, 8], F32); goh = p.sb("goh", [128, 8], F32)
    ex = p.sb("ex", [128, 8], F32); pg = p.sb("pg", [128, 2], F32); es_ = p.sb("esel", [128, 64], F32)
    t8 = p.sb("t8", [128, 8], F32); w12 = p.sb("w12", [128, 4], F32); gw = p.sb("gw", [128, 64], F32); gw2 = p.sb("gw2", [128, 64], F32)
    b_rt = p.buf()
    hv = hfT.rearrange("(k p) t -> p k t", p=128)

    def router(tc):
        p.dma("sp", hv[:, :, tc * 512:(tc + 1) * 512], hf32[:], reads=[b_hf], is_output=True)
        for sub in range(4):
            tsl = slice(sub * 128, (sub + 1) * 128)
            for k in range(KC):
                p.mm(psR[:, :72], hf32[:, k, tsl], wr[:, k, :], k == 0, k == KC - 1, reads=[b_hf, b_c], writes=[b_psR])
            p.tt("dve", lg[:], psR[:, :72], br[:], ALU.add, reads=[b_psR, b_c], writes=[b_rt])
            p.op("dve", lambda t: t.tensor_reduce(out=gm[:, 0:1], in_=lg[:, 0:8], axis=AX.X, op=ALU.max), reads=[b_rt], writes=[b_rt])
            p.ts("dve", goh[:], lg[:, 0:8], gm[:, 0:1], None, ALU.is_ge, None, reads=[b_rt], writes=[b_rt])
            p.ts("dve", ex[:], lg[:, 0:8], gm[:, 0:1], None, ALU.subtract, None, reads=[b_rt], writes=[b_rt])
            p.act(ex[:], ex[:], AF.Exp, reads=[b_rt], writes=[b_rt])
            p.op("dve", lambda t: t.tensor_reduce(out=pg[:, 0:1], in_=ex[:], axis=AX.X, op=ALU.add), reads=[b_rt], writes=[b_rt])
            p.op("dve", lambda t: t.reciprocal(out=pg[:, 0:1], in_=pg[:, 0:1]), reads=[b_rt], writes=[b_rt])
            p.ts("dve", goh[:], goh[:], 1e9, -1e9, ALU.mult, ALU.add, reads=[b_rt], writes=[b_rt])
            for g in range(8):
                p.ts("dve", es_[:, g * 8:(g + 1) * 8], lg[:, 8 + g * 8:16 + g * 8], goh[:, g:g + 1], None, ALU.add, None, reads=[b_rt], writes=[b_rt])
            p.op("dve", lambda t: t.max(out=t8[:], in_=es_[:]), reads=[b_rt], writes=[b_rt])
            p.tt("dve", w12[:, 0:1], t8[:, 1:2], t8[:, 0:1], ALU.subtract, reads=[b_rt], writes=[b_rt])
            p.act(w12[:, 0:1], w12[:, 0:1], AF.Exp, reads=[b_rt], writes=[b_rt])
            p.ts("dve", w12[:, 1:2], w12[:, 0:1], 1.0, None, ALU.add, None, reads=[b_rt], writes=[b_rt])
            p.op("dve", lambda t: t.reciprocal(out=w12[:, 1:2], in_=w12[:, 1:2]), reads=[b_rt], writes=[b_rt])
            p.tt("dve", w12[:, 2:3], w12[:, 0:1], w12[:, 1:2], ALU.mult, reads=[b_rt], writes=[b_rt])
            p.tt("dve", w12[:, 1:2], w12[:, 1:2], pg[:, 0:1], ALU.mult, reads=[b_rt], writes=[b_rt])
            p.tt("dve", w12[:, 2:3], w12[:, 2:3], pg[:, 0:1], ALU.mult, reads=[b_rt], writes=[b_rt])
            p.ts("dve", gw[:], es_[:], t8[:, 0:1], w12[:, 1:2], ALU.is_equal, ALU.mult, reads=[b_rt], writes=[b_rt])
            p.ts("dve", gw2[:], es_[:], t8[:, 1:2], w12[:, 2:3], ALU.is_equal, ALU.mult, reads=[b_rt], writes=[b_rt])
            p.tt("dve", gw[:], gw[:], gw2[:], ALU.add, reads=[b_rt], writes=[b_rt])
            p.transpose(psR[:64, 128:256], gw[:], ident[:], reads=[b_rt, b_c], writes=[b_psR])
            g0 = tc * 512 + sub * 128
            p.copy("dve", gwT[:, g0:g0 + 128], psR[:64, 128:256], reads=[b_psR], writes=[b_gw])

    emit_rms_mod2(p, xt, b_x, coef, vt[:, 3, :], b_v, hf32, b_hf, TL, scratch, router)
    p.dma("sp", gwTo, gwT[:], reads=[b_gw], is_output=True)
    return p.finish()


def build_moe(nchunk=T // 1024):
    p = Prog()
    hfT = p.dram_in("hfT", [D, T]); gw_d = p.dram_in("gw", [8, T])
    wg_d = p.dram_in("wg", [8, D, DE]); wu_d = p.dram_in("wu", [8, D, DE]); wd_d = p.dram_in("wd", [8, DE, D])
    yT = p.dram_out("yT", [D, T])
    CH = 1024
    HF = 256
    hbf = p.sb("hbf", [128, KC, CH], BF16); b_h = p.buf()
    acc = [p.sb("acc%d" % i, [128, KC, 512], F32) for i in range(2)]; b_acc = [p.buf(), p.buf()]
    wgt = [p.sb("wg%d" % i, [128, KC, HF], BF16) for i in range(2)]
    wut = [p.sb("wu%d" % i, [128, KC, HF], BF16) for i in range(2)]
    wdt = [p.sb("wd%d" % i, [128, HF // 128, D], BF16) for i in range(2)]
    b_we = [p.buf(), p.buf()]
    stg = [p.sb("stg%d" % i, [128, KC, HF], F32) for i in range(3)]; b_stg = [p.buf() for _ in range(3)]
    gwb = [p.sb("gwb%d" % i, [128, 512], F32) for i in range(2)]; b_gwb = [p.buf(), p.buf()]
    psG = p.ps("psG", [128, 512]); psU = p.ps("psU", [128, 512]); b_psG, b_psU = p.buf(), p.buf()
    pp = [p.ps("pp%d" % i, [128, 512]) for i in range(2)]; b_pp = [p.buf(), p.buf()]
    sg = p.sb("sg", [128, 512], F32); b_sg = p.buf()
    hw = [p.sb("hw%d" % i, [128, HF // 128, 512], BF16) for i in range(2)]; b_hw = [p.buf(), p.buf()]
    hv = hfT.rearrange("(k p) t -> p k t", p=128); yv = yT.rearrange("(k p) t -> p k t", p=128)
    unit = 0
    it = 0
    for c in range(nchunk):
        for k4 in range(4):
            p.dma("pool", hbf[:, k4 * 4:(k4 + 1) * 4, :], hv[:, k4 * 4:(k4 + 1) * 4, c * CH:(c + 1) * CH], writes=[b_h])
        for e in range(8):
            for half in range(DE // HF):
                s = unit % 2
                fs = slice(half * HF, (half + 1) * HF)
                p.dma("sp", stg[0][:], wg_d[e, :, fs].rearrange("(k p) f -> p k f", p=128), writes=[b_stg[0]])
                p.dma("act", stg[1][:], wu_d[e, :, fs].rearrange("(k p) f -> p k f", p=128), writes=[b_stg[1]])
                p.dma("sp", stg[2][:].rearrange("p k f -> p (k f)").rearrange("p (c d) -> p c d", d=D),
                      wd_d[e, fs, :].rearrange("(c p) d -> p c d", p=128), writes=[b_stg[2]])
                p.copy("act", wgt[s][:], stg[0][:], reads=[b_stg[0]], writes=[b_we[s]])
                p.copy("act", wut[s][:], stg[1][:], reads=[b_stg[1]], writes=[b_we[s]])
                p.copy("pool", wdt[s][:], stg[2][:].rearrange("p k f -> p (k f)").rearrange("p (c d) -> p c d", d=D),
                       reads=[b_stg[2]], writes=[b_we[s]])
                for tc in range(CH // 512):
                    tsl = slice(tc * 512, (tc + 1) * 512)
                    gsl = slice(c * CH + tc * 512, c * CH + (tc + 1) * 512)
                    hs = it % 2; it += 1
                    p.dma("sp", gwb[hs][:], gw_d[e:e + 1, gsl].partition_broadcast(128), writes=[b_gwb[hs]])
                    for fc in range(HF // 128):
                        fsl = slice(fc * 128, (fc + 1) * 128)
                        for k in range(KC):
                            p.mm(psG[:], wgt[s][:, k, fsl], hbf[:, k, tsl], k == 0, k == KC - 1, reads=[b_we[s], b_h], writes=[b_psG])
                        for k in range(KC):
                            p.mm(psU[:], wut[s][:, k, fsl], hbf[:, k, tsl], k == 0, k == KC - 1, reads=[b_we[s], b_h], writes=[b_psU])
                        p.act(sg[:], psG[:], AF.Silu, reads=[b_psG], writes=[b_sg])
                        p.tt("dve", sg[:], sg[:], psU[:], ALU.mult, reads=[b_sg, b_psU], writes=[b_sg])
                        p.tt("pool", hw[hs][:, fc, :], sg[:], gwb[hs][:], ALU.mult, reads=[b_sg, b_gwb[hs]], writes=[b_hw[hs]])
                    first = (e == 0 and half == 0)
                    for dc in range(KC):
                        q = dc % 2
                        for fc in range(HF // 128):
                            p.mm(pp[q][:], wdt[s][:, fc, dc * 128:(dc + 1) * 128], hw[hs][:, fc, :], fc == 0, fc == HF // 128 - 1,
                                 reads=[b_we[s], b_hw[hs]], writes=[b_pp[q]])
                        if first:
                            p.copy("act", acc[tc][:, dc, :], pp[q][:], reads=[b_pp[q]], writes=[b_acc[tc]])
                        else:
                            p.tt("dve", acc[tc][:, dc, :], acc[tc][:, dc, :], pp[q][:], ALU.add, reads=[b_pp[q], b_acc[tc]], writes=[b_acc[tc]])
                unit += 1
        for tc in range(CH // 512):
            gsl = slice(c * CH + tc * 512, c * CH + (tc + 1) * 512)
            for k4 in range(4):
                p.dma("sp", yv[:, k4 * 4:(k4 + 1) * 4, gsl], acc[tc][:, k4 * 4:(k4 + 1) * 4, :], reads=[b_acc[tc]], is_output=True)
    return p.finish()


def build_postB(layer):
    last = (layer == 1)
    p = Prog()
    x1T = p.dram_in("x1T", [D, TL]); yP = p.dram_in("yP", [8, D, TL])
    vecs = p.dram_in("vecs", [128, 4, KC])
    xt = p.sb("xt", [128, KC, TL], F32); b_x = p.buf()
    vt = p.sb("vt", [128, 4, KC], F32); b_v = p.buf()
    coef = p.sb("coef", [128, KC], F32)
    scratch = alloc_rms_scratch(p)
    KG = 2
    ysum = p.sb("ysum", [128, KG, TL], F32); b_ys = p.buf()
    yt = [p.sb("yt%d" % i, [128, KG, TL], F32) for i in range(2)]; b_yt = [p.buf(), p.buf()]
    xv = x1T.rearrange("(k p) t -> p k t", p=128)
    for k4 in range(4):
        p.dma("sp", xt[:, k4 * 4:(k4 + 1) * 4, :], xv[:, k4 * 4:(k4 + 1) * 4, :], writes=[b_x])
    p.dma("sp", vt[:], vecs, writes=[b_v])
    p.ts("dve", coef[:], vt[:, 2, :], 1.0, None, ALU.add, None, reads=[b_v], writes=[b_v])
    p.tt("dve", coef[:], coef[:], vt[:, 1, :], ALU.mult, reads=[b_v], writes=[b_v])
    it = 0
    for k4 in range(KC // KG):
        ks = slice(k4 * KG, (k4 + 1) * KG)
        for g in range(8):
            s = it % 2; it += 1
            yv = yP[g].rearrange("(k p) t -> p k t", p=128)
            if g == 0:
                p.dma("sp", ysum[:], yv[:, ks, :], writes=[b_ys])
            else:
                p.dma("sp" if s == 0 else "act", yt[s][:], yv[:, ks, :], writes=[b_yt[s]])
                p.tt("dve" if g % 2 else "pool", ysum[:], ysum[:], yt[s][:], ALU.add, reads=[b_yt[s], b_ys], writes=[b_ys])
        for kk in range(KG):
            k = k4 * KG + kk
            p.op("dve", lambda t, k=k, kk=kk: t.scalar_tensor_tensor(out=xt[:, k, :], in0=ysum[:, kk, :], scalar=vt[:, 0, k:k + 1], in1=xt[:, k, :],
                                                                    op0=ALU.mult, op1=ALU.add), reads=[b_ys, b_v, b_x], writes=[b_x])
    if last:
        out = p.dram_out("outT", [D, TL])
        fo = p.sb("fo", [128, KC, TL], F32); b_fo = p.buf()
        emit_rms_plain(p, xt, b_x, vt[:, 1, :], b_v, fo, b_fo, TL, scratch)
        ov = out.rearrange("(k p) t -> p k t", p=128)
        for k4 in range(4):
            p.dma("sp", ov[:, k4 * 4:(k4 + 1) * 4, :], fo[:, k4 * 4:(k4 + 1) * 4, :], reads=[b_fo], is_output=True)
    else:
        x2T = p.dram_out("x2T", [D, TL]); w_next = p.dram_in("w_next", [D, ODD_COLS]); projN = p.dram_out("projN", [ODD_COLS, TL])
        hbf = p.sb("hbf", [128, KC, TL], BF16); b_h = p.buf()
        ov = x2T.rearrange("(k p) t -> p k t", p=128)
        for k4 in range(4):
            p.dma("sp", ov[:, k4 * 4:(k4 + 1) * 4, :], xt[:, k4 * 4:(k4 + 1) * 4, :], reads=[b_x], is_output=True)
        emit_rms_mod(p, xt, b_x, coef, vt[:, 3, :], b_v, hbf, b_h, TL, scratch)
        emit_proj(p, hbf, b_h, w_next, ODD_COLS, TL, projN, stage=(scratch[2], scratch[6]))
    return p.finish()


def stage_post(inp, mod, layer, xT_full, mixT_full, gateT_full=None, dbg=None):
    ident = np.eye(128, dtype=np.float32)
    m = mod[layer]
    g1, sh2, sc2, g2 = m[2 * D:3 * D], m[3 * D:4 * D], m[4 * D:5 * D], m[5 * D:6 * D]
    z = np.zeros(D, np.float32)
    w_out = inp["ev_w_out"][0] if layer == 0 else inp["od_w_out"][0]
    vecsA = np.ascontiguousarray(np.stack([fm(v) for v in (g1, inp["norm_ffn_g"][layer], sc2, sh2)], axis=1))
    wr = np.ascontiguousarray(np.concatenate([inp["moe_w_grp"][layer], inp["moe_w_exp"][layer]], axis=1))
    br = np.ascontiguousarray(np.tile(np.concatenate([inp["moe_b_grp"][layer], inp["moe_b_exp"][layer]])[None, :], (128, 1)))
    maps = []
    for c in range(NCORE):
        sl = slice(c * TL, (c + 1) * TL)
        mp = {"xT": np.ascontiguousarray(xT_full[:, sl]), "mixT": np.ascontiguousarray(mixT_full[:, sl]), "w_out": w_out, "vecs": vecsA,
              "wr": wr, "br": br, "ident": ident}
        if layer == 1:
            mp["gateT"] = np.ascontiguousarray(gateT_full[:, sl])
        maps.append(mp)
    res = run_spmd(build_postA(layer), maps)
    x1T = np.concatenate([r["x1T"] for r in res], axis=1)
    hfT = np.concatenate([r["hfT"] for r in res], axis=1)
    gwT = np.concatenate([r["gwT"] for r in res], axis=1)
    if dbg is not None:
        dbg["x1T"], dbg["hfT"], dbg["gwT"] = x1T, hfT, gwT
    maps = []
    for g in range(NCORE):
        es = slice(g * 8, (g + 1) * 8)
        maps.append({"hfT": hfT, "gw": np.ascontiguousarray(gwT[es]), "wg": inp["moe_w_gate"][layer][es],
                     "wu": inp["moe_w_up"][layer][es], "wd": inp["moe_w_down"][layer][es]})
    res = run_spmd(build_moe(), maps)
    yP = np.stack([r["yT"] for r in res], axis=0)
    if dbg is not None:
        dbg["yP"] = yP
    if layer == 0:
        mn = mod[1]
        vecsB = np.ascontiguousarray(np.stack([fm(v) for v in (g2, inp["norm_mix_g"][1], mn[D:2 * D], mn[0:D])], axis=1))
    else:
        vecsB = np.ascontiguousarray(np.stack([fm(v) for v in (g2, inp["final_g"], z, z)], axis=1))
    maps = []
    for c in range(NCORE):
        sl = slice(c * TL, (c + 1) * TL)
        mp = {"x1T": np.ascontiguousarray(x1T[:, sl]), "yP": np.ascontiguousarray(yP[:, :, sl]), "vecs": vecsB}
        if layer == 0:
            mp["w_next"] = inp["od_w_in"][0]
        maps.append(mp)
    res = run_spmd(build_postB(layer), maps)
    if layer == 0:
        return (np.concatenate([r["x2T"] for r in res], axis=1), np.concatenate([r["projN"] for r in res], axis=1))
    return np.concatenate([r["outT"] for r in res], axis=1)

HC = 32
SEG = 1024


def build_L4(nseg=T // SEG):
    p = Prog()
    qT = p.dram_in("qT", [2, 128, T]); fT = p.dram_in("fT", [2, 128, T])
    vtok_d = p.dram_in("vtok", [2, HC, T // HC, 128])
    lbl_d = p.dram_in("lbl", [128, 2, 2])
    gn_d = p.dram_in("gn", [128, 2])
    rflag_d = p.dram_in("rflag", [128, SEG]); cmask_d = p.dram_in("cmask", [HC, HC]); ident_d = p.dram_in("ident", [128, 128])
    oT = p.dram_out("oT", [2, 128, T])
    NCH = SEG // HC
    ident = p.sb("ident", [128, 128], F32); rflag = p.sb("rflag", [128, SEG], F32); cmask = p.sb("cmask", [HC, HC], F32)
    onesf = p.sb("onesf", [128, 128], F32); epst = p.sb("epst", [128, 1], F32)
    lbl = p.sb("lbl", [128, 2, 2], F32); lb = p.sb("lb", [128, 2], F32); oml = p.sb("oml", [128, 2], F32); gn = p.sb("gn", [128, 2], F32)
    b_c = p.buf()
    p.dma("sp", ident[:], ident_d, writes=[b_c]); p.dma("sp", rflag[:], rflag_d, writes=[b_c]); p.dma("sp", cmask[:], cmask_d, writes=[b_c])
    p.dma("sp", lbl[:], lbl_d, writes=[b_c]); p.dma("sp", gn[:], gn_d, writes=[b_c])
    p.memset("dve", onesf[:], 1.0, [b_c]); p.memset("dve", epst[:], EPS, [b_c])
    p.tt("dve", lb[:], lbl[:, :, 1], lbl[:, :, 0], ALU.subtract, reads=[b_c], writes=[b_c])
    p.act(lb[:], lb[:], AF.Sigmoid, reads=[b_c], writes=[b_c])
    p.ts("dve", oml[:], lb[:], -1.0, 1.0, ALU.mult, ALU.add, reads=[b_c], writes=[b_c])
    psT = p.ps("psT", [128, 512]); b_psT = p.buf()
    psN = p.ps("psN", [128, 512]); b_psN = p.buf()
    H = []
    for hd in range(2):
        n = "h%d_" % hd
        d = dict(
            A=p.sb(n + "A", [128, SEG], F32), B=p.sb(n + "B", [128, SEG], F32), C=p.sb(n + "C", [128, SEG], F32),
            Bt=p.sb(n + "Bt", [128, SEG], F32), E=p.sb(n + "E", [128, SEG], F32), KA=p.sb(n + "KA", [128, SEG], F32),
            KD=p.sb(n + "KD", [128, SEG], F32), OS=p.sb(n + "OS", [128, SEG], F32),
            qe=p.sb(n + "qe", [128, SEG], BF16), ka=p.sb(n + "ka", [128, SEG], BF16),
            vt=p.sb(n + "vt", [HC, NCH, 128], BF16), kt=p.sb(n + "kt", [HC, NCH, 128], BF16),
            ebl=p.sb(n + "ebl", [128, NCH], F32), S32=p.sb(n + "S32", [128, 128], F32), Sbf=p.sb(n + "Sbf", [128, 128], BF16),
            attm=p.sb(n + "attm", [HC, HC], BF16), rs=p.sb(n + "rs", [128, 512], F32), oo=p.sb(n + "oo", [128, 512], F32),
            psA=p.ps(n + "psA", [128, 512]), psO=p.ps(n + "psO", [128, 512]), psS=p.ps(n + "psS", [128, 512]),
        )
        for k in ("A", "B", "C", "Bt", "E", "KA", "KD", "OS", "qe", "ka", "vt", "kt", "ebl", "S", "attm", "rs", "oo", "psA", "psO", "psS"):
            d["b_" + k] = p.buf()
        p.memset("dve", d["S32"][:], 0.0, [d["b_S"]])
        p.memset("dve", d["Sbf"][:], 0.0, [d["b_S"]])
        H.append(d)
    for sgi in range(nseg):
        tsl = slice(sgi * SEG, (sgi + 1) * SEG)
        for hd in range(2):
            d = H[hd]
            p.dma("sp", d["A"][:], qT[hd, :, tsl], writes=[d["b_A"]])
            p.dma("act", d["B"][:], fT[hd, :, tsl], writes=[d["b_B"]])
            p.dma("pool", d["vt"][:], vtok_d[hd, :, sgi * NCH:(sgi + 1) * NCH, :], writes=[d["b_vt"]])
            p.act(d["B"][:], d["B"][:], AF.Sigmoid, reads=[d["b_B"]], writes=[d["b_B"]])
            p.ts("dve", d["B"][:], d["B"][:], oml[:, hd:hd + 1], lb[:, hd:hd + 1], ALU.mult, ALU.add, reads=[d["b_B"], b_c], writes=[d["b_B"]])
            p.ts("dve", d["B"][:], d["B"][:], 1e-30, None, ALU.max, None, reads=[d["b_B"]], writes=[d["b_B"]])
            p.act(d["C"][:], d["B"][:], AF.Ln, reads=[d["b_B"]], writes=[d["b_C"]])
            p.op("dve", lambda t, d=d: t.tensor_tensor_scan(out=d["Bt"][:], data0=rflag[:], data1=d["C"][:], initial=0.0, op0=ALU.mult, op1=ALU.add),
                 reads=[d["b_C"], b_c], writes=[d["b_Bt"]])
            p.act(d["E"][:], d["Bt"][:], AF.Exp, reads=[d["b_Bt"]], writes=[d["b_E"]])
            p.act(d["A"][:], d["A"][:], AF.Silu, reads=[d["b_A"]], writes=[d["b_A"]])
            p.tt("dve", d["qe"][:], d["A"][:], d["E"][:], ALU.mult, reads=[d["b_A"], d["b_E"]], writes=[d["b_qe"]])
            p.act(d["E"][:], d["Bt"][:], AF.Exp, reads=[d["b_Bt"]], writes=[d["b_E"]], scale=-1.0)
            p.ts("dve", d["B"][:], d["B"][:], -1.0, 1.0, ALU.mult, ALU.add, reads=[d["b_B"]], writes=[d["b_B"]])
            p.tt("dve", d["KA"][:], d["B"][:], d["E"][:], ALU.mult, reads=[d["b_B"], d["b_E"]], writes=[d["b_KA"]])
            p.copy("pool", d["ka"][:], d["KA"][:], reads=[d["b_KA"]], writes=[d["b_ka"]])
            p.act(d["ebl"][:], d["Bt"][:].rearrange("p (c t) -> p c t", t=HC)[:, :, HC - 1], AF.Exp, reads=[d["b_Bt"]], writes=[d["b_ebl"]])
            for c in range(NCH):
                cs = slice(c * HC, (c + 1) * HC)
                p.ts("dve" if c % 2 == 0 else "pool", d["KD"][:, cs], d["KA"][:, cs], d["ebl"][:, c:c + 1], None, ALU.mult, None,
                     reads=[d["b_KA"], d["b_ebl"]], writes=[d["b_KD"]])
            for c in range(NCH):
                cs = slice(c * HC, (c + 1) * HC)
                p.transpose(psT[:HC, :128], d["KD"][:, cs], ident[:], reads=[d["b_KD"], b_c], writes=[b_psT])
                p.copy("act" if c % 2 == 0 else "dve", d["kt"][:, c, :], psT[:HC, :128], reads=[b_psT], writes=[d["b_kt"]])
        for c in range(NCH):
            cs = slice(c * HC, (c + 1) * HC)
            for hd in range(2):
                d = H[hd]
                p.mm(d["psA"][:HC, :HC], d["ka"][:, cs], d["qe"][:, cs], True, True, reads=[d["b_ka"], d["b_qe"]], writes=[d["b_psA"]])
                p.tt("dve", d["attm"][:], d["psA"][:HC, :HC], cmask[:], ALU.mult, reads=[d["b_psA"], b_c], writes=[d["b_attm"]])
                p.mm(d["psO"][:, :HC], d["Sbf"][:], d["qe"][:, cs], True, False, reads=[d["b_S"], d["b_qe"]], writes=[d["b_psO"]])
                p.mm(d["psO"][:, :HC], d["vt"][:, c, :], d["attm"][:], False, True, reads=[d["b_vt"], d["b_attm"]], writes=[d["b_psO"]])
                p.copy("act", d["OS"][:, cs], d["psO"][:, :HC], reads=[d["b_psO"]], writes=[d["b_OS"]])
                p.mm(d["psS"][:, :128], d["kt"][:, c, :], d["vt"][:, c, :], True, True, reads=[d["b_kt"], d["b_vt"]], writes=[d["b_psS"]])
                p.op("dve", lambda t, d=d, c=c: t.scalar_tensor_tensor(out=d["S32"][:], in0=d["S32"][:], scalar=d["ebl"][:, c:c + 1],
                                                                      in1=d["psS"][:, :128], op0=ALU.mult, op1=ALU.add),
                     reads=[d["b_psS"], d["b_ebl"], d["b_S"]], writes=[d["b_S"]])
                p.copy("pool", d["Sbf"][:], d["S32"][:], reads=[d["b_S"]], writes=[d["b_S"]])
        for hd in range(2):
            d = H[hd]
            p.act(d["C"][:], d["OS"][:], AF.Square, reads=[d["b_OS"]], writes=[d["b_C"]])
            for blk in range(SEG // 512):
                bs = slice(blk * 512, (blk + 1) * 512)
                p.mm(psN[:], onesf[:], d["C"][:, bs], True, True, reads=[b_c, d["b_C"]], writes=[b_psN])
                p.act(d["rs"][:], psN[:], AF.Sqrt, reads=[b_psN, b_c], writes=[d["b_rs"]], scale=1.0 / 128, bias=epst[:])
                p.op("dve", lambda t, d=d: t.reciprocal(out=d["rs"][:], in_=d["rs"][:]), reads=[d["b_rs"]], writes=[d["b_rs"]])
                p.op("dve", lambda t, d=d, bs=bs, hd=hd: t.scalar_tensor_tensor(out=d["oo"][:], in0=d["OS"][:, bs], scalar=gn[:, hd:hd + 1],
                                                                               in1=d["rs"][:], op0=ALU.mult, op1=ALU.mult),
                     reads=[d["b_OS"], d["b_rs"], b_c], writes=[d["b_oo"]])
                p.dma("sp", oT[hd, :, sgi * SEG + blk * 512:sgi * SEG + (blk + 1) * 512], d["oo"][:], reads=[d["b_oo"]], is_output=True)
    return p.finish()


def stage_L4(inp, projN, nseg=T // SEG):
    nc = build_L4(nseg)
    rflag = np.ones((128, SEG), np.float32); rflag[:, ::HC] = 0.0
    s = np.arange(HC)
    cmask = (s[:, None] <= s[None, :]).astype(np.float32)
    ident = np.eye(128, dtype=np.float32)
    maps = []
    for c in range(NCORE):
        hs = [2 * c, 2 * c + 1]
        qT = np.ascontiguousarray(np.stack([projN[h * 128:(h + 1) * 128] for h in hs]))
        fT = np.ascontiguousarray(np.stack([projN[2048 + h * 128:2048 + (h + 1) * 128] for h in hs]))
        vt = np.stack([projN[4096 + h * 128:4096 + (h + 1) * 128].T.reshape(T // HC, HC, 128).transpose(1, 0, 2) for h in hs])
        lbl = np.stack([inp["hg_lb_logits"][:, h * 128:(h + 1) * 128].T for h in hs], axis=1)
        gn = np.stack([inp["hg_norm_g"][0][h * 128:(h + 1) * 128] for h in hs], axis=1)
        maps.append({"qT": qT, "fT": fT, "vtok": np.ascontiguousarray(vt), "lbl": np.ascontiguousarray(lbl.astype(np.float32)),
                     "gn": np.ascontiguousarray(gn.astype(np.float32)), "rflag": rflag, "cmask": cmask, "ident": ident})
    res = run_spmd(nc, maps)
    return np.concatenate([r["oT"].reshape(256, T) for r in res], axis=0)


def kernel(**inp):
    import os, time
    dbgdir = os.environ.get("MK_DEBUG_DIR")
    t0 = time.time()

    def dump(name, a):
        print("[mk] %s done at %.0fs" % (name, time.time() - t0), flush=True)
        if dbgdir:
            np.save(os.path.join(dbgdir, "kd_%s.npy" % name), a)
    inp = {k: np.asarray(v) for k, v in inp.items()}
    mod = stage_L0(inp); dump("mod", mod)
    projT = stage_L1(inp, mod); dump("projT", projT)
    nsaT = stage_L2(inp, projT); dump("nsaT", nsaT)
    lruT = stage_L2b(inp, projT); dump("lruT", lruT)
    xT0 = np.ascontiguousarray(inp["x"][0].T)
    mixT = np.concatenate([nsaT, lruT], axis=0)
    dbg = {} if dbgdir else None
    x2T, projN = stage_post(inp, mod, 0, xT0, mixT, dbg=dbg); dump("x2T", x2T); dump("projN", projN)
    if dbg:
        for k, v in dbg.items():
            dump("L0_" + k, v)
    oT = stage_L4(inp, projN); dump("oT", oT)
    dbg = {} if dbgdir else None
    outT = stage_post(inp, mod, 1, x2T, oT, gateT_full=projN[6144:8192], dbg=dbg); dump("outT", outT)
    if dbg:
        for k, v in dbg.items():
            if k != "yP":
                dump("L1_" + k, v)
    return np.ascontiguousarray(outT.T)[None].astype(np.float32)
```
